# Optimizing a Trainium2 kernel written in Bass

```python
import jax, jax.numpy as jnp
from jax import lax
import numpy as np

D_MODEL = 1024
BATCH = 16
SEQ = 4096
DEPTH = 2

CHUNK = 64
CONV_CH = 512
CONV_WIDTH = 31
ATT_HEADS = 8
HEAD_DIM = 64
ATT_WIDTH = ATT_HEADS * HEAD_DIM
IDX_HEADS = 8
IDX_DIM = 64
TOPK_MAX = 256
Q_BLOCK = CHUNK
MIX_WIDTH = CONV_CH + ATT_WIDTH
IN0_WIDTH = 2 * CONV_CH + 3 * ATT_WIDTH + IDX_HEADS * IDX_DIM + IDX_DIM + IDX_HEADS
SC_WIDTH = 1024
SC_CONV_WIDTH = 3
D_FF = 2816
N_EXPERTS = 8
TOP_K_EXPERTS = 2
D_FF_EXPERT = 2816
N_EVEN = (DEPTH + 1) // 2
N_ODD = DEPTH // 2
EPS = 1e-6

kernel_name = "chunk_causal_conformer_dsa_shortconv_moe_adaln"


def _split(t, sizes):
    out, o = [], 0
    for n in sizes:
        out.append(t[..., o:o + n])
        o += n
    return out


def rmsnorm(x, g):
    xf = x.astype(jnp.float32)
    y = xf * lax.rsqrt(jnp.mean(xf * xf, axis=-1, keepdims=True) + EPS)
    return (y * g.astype(jnp.float32)).astype(x.dtype)


def layernorm(x, g, b):
    xf = x.astype(jnp.float32)
    mu = jnp.mean(xf, axis=-1, keepdims=True)
    var = jnp.mean(jnp.square(xf - mu), axis=-1, keepdims=True)
    y = (xf - mu) * lax.rsqrt(var + EPS)
    return (y * g.astype(jnp.float32) + b.astype(jnp.float32)).astype(x.dtype)


def causal_depthwise_conv(x, w):
    W, C = w.shape
    return lax.conv_general_dilated(
        x, w[:, None, :], window_strides=(1,), padding=[(W - 1, 0)],
        dimension_numbers=("NWC", "WIO", "NWC"), feature_group_count=C)


def adaln(c, w_ada, b_ada):
    mod = jax.nn.silu(c) @ w_ada + b_ada
    return _split(mod[:, None, :], [D_MODEL] * 6)


def swiglu(h, w_gate, w_up, w_down):
    return (jax.nn.silu(h @ w_gate) * (h @ w_up)) @ w_down


def dsa_attention(q, k, v, qi, ki, wi):
    B_, S, H, Dh = q.shape
    topk = min(TOPK_MAX, S // 4)
    nblk = S // Q_BLOCK
    key_pos = jnp.arange(S)
    gather = jax.vmap(lambda t, idx: t[idx])

    def to_blocks(t):
        return t.reshape(B_, nblk, Q_BLOCK, *t.shape[2:]).swapaxes(0, 1)

    def block(args):
        blk, qb, qib, wib = args
        q_pos = blk * Q_BLOCK + jnp.arange(Q_BLOCK)
        limit = (q_pos // CHUNK + 1) * CHUNK
        admissible = key_pos[None, :] < limit[:, None]
        dots = jnp.einsum("bqhd,bsd->bqhs", qib, ki).astype(jnp.float32)
        score = jnp.einsum("bqh,bqhs->bqs", wib.astype(jnp.float32), jax.nn.relu(dots))
        score = jnp.where(admissible[None], score, -jnp.inf)
        _, idx = lax.top_k(score, topk)
        valid = idx < limit[None, :, None]
        k_sel = gather(k, idx)
        v_sel = gather(v, idx)
        logits = jnp.einsum("bqhd,bqkhd->bhqk", qb, k_sel).astype(jnp.float32) * (Dh ** -0.5)
        logits = jnp.where(valid[:, None], logits, -jnp.inf)
        p = jax.nn.softmax(logits, axis=-1).astype(v.dtype)
        return jnp.einsum("bhqk,bqkhd->bqhd", p, v_sel)

    out = lax.map(block, (jnp.arange(nblk), to_blocks(q), to_blocks(qi), to_blocks(wi)))
    return out.swapaxes(0, 1).reshape(B_, S, H, Dh)


def conv_attn_mixer(h, w_in, conv_w, conv_b, cn_g, cn_b, q_g, k_g, w_out):
    B_, S, _ = h.shape
    proj = h @ w_in
    u, q, k, v, qi, ki, wi = _split(
        proj, [2 * CONV_CH, ATT_WIDTH, ATT_WIDTH, ATT_WIDTH, IDX_HEADS * IDX_DIM, IDX_DIM, IDX_HEADS])
    a, g = _split(u, [CONV_CH, CONV_CH])
    a = a * jax.nn.sigmoid(g)
    a = causal_depthwise_conv(a, conv_w) + conv_b
    a = jax.nn.silu(layernorm(a, cn_g, cn_b))
    q = rmsnorm(q.reshape(B_, S, ATT_HEADS, HEAD_DIM), q_g)
    k = rmsnorm(k.reshape(B_, S, ATT_HEADS, HEAD_DIM), k_g)
    v = v.reshape(B_, S, ATT_HEADS, HEAD_DIM)
    qi = qi.reshape(B_, S, IDX_HEADS, IDX_DIM)
    wi = wi * ((IDX_DIM * IDX_HEADS) ** -0.5)
    att = dsa_attention(q, k, v, qi, ki, wi).reshape(B_, S, ATT_WIDTH)
    return jnp.concatenate([a, att], axis=-1) @ w_out


def short_conv_mixer(h, w_in, conv_w, w_out):
    bg, cg, v = _split(h @ w_in, [SC_WIDTH] * 3)
    return (bg * causal_depthwise_conv(cg * v, conv_w)) @ w_out


def moe_swiglu(h, w_router, w_gate, w_up, w_down):
    B_, S, D = h.shape
    t = h.reshape(-1, D)
    logits = (t @ w_router).astype(jnp.float32)
    top_val, top_idx = lax.top_k(logits, TOP_K_EXPERTS)
    top_w = jax.nn.softmax(top_val, axis=-1)
    gates = jnp.sum(jax.nn.one_hot(top_idx, N_EXPERTS, dtype=jnp.float32) * top_w[..., None], axis=1)
    gates = gates.astype(t.dtype)
    out = jnp.zeros_like(t)
    for e in range(N_EXPERTS):
        out = out + gates[:, e:e + 1] * swiglu(t, w_gate[e], w_up[e], w_down[e])
    return out.reshape(B_, S, D)


def setup_inputs(seed: int = 0) -> dict:
    key = jax.random.key(seed)
    ks = iter(jax.random.split(key, 32))
    f32 = jnp.float32

    def nrm(shape, scale):
        return jax.random.normal(next(ks), shape, f32) * scale

    D = D_MODEL
    return {
        "x": nrm((BATCH, SEQ, D), 1.0),
        "c": nrm((BATCH, D), 1.0),
        "ada_w": nrm((DEPTH, D, 6 * D), 0.5 * D ** -0.5),
        "ada_b": nrm((DEPTH, 6 * D), 0.02),
        "norm_g": 1.0 + nrm((DEPTH, 2, D), 0.1),
        "ab_w_in": nrm((N_EVEN, D, IN0_WIDTH), D ** -0.5),
        "ab_conv_w": nrm((N_EVEN, CONV_WIDTH, CONV_CH), CONV_WIDTH ** -0.5),
        "ab_conv_b": nrm((N_EVEN, CONV_CH), 0.02),
        "ab_cnorm_g": 1.0 + nrm((N_EVEN, CONV_CH), 0.1),
        "ab_cnorm_b": nrm((N_EVEN, CONV_CH), 0.02),
        "ab_q_g": 1.0 + nrm((N_EVEN, HEAD_DIM), 0.1),
        "ab_k_g": 1.0 + nrm((N_EVEN, HEAD_DIM), 0.1),
        "ab_w_out": nrm((N_EVEN, MIX_WIDTH, D), MIX_WIDTH ** -0.5),
        "ffn_w_gate": nrm((N_EVEN, D, D_FF), D ** -0.5),
        "ffn_w_up": nrm((N_EVEN, D, D_FF), D ** -0.5),
        "ffn_w_down": nrm((N_EVEN, D_FF, D), D_FF ** -0.5),
        "sc_w_in": nrm((N_ODD, D, 3 * SC_WIDTH), D ** -0.5),
        "sc_conv_w": nrm((N_ODD, SC_CONV_WIDTH, SC_WIDTH), SC_CONV_WIDTH ** -0.5),
        "sc_w_out": nrm((N_ODD, SC_WIDTH, D), SC_WIDTH ** -0.5),
        "moe_router": nrm((N_ODD, D, N_EXPERTS), D ** -0.5),
        "moe_w_gate": nrm((N_ODD, N_EXPERTS, D, D_FF_EXPERT), D ** -0.5),
        "moe_w_up": nrm((N_ODD, N_EXPERTS, D, D_FF_EXPERT), D ** -0.5),
        "moe_w_down": nrm((N_ODD, N_EXPERTS, D_FF_EXPERT, D), D_FF_EXPERT ** -0.5),
    }


def reference(x, c, ada_w, ada_b, norm_g, ab_w_in, ab_conv_w, ab_conv_b, ab_cnorm_g,
              ab_cnorm_b, ab_q_g, ab_k_g, ab_w_out, ffn_w_gate, ffn_w_up, ffn_w_down,
              sc_w_in, sc_conv_w, sc_w_out, moe_router, moe_w_gate, moe_w_up, moe_w_down):
    for i in range(DEPTH):
        shift1, scale1, gate1, shift2, scale2, gate2 = adaln(c, ada_w[i], ada_b[i])
        h = rmsnorm(x, norm_g[i, 0]) * (1.0 + scale1) + shift1
        if i % 2 == 0:
            j = i // 2
            mix = conv_attn_mixer(h, ab_w_in[j], ab_conv_w[j], ab_conv_b[j], ab_cnorm_g[j],
                                  ab_cnorm_b[j], ab_q_g[j], ab_k_g[j], ab_w_out[j])
            x = x + gate1 * mix
            h = rmsnorm(x, norm_g[i, 1]) * (1.0 + scale2) + shift2
            x = x + gate2 * swiglu(h, ffn_w_gate[j], ffn_w_up[j], ffn_w_down[j])
        else:
            j = i // 2
            mix = short_conv_mixer(h, sc_w_in[j], sc_conv_w[j], sc_w_out[j])
            x = x + gate1 * mix
            h = rmsnorm(x, norm_g[i, 1]) * (1.0 + scale2) + shift2
            x = x + gate2 * moe_swiglu(h, moe_router[j], moe_w_gate[j], moe_w_up[j], moe_w_down[j])
    return x
```

```python
import numpy as np
from contextlib import ExitStack
import concourse.bass as bass
import concourse.mybir as mybir
from concourse.bass_utils import run_bass_kernel_spmd

F32 = mybir.dt.float32
BF16 = mybir.dt.bfloat16
ALU = mybir.AluOpType
AF = mybir.ActivationFunctionType
AX = mybir.AxisListType

D = 1024
S = 4096
T = 512
NT = S // T
DFF = 2816
NJ = DFF // 128
INW = 3144
NE = 8
EPS = 1e-6
NIT = 16
TOPK = 256

CH = 4096
NLANES = 8


class Buf:
    __slots__ = ("name", "w", "r")

    def __init__(self, name=""):
        self.name = name
        self.w = None
        self.r = {}


class Prog:
    ENG = ("pe", "act", "dve", "pool", "sp")

    def __init__(self, nc):
        self.nc = nc
        self.q = {e: [] for e in self.ENG}
        self.n = {e: 0 for e in self.ENG}
        self.seen = {e: {} for e in self.ENG}
        self.sems = {}
        self.lane_rr = {e: 0 for e in self.ENG}
        self.lane_val = {}
        self.used_lanes = set()

    @staticmethod
    def _kv(t):
        if t[0] == "eng":
            return ("eng", t[1]), t[2]
        return ("dma", t[1], t[2]), t[3]

    def _collect(self, eng, reads, writes):
        out = {}

        def add(kind, t):
            key, val = self._kv(t)
            if t[0] == "eng" and t[1] == eng:
                if eng == "pe" or kind != "raw":
                    return
            if self.seen[eng].get(key, -1) >= val:
                return
            if out.get(key, -1) < val:
                out[key] = val

        for b in reads:
            if b.w is not None:
                add("raw", b.w)
        for b in writes:
            if b.w is not None:
                add("waw", b.w)
            for t in b.r.values():
                add("war", t)
        return out

    def _emit_waits(self, eng, waits):
        for key, val in waits.items():
            self.seen[eng][key] = val
            if key[0] == "eng":
                seg, off = divmod(val, CH)
                self.q[eng].append(("wait", ("eng", key[1], seg), off + 1))
            else:
                self.q[eng].append(("wait", key, val))

    def _update(self, tok, reads, writes):
        key, _ = self._kv(tok)
        for b in reads:
            b.r[key] = tok
        for b in writes:
            b.w = tok
            b.r = {}

    def op(self, eng, fn, reads=(), writes=()):
        self._emit_waits(eng, self._collect(eng, reads, writes))
        n = self.n[eng]
        self.n[eng] = n + 1
        self.q[eng].append(("op", fn, ("eng", eng, n // CH)))
        tok = ("eng", eng, n)
        self._update(tok, reads, writes)
        return tok

    def dma(self, eng, out, in_, reads=(), writes=(), **kw):
        lane = self.lane_rr[eng]
        self.lane_rr[eng] = (lane + 1) % NLANES
        waits = self._collect(eng, reads, writes)
        prev = self.lane_val.get((eng, lane), 0)
        key = ("dma", eng, lane)
        if prev > 0 and self.seen[eng].get(key, -1) < prev:
            waits[key] = max(waits.get(key, -1), prev)
        self._emit_waits(eng, waits)
        v = prev + 16
        self.lane_val[(eng, lane)] = v
        self.used_lanes.add((eng, lane))
        self.q[eng].append(("dma", out, in_, kw, key))
        tok = ("dma", eng, lane, v)
        self._update(tok, reads, writes)
        return tok

    def wait_tok(self, eng, tok):
        key, val = self._kv(tok)
        if self.seen[eng].get(key, -1) >= val:
            return
        self._emit_waits(eng, {key: val})

    def handoff(self, old, new):
        toks = {}
        for b in old:
            cand = list(b.r.values())
            if b.w is not None:
                cand.append(b.w)
            for t in cand:
                key, val = self._kv(t)
                if key not in toks or self._kv(toks[key])[1] < val:
                    toks[key] = t
        for b in new:
            for key, t in toks.items():
                if key not in b.r or self._kv(b.r[key])[1] < self._kv(t)[1]:
                    b.r[key] = t

    def build(self):
        nc = self.nc
        with ExitStack() as es:
            for e in self.ENG:
                nseg = (self.n[e] + CH - 1) // CH
                for s in range(nseg):
                    self.sems[("eng", e, s)] = es.enter_context(nc.semaphore(f"c_{e}_{s}"))
            for (e, lane) in sorted(self.used_lanes):
                self.sems[("dma", e, lane)] = es.enter_context(nc.semaphore(f"d_{e}_{lane}"))
            block = es.enter_context(nc.Block())
            sems = self.sems

            def run(engobj, items):
                for it in items:
                    if it[0] == "wait":
                        engobj.wait_ge(sems[it[1]], it[2])
                    elif it[0] == "op":
                        it[1](engobj).then_inc(sems[it[2]], 1)
                    else:
                        _, out, in_, kw, key = it
                        engobj.dma_start(out=out, in_=in_, **kw).then_inc(sems[key], 16)

            @block.tensor
            def _(e):
                run(e, self.q["pe"])

            @block.scalar
            def _(e):
                run(e, self.q["act"])

            @block.vector
            def _(e):
                run(e, self.q["dve"])

            @block.gpsimd
            def _(e):
                run(e, self.q["pool"])

            @block.sync
            def _(e):
                run(e, self.q["sp"])


def build_program(NSEQ=2, DO_L0=True, DO_L1=True, NTILES=NT, DEBUG=False):
    nc = bass.Bass("TRN2", target_bir_lowering=False)
    P = Prog(nc)
    NTOK = NSEQ * S

    def din(name, shape):
        return nc.dram_tensor(name, shape, F32, kind="ExternalInput").ap()

    def dscr(name, shape, dt=BF16):
        return nc.dram_tensor(name, shape, dt, kind="Internal").ap()

    x_d = din("x", [NTOK, D])
    cT_d = din("cT", [D, NSEQ])
    ada_w_d = din("ada_w", [2, D, 6 * D])
    ada_b_d = din("ada_b", [2, 6 * D])
    norm_g_d = din("norm_g", [4, D])
    w_in0_d = din("w_in0", [D, INW])
    convw_d = din("convwT", [512, 31])
    convb_d = din("convb", [128, 4])
    cng_d = din("cng", [128, 4])
    cnb_d = din("cnb", [128, 4])
    qg_d = din("qg2", [128, 1])
    kg_d = din("kg2", [128, 1])
    w_out0_d = din("w_out0", [D, D])
    ffn_g_d = din("ffn_g", [D, DFF])
    ffn_u_d = din("ffn_u", [D, DFF])
    ffn_d_d = din("ffn_d", [DFF, D])
    sc_in_d = din("sc_in", [D, 3 * D])
    sc_cw_d = din("sc_cwT", [D, 3])
    sc_out_d = din("sc_out", [D, D])
    router_d = din("router", [D, NE])
    if DO_L1:
        moe_g_d = din("moe_g", [NE, D, DFF])
        moe_u_d = din("moe_u", [NE, D, DFF])
        moe_d_d = din("moe_d", [NE, DFF, D])
    y_d = nc.dram_tensor("y", [NTOK, D], F32, kind="ExternalOutput").ap()

    s_w_in0 = dscr("s_w_in0", [D, INW])
    s_w_out0 = dscr("s_w_out0", [D, D])
    s_ffn_g = dscr("s_ffn_g", [D, DFF])
    s_ffn_u = dscr("s_ffn_u", [D, DFF])
    s_ffn_d = dscr("s_ffn_d", [DFF, D])
    s_sc_in = dscr("s_sc_in", [D, 3 * D])
    s_sc_out = dscr("s_sc_out", [D, D])
    s_moe_g = dscr("s_moe_g", [NE, D, DFF])
    s_moe_u = dscr("s_moe_u", [NE, D, DFF])
    s_moe_d = dscr("s_moe_d", [NE, DFF, D])
    modv = dscr("modv", [2, 6, NSEQ, D], F32)

    es = ExitStack()

    def sb(name, shape, dt):
        return es.enter_context(nc.sbuf_tensor(name, shape, dt))

    xt = sb("xt", [128, 4, D], F32)
    Bxt = [Buf(f"xt{i}") for i in range(4)]
    hT = sb("hT", [128, 8, T], BF16)
    BhT = Buf("hT")
    wring = sb("wring", [128, 4, 4096], BF16)
    Bw = [Buf(f"w{i}") for i in range(4)]
    modt = sb("modt", [128, D], F32)
    Bmod = Buf("mod")
    modt2 = sb("modt2", [128, D], F32)
    Bmod2 = Buf("mod2")
    ident = sb("ident", [128, 128], BF16)
    Bconst = Buf("const")
    idf = sb("idf", [128, 128], F32)
    bd = sb("bd", [128, 128], BF16)
    qz = sb("qz", [128, 4, 2, 128], BF16)
    Bqz = Buf("qz")
    onesD = sb("onesD", [128, 128], BF16)
    cneg = sb("cneg", [128, 128], F32)
    sil = sb("sil", [128, 2, T], BF16)
    Bsil = [Buf("sil0"), Buf("sil1")]
    ntmp = sb("ntmp", [128, D], F32)
    Bntmp = Buf("ntmp")
    nbf = sb("nbf", [128, D], BF16)
    Bnbf = Buf("nbf")
    sv = sb("sv", [128, 256], F32)
    Bsv = Buf("sv")
    cw = sb("cw", [128, 4, 31], F32)
    cb = sb("cb", [128, 4], F32)
    cng = sb("cng_s", [128, 4], F32)
    cnb = sb("cnb_s", [128, 4], F32)
    qg = sb("qg_s", [128, 1], F32)
    kg = sb("kg_s", [128, 1], F32)
    sccw = sb("sccw", [128, 8, 3], F32)
    Bpar = Buf("par")
    NBIG = 66048
    big = sb("big", [128, NBIG], BF16)

    def carve(off_bytes, nbytes, dt, pat=None, **kw):
        a = big[:, off_bytes // 2:(off_bytes + nbytes) // 2]
        if dt == F32:
            a = a.bitcast(F32)
        if pat:
            a = a.rearrange(pat, **kw)
        return a

    K = 1024
    o = 0
    kT = carve(o, 32 * K, BF16, "p (c s) -> p c s", c=4); o += 32 * K
    vc = carve(o, 32 * 8 * 65 * 2, BF16, "p (b f) -> p b f", b=32); o += 32 * 8 * 65 * 2
    o = (o + 63) // 64 * 64
    kiT = carve(o, 8 * K, BF16); o += 8 * K
    qT = carve(o, 4 * K, BF16, "p (c s) -> p c s", c=4); o += 4 * K
    qiT = carve(o, 4 * K, BF16, "p (c s) -> p c s", c=4); o += 4 * K
    mixT = carve(o, 8 * K, BF16, "p (c s) -> p c s", c=8); o += 8 * K
    yhist = carve(o, 4 * 32 * 2, BF16, "p (c s) -> p c s", c=4); o += 4 * 32 * 2
    wi_t = carve(o, 4 * 8 * 4, F32, "p (c s) -> p c s", c=4); o += 4 * 8 * 4
    W0 = o
    assert W0 + 40 * K <= NBIG * 2, (W0, NBIG * 2)
    BkT, Bvc, BkiT, BqT, BqiT, BmixT, Byh, Bwi = (Buf(n) for n in
                                                   ("kT", "vc", "kiT", "qT", "qiT", "mixT", "yh", "wi"))
    o = W0
    yv = carve(o, 4 * 544 * 2, BF16, "p (c s) -> p c s", c=4); o += 4 * 544 * 2
    dg = carve(o, 2 * 31 * 128 * 2, BF16, "p (b j s) -> p b j s", b=2, j=31); o += 2 * 31 * 128 * 2
    a_bf = carve(o, 4 * K, BF16, "p (c s) -> p c s", c=4); o += 4 * K
    a2_bf = carve(o, 4 * K, BF16, "p (c s) -> p c s", c=4); o += 4 * K
    mean_sb = carve(o, 2 * K, F32); o += 2 * K
    rstd_sb = carve(o, 2 * K, F32); o += 2 * K
    sgm = carve(o, 2 * K, F32); o += 2 * K
    assert o <= W0 + 40 * K, o - W0
    Byv, Babf, Ba2, Bmean, Brstd, Bsgm = (Buf(n) for n in ("yv", "abf", "a2", "mean", "rstd", "sgm"))
    Bdg = [[Buf(f"dg{b}_{j}") for j in range(31)] for b in range(2)]
    conv_bufs = [Byv, Babf, Ba2, Bmean, Brstd, Bsgm] + Bdg[0] + Bdg[1]
    o = W0
    score = carve(o, 16 * K, F32); o += 16 * K
    negm2 = carve(o, 16 * K, BF16, "p (c s) -> p c s", c=2); o += 16 * K
    rl = carve(o, 4 * K, F32, "p (c s) -> p c s", c=2); o += 4 * K
    PT = carve(o, 2 * K, BF16, "p (c s) -> p c s", c=2); o += 2 * K
    att = carve(o, 1 * K, BF16); o += 1 * K
    assert o <= W0 + 40 * K
    Bscore, Brl0, Brl1, BPT0, BPT1, Batt = (Buf(n) for n in ("score", "rl0", "rl1", "PT0", "PT1", "att"))
    Bnegm2 = [Buf("negm0"), Buf("negm1")]
    Bjd = [Buf("jd0"), Buf("jd1")]
    Bja = [Buf("ja0"), Buf("ja1")]
    attn_bufs = [Bscore, Brl0, Brl1, BPT0, BPT1, Batt] + Bnegm2 + Bjd + Bja
    act0 = carve(W0, 22 * K, BF16, "p (j s) -> p j s", j=NJ)
    Bact0 = [Buf(f"act0_{j}") for j in range(NJ)]
    l0_persist = [BkT, Bvc, BkiT, BqT, BqiT, BmixT, Byh, Bwi]
    o = 0
    wr32 = carve(o, 256, F32, "p (k e) -> p k e", e=NE)
    wr_hi = carve(o + 256, 128, BF16, "p (k e) -> p k e", e=NE)
    wr_lo = carve(o + 384, 128, BF16, "p (k e) -> p k e", e=NE)
    wrtmp = carve(o + 512, 256, F32, "p (k e) -> p k e", e=NE)
    lobf = carve(o + 1 * K, 2 * K, BF16)
    hTlo = carve(o + 3 * K, 2 * K, BF16, "p (c t) -> p c t", c=8)
    o += 32 * K
    Blo, BhTlo = Buf("lobf"), Buf("hTlo")
    act1 = carve(o, 22 * K, BF16, "p (j s) -> p j s", j=NJ); o += 22 * K
    mT = carve(o, 8 * K, BF16, "p (c s) -> p c s", c=8); o += 8 * K
    wdres = carve(o, 44 * K, BF16, "p (j n) -> p j n", j=NJ); o += 44 * K
    zt = carve(o, 516 * 4, F32); o += 516 * 4
    vsb = carve(o, 2 * K, F32); o += 2 * K
    c3 = carve(o, 2 * K, F32); o += 2 * K
    zh = carve(o, 8 * 2 * 4, F32, "p (c s) -> p c s", c=8); o += 64
    prod = carve(o, 8 * K, F32, "p (c s) -> p c s", c=2); o += 8 * K
    assert o <= NBIG * 2, o
    BwrB, BmT, Bzt, Bvsb, Bc3, Bzh = (Buf(n) for n in ("wrB", "mT", "zt", "vsb", "c3", "zh"))
    Bprod = [Buf("prod0"), Buf("prod1")]
    Bwd = [Buf(f"wd{j}") for j in range(NJ // 2)]
    Bact1 = [Buf(f"act1_{j}") for j in range(NJ)]
    l1_bufs = [BwrB, BmT, Bzt, Bvsb, Bc3, Bzh, Blo, BhTlo] + Bprod + Bact1 + Bwd

    SS, RS, MX, MN, RNG, LG, GT, W12, RD = 0, 4, 8, 9, 10, 16, 48, 80, 96
    NB2 = NIT + 2
    thr_t = sb("thr_t", [128, 2, NB2], F32)
    cnt_t = sb("cnt_t", [128, 2, NB2], F32)
    cact_t = sb("cact_t", [128, 2, NB2], F32)
    uu_t = sb("uu_t", [128, 2, NB2], F32)
    stp_t = sb("stp_t", [128, 2, NB2], F32)
    stp2_t = sb("stp2_t", [128, 2, NB2], F32)
    dd_t = sb("dd_t", [128, 2, NB2], F32)
    Bthr = [Buf("thr0"), Buf("thr1")]
    Bcd = [Buf("cd0"), Buf("cd1")]
    Bca = [Buf("ca0"), Buf("ca1")]
    Bst = [Buf("st0"), Buf("st1")]
    Bmm = [Buf("mm0"), Buf("mm1")]
    pw_t = sb("pw_t", [128, NIT + 2], F32)
    epsc = sb("epsc", [128, 1], F32)
    Bbis = Buf("bis")

    banks = [es.enter_context(nc.psum_tensor(f"ps{i}", [128, 512], F32)) for i in range(8)]
    Bbank = [Buf(f"bank{i}") for i in range(8)]
    st = {"ring": 0, "acc": 0, "w": 0}

    st.update({"ra": 0, "rb": 0})

    def ring_bank(group=None):
        if group == "a":
            i = 4 + st["ra"] % 2
            st["ra"] += 1
        elif group == "b":
            i = 6 + st["rb"] % 2
            st["rb"] += 1
        else:
            i = 4 + st["ring"] % 4
            st["ring"] += 1
        return banks[i], Bbank[i]

    def acc_pair():
        i = (st["acc"] % 2) * 2
        st["acc"] += 1
        return (banks[i], Bbank[i]), (banks[i + 1], Bbank[i + 1])

    def wslot():
        i = st["w"] % 4
        st["w"] += 1
        return wring[:, i, :], Bw[i]

    def mm(out, lhsT, rhs, start, stop, reads, writes):
        P.op("pe", lambda e: e.matmul(out, lhsT=lhsT, rhs=rhs, start=start, stop=stop), reads, writes)

    def recip3(out, in_, reads, writes):
        P.op("dve", lambda e: e.reciprocal(out=out, in_=in_), reads, writes)

    def tr(out, in_, reads, writes):
        P.op("pe", lambda e: e.transpose(out, in_, ident[:]), reads, writes)

    def act_(out, in_, func, reads, writes, **kw):
        P.op("act", lambda e: e.activation(out=out, in_=in_, func=func, **kw), reads, writes)

    def tt(eng, out, in0, in1, op, reads, writes):
        P.op(eng, lambda e: e.tensor_tensor(out=out, in0=in0, in1=in1, op=op), reads, writes)

    def ts(eng, out, in0, s1, s2, op0, op1, reads, writes, accum_out=None):
        if op1 is None:
            P.op(eng, lambda e: e.tensor_scalar(out=out, in0=in0, scalar1=s1, scalar2=None, op0=op0), reads, writes)
        elif accum_out is None:
            P.op(eng, lambda e: e.tensor_scalar(out=out, in0=in0, scalar1=s1, scalar2=s2, op0=op0, op1=op1), reads, writes)
        else:
            P.op(eng, lambda e: e.tensor_scalar(out=out, in0=in0, scalar1=s1, scalar2=s2, op0=op0, op1=op1,
                                                accum_out=accum_out), reads, writes)

    def stt(eng, out, in0, scalar, in1, op0, op1, reads, writes):
        P.op(eng, lambda e: e.scalar_tensor_tensor(out=out, in0=in0, scalar=scalar, in1=in1, op0=op0, op1=op1),
             reads, writes)

    def cp(eng, out, in_, reads, writes):
        if eng == "act":
            act_(out, in_, AF.Identity, reads, writes)
        else:
            P.op(eng, lambda e: e.tensor_copy(out=out, in_=in_), reads, writes)

    def memset(eng, ap, val, writes):
        P.op(eng, lambda e: e.memset(ap, val), (), writes)

    dbg_list = []

    def dump(name, ap, bufs):
        if not DEBUG:
            return
        if any(n == name for n, _ in dbg_list):
            return
        shp = list(ap.shape)
        dt_ = ap.dtype
        dten = nc.dram_tensor("dbg_" + name, shp, dt_, kind="ExternalOutput").ap()
        dbg_list.append((name, shp))
        P.dma("sp", dten, ap, reads=bufs)

    memset("pool", idf[:], 0.0, [Bconst])
    P.op("pool", lambda e: e.affine_select(out=idf[:], in_=idf[:], pattern=[[-1, 128]], compare_op=ALU.not_equal,
                                           fill=1.0, base=0, channel_multiplier=1), [Bconst], [Bconst])
    cp("dve", ident[:], idf[:], [Bconst], [Bconst])
    ident2 = idf[:].bitcast(BF16)
    cp("dve", ident2[:, 0:128], ident[:], [Bconst], [Bconst])
    cp("dve", ident2[:, 128:256], ident[:], [Bconst], [Bconst])
    memset("dve", bd[:], 0.0, [Bconst])
    memset("dve", bd[0:64, 0:64], 1.0 / 64, [Bconst])
    memset("dve", bd[64:128, 64:128], 1.0 / 64, [Bconst])
    memset("dve", onesD[:], 1.0 / 512, [Bconst])
    memset("dve", cneg[:], 0.0, [Bconst])
    memset("dve", cneg[0:64, 64:128], -1e30, [Bconst])
    memset("dve", epsc[:], EPS, [Bconst])
    for n in range(NIT + 2):
        memset("dve", pw_t[:, n:n + 1], 2.0 ** -(n + 2), [Bconst])
    P.dma("sp", cw[:], convw_d.rearrange("(c p) j -> p c j", p=128), writes=[Bpar])
    P.dma("sp", cb[:], convb_d, writes=[Bpar])
    P.dma("sp", cng[:], cng_d, writes=[Bpar])
    P.dma("sp", cnb[:], cnb_d, writes=[Bpar])
    P.dma("sp", qg[:], qg_d, writes=[Bpar])
    P.dma("sp", kg[:], kg_d, writes=[Bpar])
    P.dma("sp", sccw[:], sc_cw_d.rearrange("(c p) j -> p c j", p=128), writes=[Bpar])
    ts("dve", qg[:], qg[:], 0.125, None, ALU.mult, None, [Bpar], [Bpar])

    def convert(src, dst, rows, blk=256):
        bufs = []
        for r0 in range(0, rows, blk):
            r1 = min(rows, r0 + blk)
            b = Buf("cv")
            P.dma("pool", dst[r0:r1, :], src[r0:r1, :], writes=[b])
            bufs.append(b)
        return bufs

    cv = {}
    if DO_L0:
        cv["w_in0"] = convert(w_in0_d, s_w_in0, D)
        cv["w_out0"] = convert(w_out0_d, s_w_out0, D)
        cv["ffn_g"] = convert(ffn_g_d, s_ffn_g, D)
        cv["ffn_u"] = convert(ffn_u_d, s_ffn_u, D)
        cv["ffn_d"] = convert(ffn_d_d, s_ffn_d, DFF)
    l1_conv_jobs = []
    if DO_L1:
        l1_conv_jobs.append(("sc_in", sc_in_d, s_sc_in, D))
        l1_conv_jobs.append(("sc_out", sc_out_d, s_sc_out, D))
        for e in range(NE):
            l1_conv_jobs.append((f"moe_g{e}", moe_g_d[e], s_moe_g[e], D))
            l1_conv_jobs.append((f"moe_u{e}", moe_u_d[e], s_moe_u[e], D))
            l1_conv_jobs.append((f"moe_d{e}", moe_d_d[e], s_moe_d[e], DFF))

    def run_l1_conv(njobs):
        for _ in range(njobs):
            if l1_conv_jobs:
                name, src, dst, rows = l1_conv_jobs.pop(0)
                cv[name] = convert(src, dst, rows)

    cs32 = ntmp[:, 0:8 * NSEQ].rearrange("p (k s) -> p k s", k=8)
    csb = nbf[:, 0:8 * NSEQ].rearrange("p (k s) -> p k s", k=8)
    P.dma("sp", cs32, cT_d.rearrange("(k p) s -> p k s", p=128), writes=[Bntmp], allow_slow_non_contiguous=True)
    act_(csb, cs32, AF.Silu, [Bntmp], [Bnbf])
    modrow = big[0:NSEQ, 0:12288].bitcast(F32)
    gt2 = big[0:NSEQ, 12288:12288 + 4096].bitcast(F32)
    bias2 = big[0:NSEQ, 16384:16384 + 12288].bitcast(F32)
    Bmodrow, Bg2, Bb2 = Buf("modrow"), Buf("g2"), Buf("b2")
    Bmodv = Buf("modv")
    layers = ([0] if DO_L0 else []) + ([1] if DO_L1 else [])
    for l in layers:
        P.dma("sp", bias2, ada_b_d[l:l + 1, :].partition_broadcast(NSEQ), writes=[Bb2])
        P.dma("sp", gt2[:, 0:D], norm_g_d[2 * l:2 * l + 1, :].partition_broadcast(NSEQ), writes=[Bg2])
        P.dma("sp", gt2[:, D:2 * D], norm_g_d[2 * l + 1:2 * l + 2, :].partition_broadcast(NSEQ), writes=[Bg2])
        for nchunk in range(12):
            slot, bslot = wslot()
            sv3 = slot.rearrange("p (k n) -> p k n", k=8)
            P.dma("pool", sv3, ada_w_d[l].rearrange("(k p) n -> p k n", p=128)[:, :, nchunk * 512:(nchunk + 1) * 512],
                  writes=[bslot])
            pb, bpb = ring_bank()
            for kc in range(8):
                mm(pb[0:NSEQ, :], csb[:, kc, :], sv3[:, kc, :], kc == 0, kc == 7, [Bnbf, bslot], [bpb])
            tt("dve", modrow[:, nchunk * 512:(nchunk + 1) * 512], pb[0:NSEQ, :],
               bias2[:, nchunk * 512:(nchunk + 1) * 512], ALU.add, [bpb, Bb2], [Bmodrow])
        stt("dve", modrow[:, D:2 * D], modrow[:, D:2 * D], 1.0, gt2[:, 0:D], ALU.add, ALU.mult, [Bmodrow, Bg2], [Bmodrow])
        stt("dve", modrow[:, 4 * D:5 * D], modrow[:, 4 * D:5 * D], 1.0, gt2[:, D:2 * D], ALU.add, ALU.mult,
            [Bmodrow, Bg2], [Bmodrow])
        for k, src in enumerate((1, 0, 2, 4, 3, 5)):
            P.dma("sp", modv[l, k, :, :], modrow[:, src * D:(src + 1) * D], reads=[Bmodrow], writes=[Bmodv])
    P.handoff([Bmodrow, Bg2, Bb2], l0_persist + conv_bufs + l1_bufs)

    def load_mod(l, k, seq, second=False):
        if second:
            P.dma("sp", modt2[:], modv[l, k, seq:seq + 1, :].partition_broadcast(128), reads=[Bmodv], writes=[Bmod2])
        else:
            P.dma("sp", modt[:], modv[l, k, seq:seq + 1, :].partition_broadcast(128), reads=[Bmodv], writes=[Bmod])

    By = {}

    def load_x(src, row0):
        for s in range(4):
            r = row0 + s * 128
            rd = [By[r]] if (src is y_d and r in By) else []
            P.dma("pool", xt[:, s, :], src[r:r + 128, :], reads=rd, writes=[Bxt[s]])

    def store_x(row0):
        toks = []
        for s in range(4):
            r = row0 + s * 128
            By.setdefault(r, Buf(f"y{r}"))
            toks.append(P.dma("pool", y_d[r:r + 128, :], xt[:, s, :], reads=[Bxt[s]], writes=[By[r]]))
        return toks

    def norm_to_hT(l, which, seq, router=False):
        memset("dve", sv[:, SS:SS + 4], 0.0, [Bsv])
        if router:
            memset("dve", sv[:, LG:LG + 32], 0.0, [Bsv])
        for s in range(4):
            act_(nbf[:], xt[:, s, :], AF.Square, [Bxt[s]], [Bnbf, Bsv], accum_out=sv[:, SS + s:SS + s + 1])
        act_(sv[:, RS:RS + 4], sv[:, SS:SS + 4], AF.Sqrt, [Bsv, Bconst], [Bsv], bias=epsc[:, 0:1], scale=1.0 / D)
        P.op("dve", lambda e: e.reciprocal(out=sv[:, RS:RS + 4], in_=sv[:, RS:RS + 4]), [Bsv], [Bsv])
        load_mod(l, 3 * which + 0, seq)
        load_mod(l, 3 * which + 1, seq, second=True)
        for s in range(4):
            stt("dve", ntmp[:], xt[:, s, :], sv[:, RS + s:RS + s + 1], modt[:], ALU.mult, ALU.mult,
                [Bxt[s], Bsv, Bmod], [Bntmp])
            dump("modA", modt[:], [Bmod])
            dump("sv0", sv[:, 0:16], [Bsv])
            dump("ntmp0", ntmp[:], [Bntmp])
            if router:
                tt("dve", ntmp[:], ntmp[:], modt2[:], ALU.add, [Bntmp, Bmod2], [Bntmp])
                cp("act", nbf[:], ntmp[:], [Bntmp], [Bnbf])
                tt("dve", lobf, ntmp[:], nbf[:], ALU.subtract, [Bntmp, Bnbf], [Blo])
            else:
                tt("dve", nbf[:], ntmp[:], modt2[:], ALU.add, [Bntmp, Bmod2], [Bnbf])
            pb, bpb = ring_bank()
            pbb = pb[:].bitcast(BF16)
            for c in range(8):
                tr(pbb[:, c * 128:(c + 1) * 128], nbf[:, c * 128:(c + 1) * 128], [Bnbf, Bconst], [bpb])
            cp("act", hT[:, :, s * 128:(s + 1) * 128], pbb[:, 0:1024].rearrange("p (c t) -> p c t", c=8), [bpb], [BhT])
            if router:
                pl, bpl = ring_bank()
                plb = pl[:].bitcast(BF16)
                for c in range(8):
                    tr(plb[:, c * 128:(c + 1) * 128], lobf[:, c * 128:(c + 1) * 128], [Blo, Bconst], [bpl])
                cp("dve", hTlo, plb[:, 0:1024].rearrange("p (c t) -> p c t", c=8), [bpl], [BhTlo])
                pr, bpr = ring_bank()
                n_mm = 0
                for kc in range(8):
                    for (lh, blh, rw) in ((hT[:, kc, s * 128:(s + 1) * 128], BhT, wr_hi[:, kc, :]),
                                          (hTlo[:, kc, :], BhTlo, wr_hi[:, kc, :]),
                                          (hT[:, kc, s * 128:(s + 1) * 128], BhT, wr_lo[:, kc, :])):
                        mm(pr[:, 0:8], lh, rw, n_mm == 0, n_mm == 23, [blh, BwrB], [bpr])
                        n_mm += 1
                cp("dve", sv[:, LG + s * 8:LG + s * 8 + 8], pr[:, 0:8], [bpr], [Bsv])
            if s == 3:
                dump("hT", hT[:].rearrange("p c t -> p (c t)"), [BhT])

    def load_cols(Wbf, cvb, c0, w):
        slot, bslot = wslot()
        v = slot.rearrange("p (k n) -> p k n", k=8)
        P.dma("sp", v[:, :, 0:w], Wbf.rearrange("(k p) n -> p k n", p=128)[:, :, c0:c0 + w], reads=cvb, writes=[bslot])
        return v, bslot

    def out_proj_residual(Wbf, cvb, srcT, BsrcT, l, seq, which_gate):
        halves = []
        for hlf in range(2):
            halves.append(load_cols(Wbf, cvb, hlf * 512, 512))
        load_mod(l, which_gate, seq)
        for s in range(4):
            (pa, ba), (pb_, bb_) = acc_pair()
            for hlf, (pbk, bbk) in enumerate(((pa, ba), (pb_, bb_))):
                v, bslot = halves[hlf]
                for kc in range(8):
                    mm(pbk[:], srcT[:, kc, s * 128:(s + 1) * 128], v[:, kc, :], kc == 0, kc == 7, [BsrcT, bslot], [bbk])
                tt("dve", ntmp[:, hlf * 512:(hlf + 1) * 512], pbk[:], modt[:, hlf * 512:(hlf + 1) * 512], ALU.mult,
                   [bbk, Bmod], [Bntmp])
            tt("dve", xt[:, s, :], xt[:, s, :], ntmp[:], ALU.add, [Bxt[s], Bntmp], [Bxt[s]])

    def ffn_gate_up(Wg, Wu, cvg, cvu, actv, Bact):
        for q in range(6):
            c0 = q * 512
            w = min(512, DFF - c0)
            vg, bg = load_cols(Wg, cvg, c0, w)
            vu, bu = load_cols(Wu, cvu, c0, w)
            for jj in range(w // 128):
                j = q * 4 + jj
                pg, bpg = ring_bank()
                pu, bpu = ring_bank()
                for kc in range(8):
                    mm(pg[:], vg[:, kc, jj * 128:(jj + 1) * 128], hT[:, kc, :], kc == 0, kc == 7, [bg, BhT], [bpg])
                for kc in range(8):
                    mm(pu[:], vu[:, kc, jj * 128:(jj + 1) * 128], hT[:, kc, :], kc == 0, kc == 7, [bu, BhT], [bpu])
                si = j % 2
                act_(sil[:, si, :], pg[:], AF.Silu, [bpg], [Bsil[si]])
                tt("dve", actv[:, j, :], pu[:], sil[:, si, :], ALU.mult, [bpu, Bsil[si]], [Bact[j]])

    def layer0_tile(seq, t):
        row0 = seq * S + t * T
        tok0 = t * T
        P.handoff(Bact0, conv_bufs)
        load_x(x_d, row0)
        norm_to_hT(0, 0, seq)
        if t == 0:
            memset("pool", yhist[:], 0.0, [Byh])
            if seq == 0:
                memset("pool", vc[:], 1.0, [Bvc])
        va, ba = load_cols(s_w_in0, cv["w_in0"], 0, 512)
        vg, bg = load_cols(s_w_in0, cv["w_in0"], 512, 512)
        cp("pool", yv[:, :, 0:30], yhist[:, :, 0:30], [Byh], [Byv])
        for c in range(4):
            pa, bpa = ring_bank()
            pg, bpg = ring_bank()
            for kc in range(8):
                mm(pa[:], va[:, kc, c * 128:(c + 1) * 128], hT[:, kc, :], kc == 0, kc == 7, [ba, BhT], [bpa])
            for kc in range(8):
                mm(pg[:], vg[:, kc, c * 128:(c + 1) * 128], hT[:, kc, :], kc == 0, kc == 7, [bg, BhT], [bpg])
            act_(sgm[:], pg[:], AF.Sigmoid, [bpg], [Bsgm])
            tt("dve", yv[:, c, 30:542], pa[:], sgm[:], ALU.mult, [bpa, Bsgm], [Byv])
        cp("pool", yhist[:, :, 0:30], yv[:, :, 512:542], [Byv], [Byh])
        dump("yv", yv[:].rearrange("p c t -> p (c t)"), [Byv])
        for c in range(4):
            db = c % 2
            for j in range(31):
                if j % 2 == 0:
                    ts("dve", dg[:, db, j, :], ident[:], cw[:, c, j:j + 1], None, ALU.mult, None, [Bconst, Bpar], [Bdg[db][j]])
                else:
                    act_(dg[:, db, j, :], ident[:], AF.Identity, [Bconst, Bpar], [Bdg[db][j]], scale=cw[:, c, j:j + 1])
            pc, bpc = ring_bank()
            for j in range(31):
                mm(pc[:], dg[:, db, j, :], yv[:, c, j:j + 512], j == 0, j == 30, [Bdg[db][j], Byv], [bpc])
            act_(a_bf[:, c, :], pc[:], AF.Identity, [bpc, Bpar], [Babf], bias=cb[:, c:c + 1], scale=1.0)
            act_(a2_bf[:, c, :], pc[:], AF.Square, [bpc, Bpar], [Ba2], bias=cb[:, c:c + 1], scale=1.0)
        for which, (c0, dstT, Bdst, gain, col0) in enumerate(((1024, qT, BqT, qg, 0), (1536, kT, BkT, kg, tok0))):
            vq, bq = load_cols(s_w_in0, cv["w_in0"], c0, 512)
            for c in range(4):
                pq, bpq = ring_bank()
                for kc in range(8):
                    mm(pq[:], vq[:, kc, c * 128:(c + 1) * 128], hT[:, kc, :], kc == 0, kc == 7, [bq, BhT], [bpq])
                si = c % 2
                act_(sil[:, si, :], pq[:], AF.Square, [bpq], [Bsil[si]])
                pm, bpm = ring_bank()
                mm(pm[:], bd[:], sil[:, si, :], True, True, [Bconst, Bsil[si]], [bpm])
                act_(ntmp[:, 0:512], pm[:], AF.Sqrt, [bpm, Bconst], [Bntmp], bias=epsc[:, 0:1], scale=1.0)
                P.op("dve", lambda e: e.reciprocal(out=ntmp[:, 0:512], in_=ntmp[:, 0:512]), [Bntmp], [Bntmp])
                stt("dve", dstT[:, c, col0:col0 + 512], pq[:], gain[:, 0:1], ntmp[:, 0:512], ALU.mult, ALU.mult,
                    [bpq, Bpar, Bntmp], [Bdst])
        vv, bv = load_cols(s_w_in0, cv["w_in0"], 2048, 512)
        for s in range(4):
            pv, bpv = ring_bank()
            for kc in range(8):
                mm(pv[:], hT[:, kc, s * 128:(s + 1) * 128], vv[:, kc, :], kc == 0, kc == 7, [BhT, bv], [bpv])
            dst = vc[:, t * 4 + s, :].rearrange("p (h d) -> p h d", h=8)[:, :, 0:64]
            cp("act", dst, pv[:].rearrange("p (h d) -> p h d", h=8), [bpv], [Bvc])
        vqi, bqi = load_cols(s_w_in0, cv["w_in0"], 2560, 512)
        for c in range(4):
            pq, bpq = ring_bank()
            for kc in range(8):
                mm(pq[:], vqi[:, kc, c * 128:(c + 1) * 128], hT[:, kc, :], kc == 0, kc == 7, [bqi, BhT], [bpq])
            cp("act", qiT[:, c, :], pq[:], [bpq], [BqiT])
        slot, bslot = wslot()
        vk = slot.rearrange("p (k n) -> p k n", k=8)
        srcw = s_w_in0.rearrange("(k p) n -> p k n", p=128)
        P.dma("sp", vk[:, :, 0:64], srcw[:, :, 3072:3136], reads=cv["w_in0"], writes=[bslot])
        P.dma("sp", vk[:, :, 64:128], srcw[:, :, 3072:3136], reads=cv["w_in0"], writes=[bslot])
        P.dma("sp", vk[:, :, 128:136], srcw[:, :, 3136:3144], reads=cv["w_in0"], writes=[bslot])
        pk, bpk = ring_bank()
        for kc in range(8):
            mm(pk[:], vk[:, kc, 0:128], hT[:, kc, :], kc == 0, kc == 7, [bslot, BhT], [bpk])
        cp("act", kiT[:, tok0:tok0 + 512], pk[:], [bpk], [BkiT])
        pw, bpw = ring_bank()
        for s in range(4):
            for kc in range(8):
                mm(pw[:, s * 8:(s + 1) * 8], hT[:, kc, s * 128:(s + 1) * 128], vk[:, kc, 128:136], kc == 0, kc == 7,
                   [bslot, BhT], [bpw])
        ts("dve", wi_t[:, :, :], pw[:, 0:32].rearrange("p (s e) -> p s e", s=4), float(512 ** -0.5), None, ALU.mult, None,
           [bpw], [Bwi])
        pm, bpm = ring_bank()
        pq2, bpq2 = ring_bank()
        for c in range(4):
            mm(pm[:], onesD[:], a_bf[:, c, :], c == 0, c == 3, [Bconst, Babf], [bpm])
        for c in range(4):
            mm(pq2[:], onesD[:], a2_bf[:, c, :], c == 0, c == 3, [Bconst, Ba2], [bpq2])
        cp("act", mean_sb[:], pm[:], [bpm], [Bmean])
        tt("dve", rstd_sb[:], mean_sb[:], mean_sb[:], ALU.mult, [Bmean], [Brstd])
        tt("dve", rstd_sb[:], pq2[:], rstd_sb[:], ALU.subtract, [bpq2, Brstd], [Brstd])
        ts("dve", rstd_sb[:], rstd_sb[:], 0.0, None, ALU.max, None, [Brstd], [Brstd])
        act_(rstd_sb[:], rstd_sb[:], AF.Sqrt, [Brstd, Bconst], [Brstd], bias=epsc[:, 0:1], scale=1.0)
        P.op("dve", lambda e: e.reciprocal(out=rstd_sb[:], in_=rstd_sb[:]), [Brstd], [Brstd])
        for c in range(4):
            tt("dve", sgm[:], a_bf[:, c, :], mean_sb[:], ALU.subtract, [Babf, Bmean], [Bsgm])
            tt("dve", sgm[:], sgm[:], rstd_sb[:], ALU.mult, [Bsgm, Brstd], [Bsgm])
            act_(mixT[:, c, :], sgm[:], AF.Silu, [Bsgm, Bpar], [BmixT], bias=cnb[:, c:c + 1], scale=cng[:, c:c + 1])
        dump("abf", a_bf[:].rearrange("p c t -> p (c t)"), [Babf])
        dump("rstd", rstd_sb[:], [Brstd])
        dump("mean", mean_sb[:], [Bmean])
        dump("mixA", mixT[:, 0:4, :], [BmixT])
        dump("qT", qT[:].rearrange("p c t -> p (c t)"), [BqT])
        dump("kT", kT[:, :, 0:512], [BkT])
        dump("kiT", kiT[:, 0:512], [BkiT])
        dump("qiT", qiT[:].rearrange("p c t -> p (c t)"), [BqiT])
        dump("wi", wi_t[:].rearrange("p c t -> p (c t)"), [Bwi])
        dump("vc", vc[:, 0:4, :], [Bvc])
        P.handoff(conv_bufs, attn_bufs)
        Brl = [Brl0, Brl1]
        BPT = [BPT0, BPT1]

        def phase_a(s):
            p = s % 2
            qi_ = t * 4 + s
            nb = qi_ + 1
            L = nb * 128
            tq = slice(s * 128, (s + 1) * 128)
            negm = negm2[:, p, :]
            P.handoff([Bnegm2[p]], [Bjd[p], Bja[p]])
            ri = 0
            for k0 in range(0, L, 512):
                w = min(512, L - k0)
                for h in range(8):
                    c, hp = h // 2, h % 2
                    pd, bpd = ring_bank("a")
                    mm(pd[:, 0:w], qiT[hp * 64:(hp + 1) * 64, c, tq], kiT[hp * 64:(hp + 1) * 64, k0:k0 + w], True, True,
                       [BqiT, BkiT], [bpd])
                    r = ri % 2
                    ri += 1
                    act_(rl[:, r, 0:w], pd[:, 0:w], AF.Relu, [bpd], [Brl[r]])
                    if h == 0:
                        ts("dve", score[:, k0:k0 + w], rl[:, r, 0:w], wi_t[:, s, 0:1], None, ALU.mult, None,
                           [Brl[r], Bwi], [Bscore])
                    else:
                        stt("dve", score[:, k0:k0 + w], rl[:, r, 0:w], wi_t[:, s, h:h + 1], score[:, k0:k0 + w],
                            ALU.mult, ALU.add, [Brl[r], Bwi, Bscore], [Bscore])
                    if h % 4 == 3:
                        yield
            mxs = sv[:, 112 + p * 4:112 + p * 4 + 1]
            mns = sv[:, 113 + p * 4:113 + p * 4 + 1]
            rgs = sv[:, 114 + p * 4:114 + p * 4 + 1]
            if nb > 2:
                P.op("dve", lambda e: e.tensor_reduce(out=mxs, in_=score[:, 0:L], axis=AX.X, op=ALU.max), [Bscore], [Bmm[p]])
                P.op("dve", lambda e: e.tensor_reduce(out=mns, in_=score[:, 0:L], axis=AX.X, op=ALU.min), [Bscore], [Bmm[p]])
            tt("dve", score[:, L - 128:L], score[:, L - 128:L], cneg[:], ALU.add, [Bscore, Bconst], [Bscore])
            if nb <= 2:
                memset("dve", thr_t[:, p, NIT:NIT + 1], -1e29, [Bthr[p]])
            else:
                Lh = (nb // 2) * 128
                La = L - Lh
                tt("dve", rgs, mxs, mns, ALU.subtract, [Bmm[p]], [Bmm[p]])
                ts("dve", stp_t[:, p, :], pw_t[:, :], rgs, None, ALU.mult, None, [Bmm[p], Bconst], [Bst[p]])
                ts("dve", stp2_t[:, p, :], stp_t[:, p, :], 2.0, None, ALU.mult, None, [Bst[p]], [Bst[p]])
                stt("dve", thr_t[:, p, 0:1], rgs, 0.5, mns, ALU.mult, ALU.add, [Bmm[p]], [Bthr[p]])
                memset("dve", cnt_t[:, p, :], 0.0, [Bcd[p]])
                memset("dve", cact_t[:, p, :], 0.0, [Bca[p]])
                for n in range(NIT):
                    ts("dve", negm[:, 0:Lh], score[:, 0:Lh], thr_t[:, p, n:n + 1], 0.0, ALU.is_gt, ALU.add,
                       [Bscore, Bthr[p]], [Bjd[p], Bcd[p]], accum_out=cnt_t[:, p, n:n + 1])
                    act_(negm[:, Lh:L], score[:, Lh:L], AF.Sign, [Bscore, Bthr[p]], [Bja[p], Bca[p]],
                         bias=thr_t[:, p, n:n + 1], scale=-1.0, accum_out=cact_t[:, p, n:n + 1])
                    stt("dve", uu_t[:, p, n:n + 1], cnt_t[:, p, n:n + 1], 2.0, cact_t[:, p, n:n + 1], ALU.mult, ALU.subtract,
                        [Bcd[p], Bca[p]], [Bst[p]])
                    ts("dve", dd_t[:, p, n:n + 1], uu_t[:, p, n:n + 1], float(2 * TOPK - La), stp2_t[:, p, n:n + 1],
                       ALU.is_gt, ALU.mult, [Bst[p]], [Bst[p]])
                    stt("dve", thr_t[:, p, n + 1:n + 2], dd_t[:, p, n:n + 1], stp_t[:, p, n:n + 1], thr_t[:, p, n:n + 1],
                        ALU.subtract, ALU.add, [Bst[p], Bthr[p]], [Bthr[p]])
                    yield
            ts("dve", negm[:, 0:L], score[:, 0:L], thr_t[:, p, NIT:NIT + 1], -30000.0, ALU.is_le, ALU.mult,
               [Bscore, Bthr[p]], [Bnegm2[p], Bjd[p], Bja[p]])

        def phase_b(s):
            p = s % 2
            qi_ = t * 4 + s
            nb = qi_ + 1
            tq = slice(s * 128, (s + 1) * 128)
            negm = negm2[:, p, :]
            (oa, boa), (ob, bob) = acc_pair()
            memset("dve", qz[:], 0.0, [Bqz])
            cp("dve", qz[0:64, :, 0, :], qT[0:64, :, tq], [BqT], [Bqz])
            cp("dve", qz[64:128, :, 1, :], qT[64:128, :, tq], [BqT], [Bqz])
            units = [(c, g0) for c in range(4) for g0 in range(0, nb, 2)]

            def emit_lt(u):
                c, g0 = u
                ng = min(2, nb - g0)
                lt, blt = ring_bank("b")
                for j in range(ng):
                    kb = g0 + j
                    mm(lt[:, j * 256:(j + 1) * 256], kT[:, c, kb * 128:(kb + 1) * 128],
                       qz[:, c, :, :].rearrange("p a t -> p (a t)"), True, False, [BkT, Bqz], [blt])
                    mm(lt[:, j * 256:(j + 1) * 256], negm[:, kb * 128:(kb + 1) * 128], ident2[:, 0:256], False, True,
                       [Bnegm2[p], Bconst], [blt])
                return lt, blt

            cur = emit_lt(units[0])
            for i, (c, g0) in enumerate(units):
                lt, blt = cur
                if i + 1 < len(units):
                    cur = emit_lt(units[i + 1])
                ng = min(2, nb - g0)
                p_ = i % 2
                act_(PT[:, p_, 0:ng * 256], lt[:, 0:ng * 256], AF.Exp, [blt], [BPT[p_]])
                for j in range(ng):
                    kb = g0 + j
                    for hh, (ob_, bob_) in enumerate(((oa, boa), (ob, bob))):
                        h = 2 * c + hh
                        mm(ob_[:, c * 65:c * 65 + 65], PT[:, p_, j * 256 + hh * 128:j * 256 + (hh + 1) * 128],
                           vc[:, kb, h * 65:(h + 1) * 65], kb == 0, kb == nb - 1, [BPT[p_], Bvc], [bob_])
                yield
            yield "epilogue"
            for half, (o_, bo_) in enumerate(((oa, boa), (ob, bob))):
                ov = o_[:, 0:260].rearrange("p (h d) -> p h d", h=4)
                rd = sv[:, RD + half * 4:RD + half * 4 + 4]
                recip3(rd.unsqueeze(2), ov[:, :, 64:65], [bo_], [Bsv])
                tt("dve", att[:].rearrange("p (c hh d) -> p c hh d", c=4, hh=2)[:, :, half, :], ov[:, :, 0:64],
                   rd.unsqueeze(2).to_broadcast([128, 4, 64]), ALU.mult, [bo_, Bsv], [Batt])
            ptb, bptb = ring_bank()
            ptbb = ptb[:].bitcast(BF16)
            for c in range(4):
                tr(ptbb[:, c * 128:(c + 1) * 128], att[:, c * 128:(c + 1) * 128], [Batt, Bconst], [bptb])
            cp("act", mixT[:, 4:8, tq], ptbb[:, 0:512].rearrange("p (c t) -> p c t", c=4), [bptb], [BmixT])

        def drain(g):
            for _ in g:
                pass

        def interleave(ga, gb):
            a_alive, b_alive = True, True
            while a_alive or b_alive:
                if a_alive:
                    try:
                        next(ga)
                    except StopIteration:
                        a_alive = False
                if b_alive:
                    try:
                        r = next(gb)
                        if r == "epilogue":
                            b_alive = False
                    except StopIteration:
                        b_alive = False
            drain(ga)
            drain(gb)

        drain(phase_a(0))
        for s in range(4):
            gb = phase_b(s)
            if s + 1 < 4:
                interleave(phase_a(s + 1), gb)
            else:
                drain(gb)
        out_proj_residual(s_w_out0, cv["w_out0"], mixT, BmixT, 0, seq, 2)
        dump("mixT", mixT[:].rearrange("p c t -> p (c t)"), [BmixT])
        dump("x1", xt[:].rearrange("p c t -> p (c t)"), Bxt)
        P.handoff(attn_bufs, Bact0)
        norm_to_hT(0, 1, seq)
        ffn_gate_up(s_ffn_g, s_ffn_u, cv["ffn_g"], cv["ffn_u"], act0, Bact0)
        load_mod(0, 5, seq)
        wd_v = s_ffn_d.rearrange("(j p) n -> p j n", p=128)
        for q in range(6):
            nj = min(4, NJ - q * 4)
            slot, bslot = wslot()
            v = slot.rearrange("p (j n) -> p j n", j=4)
            P.dma("sp", v[:, 0:nj, :], wd_v[:, q * 4:q * 4 + nj, :], reads=cv["ffn_d"], writes=[bslot])
            for jj in range(nj):
                j = q * 4 + jj
                for s in range(4):
                    for hlf in range(2):
                        bk = 2 * s + hlf
                        mm(banks[bk][:], act0[:, j, s * 128:(s + 1) * 128], v[:, jj, hlf * 512:(hlf + 1) * 512], j == 0,
                           j == NJ - 1, [Bact0[j], bslot], [Bbank[bk]])
        for s in range(4):
            for hlf in range(2):
                bk = 2 * s + hlf
                tt("dve", ntmp[:, hlf * 512:(hlf + 1) * 512], banks[bk][:], modt[:, hlf * 512:(hlf + 1) * 512], ALU.mult,
                   [Bbank[bk], Bmod], [Bntmp])
            tt("dve", xt[:, s, :], xt[:, s, :], ntmp[:], ALU.add, [Bxt[s], Bntmp], [Bxt[s]])
        return store_x(row0)

    def layer1_tile(seq, t, src):
        row0 = seq * S + t * T
        load_x(src, row0)
        norm_to_hT(1, 0, seq)
        if t == 0:
            memset("pool", zh[:], 0.0, [Bzh])
        for c in range(8):
            if c % 4 == 0:
                vb, bb = load_cols(s_sc_in, cv["sc_in"], c * 128, 512)
                vc_, bc_ = load_cols(s_sc_in, cv["sc_in"], D + c * 128, 512)
                vv_, bv_ = load_cols(s_sc_in, cv["sc_in"], 2 * D + c * 128, 512)
            cc = c % 4
            pbg, bpbg = ring_bank()
            pcg, bpcg = ring_bank()
            pvv, bpvv = ring_bank()
            for (pp, bpp, vw, bw_) in ((pbg, bpbg, vb, bb), (pcg, bpcg, vc_, bc_), (pvv, bpvv, vv_, bv_)):
                for kc in range(8):
                    mm(pp[:], vw[:, kc, cc * 128:(cc + 1) * 128], hT[:, kc, :], kc == 0, kc == 7, [bw_, BhT], [bpp])
            cp("act", vsb[:], pvv[:], [bpvv], [Bvsb])
            cp("dve", zt[:, 0:2], zh[:, c, :], [Bzh], [Bzt])
            tt("dve", zt[:, 2:514], pcg[:], vsb[:], ALU.mult, [bpcg, Bvsb], [Bzt])
            cp("dve", zh[:, c, :], zt[:, 512:514], [Bzt], [Bzh])
            ts("dve", c3[:], zt[:, 2:514], sccw[:, c, 2:3], None, ALU.mult, None, [Bzt, Bpar], [Bc3])
            stt("dve", c3[:], zt[:, 1:513], sccw[:, c, 1:2], c3[:], ALU.mult, ALU.add, [Bzt, Bpar, Bc3], [Bc3])
            stt("dve", c3[:], zt[:, 0:512], sccw[:, c, 0:1], c3[:], ALU.mult, ALU.add, [Bzt, Bpar, Bc3], [Bc3])
            tt("dve", mT[:, c, :], pbg[:], c3[:], ALU.mult, [bpbg, Bc3], [BmT])
        out_proj_residual(s_sc_out, cv["sc_out"], mT, BmT, 1, seq, 2)
        norm_to_hT(1, 1, seq, router=True)
        for s in range(4):
            lg = sv[:, LG + s * 8:LG + s * 8 + 8]
            gt_ = sv[:, GT + s * 8:GT + s * 8 + 8]
            top = sv[:, W12 + 0:W12 + 8]
            P.op("dve", lambda e, lg=lg, top=top: e.max(out=top, in_=lg), [Bsv], [Bsv])
            tt("dve", sv[:, W12 + 8:W12 + 9], sv[:, W12 + 0:W12 + 1], sv[:, W12 + 1:W12 + 2], ALU.subtract, [Bsv], [Bsv])
            act_(sv[:, W12 + 9:W12 + 10], sv[:, W12 + 8:W12 + 9], AF.Sigmoid, [Bsv], [Bsv])
            ts("dve", sv[:, W12 + 10:W12 + 11], sv[:, W12 + 9:W12 + 10], -1.0, 1.0, ALU.mult, ALU.add, [Bsv], [Bsv])
            ts("dve", gt_, lg, sv[:, W12 + 0:W12 + 1], sv[:, W12 + 9:W12 + 10], ALU.is_equal, ALU.mult, [Bsv], [Bsv])
            ts("dve", sv[:, W12 + 11:W12 + 19], lg, sv[:, W12 + 1:W12 + 2], sv[:, W12 + 10:W12 + 11], ALU.is_equal, ALU.mult,
               [Bsv], [Bsv])
            tt("dve", gt_, gt_, sv[:, W12 + 11:W12 + 19], ALU.add, [Bsv], [Bsv])
        load_mod(1, 5, seq)
        for e_ in range(NE):
            wd_v = s_moe_d[e_].rearrange("(j p) n -> p j n", p=128)
            cvd = cv[f"moe_d{e_}"]
            for q in range(0, NJ, 2):
                P.dma("pool", wdres[:, q:q + 2, :], wd_v[:, q:q + 2, :], reads=cvd, writes=[Bwd[q // 2]])
            ffn_gate_up(s_moe_g[e_], s_moe_u[e_], cv[f"moe_g{e_}"], cv[f"moe_u{e_}"], act1, Bact1)
            for s in range(4):
                (pa, ba), (pb_, bb_) = acc_pair()
                for j in range(NJ):
                    for hlf, (pbk, bbk) in enumerate(((pa, ba), (pb_, bb_))):
                        mm(pbk[:], act1[:, j, s * 128:(s + 1) * 128], wdres[:, j, hlf * 512:(hlf + 1) * 512], j == 0,
                           j == NJ - 1, [Bact1[j], Bwd[j // 2]], [bbk])
                for hlf, (pbk, bbk) in enumerate(((pa, ba), (pb_, bb_))):
                    stt("dve", ntmp[:, hlf * 512:(hlf + 1) * 512], pbk[:], sv[:, GT + s * 8 + e_:GT + s * 8 + e_ + 1],
                        modt[:, hlf * 512:(hlf + 1) * 512], ALU.mult, ALU.mult, [bbk, Bsv, Bmod], [Bntmp])
                tt("dve", xt[:, s, :], xt[:, s, :], ntmp[:], ALU.add, [Bxt[s], Bntmp], [Bxt[s]])
        return store_x(row0)

    final = []
    if DO_L0:
        per_tile_jobs = (len(l1_conv_jobs) + NTILES - 1) // max(1, NTILES)
        for seq in range(NSEQ):
            for t in range(NTILES):
                toks = layer0_tile(seq, t)
                if seq == 0:
                    run_l1_conv(per_tile_jobs)
                final = toks if not DO_L1 else final
                if not DO_L1:
                    final_all = final
    run_l1_conv(len(l1_conv_jobs))
    Bystore = Buf("ystore")
    if DO_L1:
        P.handoff(l0_persist + conv_bufs + attn_bufs + Bact0, l1_bufs)
        P.dma("sp", wr32, router_d.rearrange("(k p) e -> p k e", p=128), writes=[BwrB])
        cp("dve", wr_hi, wr32, [BwrB], [BwrB])
        tt("dve", wrtmp, wr32, wr_hi, ALU.subtract, [BwrB], [BwrB])
        cp("dve", wr_lo, wrtmp, [BwrB], [BwrB])
        src = y_d if DO_L0 else x_d
        for seq in range(NSEQ):
            for t in range(NTILES):
                final = layer1_tile(seq, t, src)
    for lane in range(NLANES):
        v = P.lane_val.get(("pool", lane), 0)
        if v:
            P.wait_tok("sp", ("dma", "pool", lane, v))
    P.build()
    es.close()
    nc._dbg_list = dbg_list
    return nc


def _prep_shared(inp):
    f = lambda a: np.ascontiguousarray(a, dtype=np.float32)
    d = {}
    d["ada_w"] = f(inp["ada_w"])
    d["ada_b"] = f(inp["ada_b"])
    d["norm_g"] = f(inp["norm_g"].reshape(4, D))
    d["w_in0"] = f(inp["ab_w_in"][0])
    d["convwT"] = f(inp["ab_conv_w"][0].T)
    d["convb"] = f(inp["ab_conv_b"][0].reshape(4, 128).T)
    d["cng"] = f(inp["ab_cnorm_g"][0].reshape(4, 128).T)
    d["cnb"] = f(inp["ab_cnorm_b"][0].reshape(4, 128).T)
    d["qg2"] = f(np.concatenate([inp["ab_q_g"][0], inp["ab_q_g"][0]]).reshape(128, 1))
    d["kg2"] = f(np.concatenate([inp["ab_k_g"][0], inp["ab_k_g"][0]]).reshape(128, 1))
    d["w_out0"] = f(inp["ab_w_out"][0])
    d["ffn_g"] = f(inp["ffn_w_gate"][0])
    d["ffn_u"] = f(inp["ffn_w_up"][0])
    d["ffn_d"] = f(inp["ffn_w_down"][0])
    d["sc_in"] = f(inp["sc_w_in"][0])
    d["sc_cwT"] = f(inp["sc_conv_w"][0].T)
    d["sc_out"] = f(inp["sc_w_out"][0])
    d["router"] = f(inp["moe_router"][0])
    d["moe_g"] = f(inp["moe_w_gate"][0])
    d["moe_u"] = f(inp["moe_w_up"][0])
    d["moe_d"] = f(inp["moe_w_down"][0])
    return d


def run(inputs, n_cores=8, nseq=2, **bk):
    shared = _prep_shared(inputs)
    x = np.asarray(inputs["x"], dtype=np.float32)
    c = np.asarray(inputs["c"], dtype=np.float32)
    nc = build_program(NSEQ=nseq, **bk)
    in_maps = []
    for i in range(n_cores):
        m = dict(shared)
        m["x"] = np.ascontiguousarray(x[i * nseq:(i + 1) * nseq].reshape(nseq * S, D))
        m["cT"] = np.ascontiguousarray(c[i * nseq:(i + 1) * nseq].T)
        in_maps.append(m)
    res = run_bass_kernel_spmd(nc, in_maps, core_ids=list(range(n_cores)))
    outs = [np.asarray(r["y"]).reshape(nseq, S, D) for r in res.results]
    return np.concatenate(outs, axis=0).astype(np.float32)


def kernel(**inputs):
    return run(inputs, n_cores=8, nseq=2)
```

```python
import numpy as np
from contextlib import ExitStack
import concourse.bass as bass
import concourse.mybir as mybir
from concourse.bass_utils import run_bass_kernel_spmd

F32 = mybir.dt.float32
BF16 = mybir.dt.bfloat16
ALU = mybir.AluOpType
AF = mybir.ActivationFunctionType
AX = mybir.AxisListType

D = 1024
S = 4096
T = 512
NT = S // T
DFF = 2816
NJ = DFF // 128
INW = 3144
NE = 8
EPS = 1e-6
NIT = 16
TOPK = 256

CH = 4096
NLANES = 8


class Buf:
    __slots__ = ("name", "w", "r")

    def __init__(self, name=""):
        self.name = name
        self.w = None
        self.r = {}


class Prog:
    ENG = ("pe", "act", "dve", "pool", "sp")

    def __init__(self, nc):
        self.nc = nc
        self.q = {e: [] for e in self.ENG}
        self.n = {e: 0 for e in self.ENG}
        self.seen = {e: {} for e in self.ENG}
        self.sems = {}
        self.lane_rr = {e: 0 for e in self.ENG}
        self.lane_val = {}
        self.used_lanes = set()

    @staticmethod
    def _kv(t):
        if t[0] == "eng":
            return ("eng", t[1]), t[2]
        return ("dma", t[1], t[2]), t[3]

    def _collect(self, eng, reads, writes):
        out = {}

        def add(kind, t):
            key, val = self._kv(t)
            if t[0] == "eng" and t[1] == eng:
                if eng == "pe" or kind != "raw":
                    return
            if self.seen[eng].get(key, -1) >= val:
                return
            if out.get(key, -1) < val:
                out[key] = val

        for b in reads:
            if b.w is not None:
                add("raw", b.w)
        for b in writes:
            if b.w is not None:
                add("waw", b.w)
            for t in b.r.values():
                add("war", t)
        return out

    def _emit_waits(self, eng, waits):
        for key, val in waits.items():
            self.seen[eng][key] = val
            if key[0] == "eng":
                seg, off = divmod(val, CH)
                self.q[eng].append(("wait", ("eng", key[1], seg), off + 1))
            else:
                self.q[eng].append(("wait", key, val))

    def _update(self, tok, reads, writes):
        key, _ = self._kv(tok)
        for b in reads:
            b.r[key] = tok
        for b in writes:
            b.w = tok
            b.r = {}

    def op(self, eng, fn, reads=(), writes=()):
        self._emit_waits(eng, self._collect(eng, reads, writes))
        n = self.n[eng]
        self.n[eng] = n + 1
        self.q[eng].append(("op", fn, ("eng", eng, n // CH)))
        tok = ("eng", eng, n)
        self._update(tok, reads, writes)
        return tok

    def dma(self, eng, out, in_, reads=(), writes=(), **kw):
        lane = self.lane_rr[eng]
        self.lane_rr[eng] = (lane + 1) % NLANES
        waits = self._collect(eng, reads, writes)
        prev = self.lane_val.get((eng, lane), 0)
        key = ("dma", eng, lane)
        if prev > 0 and self.seen[eng].get(key, -1) < prev:
            waits[key] = max(waits.get(key, -1), prev)
        self._emit_waits(eng, waits)
        v = prev + 16
        self.lane_val[(eng, lane)] = v
        self.used_lanes.add((eng, lane))
        self.q[eng].append(("dma", out, in_, kw, key))
        tok = ("dma", eng, lane, v)
        self._update(tok, reads, writes)
        return tok

    def wait_tok(self, eng, tok):
        key, val = self._kv(tok)
        if self.seen[eng].get(key, -1) >= val:
            return
        self._emit_waits(eng, {key: val})

    def handoff(self, old, new):
        toks = {}
        for b in old:
            cand = list(b.r.values())
            if b.w is not None:
                cand.append(b.w)
            for t in cand:
                key, val = self._kv(t)
                if key not in toks or self._kv(toks[key])[1] < val:
                    toks[key] = t
        for b in new:
            for key, t in toks.items():
                if key not in b.r or self._kv(b.r[key])[1] < self._kv(t)[1]:
                    b.r[key] = t

    def build(self):
        nc = self.nc
        with ExitStack() as es:
            for e in self.ENG:
                nseg = (self.n[e] + CH - 1) // CH
                for s in range(nseg):
                    self.sems[("eng", e, s)] = es.enter_context(nc.semaphore(f"c_{e}_{s}"))
            for (e, lane) in sorted(self.used_lanes):
                self.sems[("dma", e, lane)] = es.enter_context(nc.semaphore(f"d_{e}_{lane}"))
            block = es.enter_context(nc.Block())
            sems = self.sems

            def run(engobj, items):
                for it in items:
                    if it[0] == "wait":
                        engobj.wait_ge(sems[it[1]], it[2])
                    elif it[0] == "op":
                        it[1](engobj).then_inc(sems[it[2]], 1)
                    else:
                        _, out, in_, kw, key = it
                        engobj.dma_start(out=out, in_=in_, **kw).then_inc(sems[key], 16)

            @block.tensor
            def _(e):
                run(e, self.q["pe"])

            @block.scalar
            def _(e):
                run(e, self.q["act"])

            @block.vector
            def _(e):
                run(e, self.q["dve"])

            @block.gpsimd
            def _(e):
                run(e, self.q["pool"])

            @block.sync
            def _(e):
                run(e, self.q["sp"])


def build_program(NSEQ=2, DO_L0=True, DO_L1=True, NTILES=NT, DEBUG=False):
    nc = bass.Bass("TRN2", target_bir_lowering=False)
    P = Prog(nc)
    NTOK = NSEQ * S

    def din(name, shape):
        return nc.dram_tensor(name, shape, F32, kind="ExternalInput").ap()

    def dscr(name, shape, dt=BF16):
        return nc.dram_tensor(name, shape, dt, kind="Internal").ap()

    x_d = din("x", [NTOK, D])
    cT_d = din("cT", [D, NSEQ])
    ada_w_d = din("ada_w", [2, D, 6 * D])
    ada_b_d = din("ada_b", [2, 6 * D])
    norm_g_d = din("norm_g", [4, D])
    w_in0_d = din("w_in0", [D, INW])
    convw_d = din("convwT", [512, 31])
    convb_d = din("convb", [128, 4])
    cng_d = din("cng", [128, 4])
    cnb_d = din("cnb", [128, 4])
    qg_d = din("qg2", [128, 1])
    kg_d = din("kg2", [128, 1])
    w_out0_d = din("w_out0", [D, D])
    ffn_g_d = din("ffn_g", [D, DFF])
    ffn_u_d = din("ffn_u", [D, DFF])
    ffn_d_d = din("ffn_d", [DFF, D])
    sc_in_d = din("sc_in", [D, 3 * D])
    sc_cw_d = din("sc_cwT", [D, 3])
    sc_out_d = din("sc_out", [D, D])
    router_d = din("router", [D, NE])
    if DO_L1:
        moe_g_d = din("moe_g", [NE, D, DFF])
        moe_u_d = din("moe_u", [NE, D, DFF])
        moe_d_d = din("moe_d", [NE, DFF, D])
    y_d = nc.dram_tensor("y", [NTOK, D], F32, kind="ExternalOutput").ap()

    s_w_in0 = dscr("s_w_in0", [D, INW])
    s_w_out0 = dscr("s_w_out0", [D, D])
    s_ffn_g = dscr("s_ffn_g", [D, DFF])
    s_ffn_u = dscr("s_ffn_u", [D, DFF])
    s_ffn_d = dscr("s_ffn_d", [DFF, D])
    s_sc_in = dscr("s_sc_in", [D, 3 * D])
    s_sc_out = dscr("s_sc_out", [D, D])
    s_moe_g = dscr("s_moe_g", [NE, D, DFF])
    s_moe_u = dscr("s_moe_u", [NE, D, DFF])
    s_moe_d = dscr("s_moe_d", [NE, DFF, D])
    modv = dscr("modv", [2, 6, NSEQ, D], F32)

    es = ExitStack()

    def sb(name, shape, dt):
        return es.enter_context(nc.sbuf_tensor(name, shape, dt))

    xt = sb("xt", [128, 4, D], F32)
    Bxt = [Buf(f"xt{i}") for i in range(4)]
    hT = sb("hT", [128, 8, T], BF16)
    BhT = Buf("hT")
    wring = sb("wring", [128, 4, 4096], BF16)
    Bw = [Buf(f"w{i}") for i in range(4)]
    modt = sb("modt", [128, D], F32)
    Bmod = Buf("mod")
    modt2 = sb("modt2", [128, D], F32)
    Bmod2 = Buf("mod2")
    ident = sb("ident", [128, 128], BF16)
    Bconst = Buf("const")
    idf = sb("idf", [128, 128], F32)
    bd = sb("bd", [128, 128], BF16)
    qz = sb("qz", [128, 4, 2, 128], BF16)
    Bqz = Buf("qz")
    onesD = sb("onesD", [128, 128], BF16)
    cneg = sb("cneg", [128, 128], F32)
    sil = sb("sil", [128, 2, T], BF16)
    Bsil = [Buf("sil0"), Buf("sil1")]
    ntmp = sb("ntmp", [128, D], F32)
    Bntmp = Buf("ntmp")
    nbf = sb("nbf", [128, D], BF16)
    Bnbf = Buf("nbf")
    sv = sb("sv", [128, 256], F32)
    Bsv = Buf("sv")
    cw = sb("cw", [128, 4, 31], F32)
    cb = sb("cb", [128, 4], F32)
    cng = sb("cng_s", [128, 4], F32)
    cnb = sb("cnb_s", [128, 4], F32)
    qg = sb("qg_s", [128, 1], F32)
    kg = sb("kg_s", [128, 1], F32)
    sccw = sb("sccw", [128, 8, 3], F32)
    Bpar = Buf("par")
    NBIG = 66048
    big = sb("big", [128, NBIG], BF16)

    def carve(off_bytes, nbytes, dt, pat=None, **kw):
        a = big[:, off_bytes // 2:(off_bytes + nbytes) // 2]
        if dt == F32:
            a = a.bitcast(F32)
        if pat:
            a = a.rearrange(pat, **kw)
        return a

    K = 1024
    o = 0
    kT = carve(o, 32 * K, BF16, "p (c s) -> p c s", c=4); o += 32 * K
    vc = carve(o, 32 * 8 * 65 * 2, BF16, "p (b f) -> p b f", b=32); o += 32 * 8 * 65 * 2
    o = (o + 63) // 64 * 64
    kiT = carve(o, 8 * K, BF16); o += 8 * K
    qT = carve(o, 4 * K, BF16, "p (c s) -> p c s", c=4); o += 4 * K
    qiT = carve(o, 4 * K, BF16, "p (c s) -> p c s", c=4); o += 4 * K
    mixT = carve(o, 8 * K, BF16, "p (c s) -> p c s", c=8); o += 8 * K
    yhist = carve(o, 4 * 32 * 2, BF16, "p (c s) -> p c s", c=4); o += 4 * 32 * 2
    wi_t = carve(o, 4 * 8 * 4, F32, "p (c s) -> p c s", c=4); o += 4 * 8 * 4
    W0 = o
    assert W0 + 40 * K <= NBIG * 2, (W0, NBIG * 2)
    BkT, Bvc, BkiT, BqT, BqiT, BmixT, Byh, Bwi = (Buf(n) for n in
                                                   ("kT", "vc", "kiT", "qT", "qiT", "mixT", "yh", "wi"))
    o = W0
    yv = carve(o, 4 * 544 * 2, BF16, "p (c s) -> p c s", c=4); o += 4 * 544 * 2
    dg = carve(o, 2 * 31 * 128 * 2, BF16, "p (b j s) -> p b j s", b=2, j=31); o += 2 * 31 * 128 * 2
    a_bf = carve(o, 4 * K, BF16, "p (c s) -> p c s", c=4); o += 4 * K
    a2_bf = carve(o, 4 * K, BF16, "p (c s) -> p c s", c=4); o += 4 * K
    mean_sb = carve(o, 2 * K, F32); o += 2 * K
    rstd_sb = carve(o, 2 * K, F32); o += 2 * K
    sgm = carve(o, 2 * K, F32); o += 2 * K
    assert o <= W0 + 40 * K, o - W0
    Byv, Babf, Ba2, Bmean, Brstd, Bsgm = (Buf(n) for n in ("yv", "abf", "a2", "mean", "rstd", "sgm"))
    Bdg = [[Buf(f"dg{b}_{j}") for j in range(31)] for b in range(2)]
    conv_bufs = [Byv, Babf, Ba2, Bmean, Brstd, Bsgm] + Bdg[0] + Bdg[1]
    o = W0
    score = carve(o, 16 * K, F32); o += 16 * K
    negm2 = carve(o, 16 * K, BF16, "p (c s) -> p c s", c=2); o += 16 * K
    rl = carve(o, 4 * K, F32, "p (c s) -> p c s", c=2); o += 4 * K
    PT = carve(o, 2 * K, BF16, "p (c s) -> p c s", c=2); o += 2 * K
    att = carve(o, 1 * K, BF16); o += 1 * K
    assert o <= W0 + 40 * K
    Bscore, Brl0, Brl1, BPT0, BPT1, Batt = (Buf(n) for n in ("score", "rl0", "rl1", "PT0", "PT1", "att"))
    Bnegm2 = [Buf("negm0"), Buf("negm1")]
    Bjd = [Buf("jd0"), Buf("jd1")]
    Bja = [Buf("ja0"), Buf("ja1")]
    attn_bufs = [Bscore, Brl0, Brl1, BPT0, BPT1, Batt] + Bnegm2 + Bjd + Bja
    act0 = carve(W0, 22 * K, BF16, "p (j s) -> p j s", j=NJ)
    Bact0 = [Buf(f"act0_{j}") for j in range(NJ)]
    l0_persist = [BkT, Bvc, BkiT, BqT, BqiT, BmixT, Byh, Bwi]
    o = 0
    wr32 = carve(o, 256, F32, "p (k e) -> p k e", e=NE)
    wr_hi = carve(o + 256, 128, BF16, "p (k e) -> p k e", e=NE)
    wr_lo = carve(o + 384, 128, BF16, "p (k e) -> p k e", e=NE)
    wrtmp = carve(o + 512, 256, F32, "p (k e) -> p k e", e=NE)
    lobf = carve(o + 1 * K, 2 * K, BF16)
    hTlo = carve(o + 3 * K, 2 * K, BF16, "p (c t) -> p c t", c=8)
    o += 32 * K
    Blo, BhTlo = Buf("lobf"), Buf("hTlo")
    act1 = carve(o, 22 * K, BF16, "p (j s) -> p j s", j=NJ); o += 22 * K
    mT = carve(o, 8 * K, BF16, "p (c s) -> p c s", c=8); o += 8 * K
    wdres = carve(o, 44 * K, BF16, "p (j n) -> p j n", j=NJ); o += 44 * K
    zt = carve(o, 516 * 4, F32); o += 516 * 4
    vsb = carve(o, 2 * K, F32); o += 2 * K
    c3 = carve(o, 2 * K, F32); o += 2 * K
    zh = carve(o, 8 * 2 * 4, F32, "p (c s) -> p c s", c=8); o += 64
    prod = carve(o, 8 * K, F32, "p (c s) -> p c s", c=2); o += 8 * K
    assert o <= NBIG * 2, o
    BwrB, BmT, Bzt, Bvsb, Bc3, Bzh = (Buf(n) for n in ("wrB", "mT", "zt", "vsb", "c3", "zh"))
    Bprod = [Buf("prod0"), Buf("prod1")]
    Bwd = [Buf(f"wd{j}") for j in range(NJ // 2)]
    Bact1 = [Buf(f"act1_{j}") for j in range(NJ)]
    l1_bufs = [BwrB, BmT, Bzt, Bvsb, Bc3, Bzh, Blo, BhTlo] + Bprod + Bact1 + Bwd

    SS, RS, MX, MN, RNG, LG, GT, W12, RD = 0, 4, 8, 9, 10, 16, 48, 80, 96
    NB2 = NIT + 2
    thr_t = sb("thr_t", [128, 2, NB2], F32)
    cnt_t = sb("cnt_t", [128, 2, NB2], F32)
    cact_t = sb("cact_t", [128, 2, NB2], F32)
    uu_t = sb("uu_t", [128, 2, NB2], F32)
    stp_t = sb("stp_t", [128, 2, NB2], F32)
    stp2_t = sb("stp2_t", [128, 2, NB2], F32)
    dd_t = sb("dd_t", [128, 2, NB2], F32)
    Bthr = [Buf("thr0"), Buf("thr1")]
    Bcd = [Buf("cd0"), Buf("cd1")]
    Bca = [Buf("ca0"), Buf("ca1")]
    Bst = [Buf("st0"), Buf("st1")]
    Bmm = [Buf("mm0"), Buf("mm1")]
    pw_t = sb("pw_t", [128, NIT + 2], F32)
    epsc = sb("epsc", [128, 1], F32)
    Bbis = Buf("bis")

    banks = [es.enter_context(nc.psum_tensor(f"ps{i}", [128, 512], F32)) for i in range(8)]
    Bbank = [Buf(f"bank{i}") for i in range(8)]
    st = {"ring": 0, "acc": 0, "w": 0}

    st.update({"ra": 0, "rb": 0})

    def ring_bank(group=None):
        if group == "a":
            i = 4 + st["ra"] % 2
            st["ra"] += 1
        elif group == "b":
            i = 6 + st["rb"] % 2
            st["rb"] += 1
        else:
            i = 4 + st["ring"] % 4
            st["ring"] += 1
        return banks[i], Bbank[i]

    def acc_pair():
        i = (st["acc"] % 2) * 2
        st["acc"] += 1
        return (banks[i], Bbank[i]), (banks[i + 1], Bbank[i + 1])

    def wslot():
        i = st["w"] % 4
        st["w"] += 1
        return wring[:, i, :], Bw[i]

    def mm(out, lhsT, rhs, start, stop, reads, writes):
        P.op("pe", lambda e: e.matmul(out, lhsT=lhsT, rhs=rhs, start=start, stop=stop), reads, writes)

    def recip3(out, in_, reads, writes):
        P.op("dve", lambda e: e.reciprocal(out=out, in_=in_), reads, writes)

    def tr(out, in_, reads, writes):
        P.op("pe", lambda e: e.transpose(out, in_, ident[:]), reads, writes)

    def act_(out, in_, func, reads, writes, **kw):
        P.op("act", lambda e: e.activation(out=out, in_=in_, func=func, **kw), reads, writes)

    def tt(eng, out, in0, in1, op, reads, writes):
        P.op(eng, lambda e: e.tensor_tensor(out=out, in0=in0, in1=in1, op=op), reads, writes)

    def ts(eng, out, in0, s1, s2, op0, op1, reads, writes, accum_out=None):
        if op1 is None:
            P.op(eng, lambda e: e.tensor_scalar(out=out, in0=in0, scalar1=s1, scalar2=None, op0=op0), reads, writes)
        elif accum_out is None:
            P.op(eng, lambda e: e.tensor_scalar(out=out, in0=in0, scalar1=s1, scalar2=s2, op0=op0, op1=op1), reads, writes)
        else:
            P.op(eng, lambda e: e.tensor_scalar(out=out, in0=in0, scalar1=s1, scalar2=s2, op0=op0, op1=op1,
                                                accum_out=accum_out), reads, writes)

    def stt(eng, out, in0, scalar, in1, op0, op1, reads, writes):
        P.op(eng, lambda e: e.scalar_tensor_tensor(out=out, in0=in0, scalar=scalar, in1=in1, op0=op0, op1=op1),
             reads, writes)

    def cp(eng, out, in_, reads, writes):
        if eng == "act":
            act_(out, in_, AF.Identity, reads, writes)
        else:
            P.op(eng, lambda e: e.tensor_copy(out=out, in_=in_), reads, writes)

    def memset(eng, ap, val, writes):
        P.op(eng, lambda e: e.memset(ap, val), (), writes)

    dbg_list = []

    def dump(name, ap, bufs):
        if not DEBUG:
            return
        if any(n == name for n, _ in dbg_list):
            return
        shp = list(ap.shape)
        dt_ = ap.dtype
        dten = nc.dram_tensor("dbg_" + name, shp, dt_, kind="ExternalOutput").ap()
        dbg_list.append((name, shp))
        P.dma("sp", dten, ap, reads=bufs)

    memset("pool", idf[:], 0.0, [Bconst])
    P.op("pool", lambda e: e.affine_select(out=idf[:], in_=idf[:], pattern=[[-1, 128]], compare_op=ALU.not_equal,
                                           fill=1.0, base=0, channel_multiplier=1), [Bconst], [Bconst])
    cp("dve", ident[:], idf[:], [Bconst], [Bconst])
    ident2 = idf[:].bitcast(BF16)
    cp("dve", ident2[:, 0:128], ident[:], [Bconst], [Bconst])
    cp("dve", ident2[:, 128:256], ident[:], [Bconst], [Bconst])
    memset("dve", bd[:], 0.0, [Bconst])
    memset("dve", bd[0:64, 0:64], 1.0 / 64, [Bconst])
    memset("dve", bd[64:128, 64:128], 1.0 / 64, [Bconst])
    memset("dve", onesD[:], 1.0 / 512, [Bconst])
    memset("dve", cneg[:], 0.0, [Bconst])
    memset("dve", cneg[0:64, 64:128], -1e30, [Bconst])
    memset("dve", epsc[:], EPS, [Bconst])
    for n in range(NIT + 2):
        memset("dve", pw_t[:, n:n + 1], 2.0 ** -(n + 2), [Bconst])
    P.dma("sp", cw[:], convw_d.rearrange("(c p) j -> p c j", p=128), writes=[Bpar])
    P.dma("sp", cb[:], convb_d, writes=[Bpar])
    P.dma("sp", cng[:], cng_d, writes=[Bpar])
    P.dma("sp", cnb[:], cnb_d, writes=[Bpar])
    P.dma("sp", qg[:], qg_d, writes=[Bpar])
    P.dma("sp", kg[:], kg_d, writes=[Bpar])
    P.dma("sp", sccw[:], sc_cw_d.rearrange("(c p) j -> p c j", p=128), writes=[Bpar])
    ts("dve", qg[:], qg[:], 0.125, None, ALU.mult, None, [Bpar], [Bpar])

    def convert(src, dst, rows, blk=256):
        bufs = []
        for r0 in range(0, rows, blk):
            r1 = min(rows, r0 + blk)
            b = Buf("cv")
            P.dma("pool", dst[r0:r1, :], src[r0:r1, :], writes=[b])
            bufs.append(b)
        return bufs

    cv = {}
    if DO_L0:
        cv["w_in0"] = convert(w_in0_d, s_w_in0, D)
        cv["w_out0"] = convert(w_out0_d, s_w_out0, D)
        cv["ffn_g"] = convert(ffn_g_d, s_ffn_g, D)
        cv["ffn_u"] = convert(ffn_u_d, s_ffn_u, D)
        cv["ffn_d"] = convert(ffn_d_d, s_ffn_d, DFF)
    l1_conv_jobs = []
    if DO_L1:
        l1_conv_jobs.append(("sc_in", sc_in_d, s_sc_in, D))
        l1_conv_jobs.append(("sc_out", sc_out_d, s_sc_out, D))
        for e in range(NE):
            l1_conv_jobs.append((f"moe_g{e}", moe_g_d[e], s_moe_g[e], D))
            l1_conv_jobs.append((f"moe_u{e}", moe_u_d[e], s_moe_u[e], D))
            l1_conv_jobs.append((f"moe_d{e}", moe_d_d[e], s_moe_d[e], DFF))

    def run_l1_conv(njobs):
        for _ in range(njobs):
            if l1_conv_jobs:
                name, src, dst, rows = l1_conv_jobs.pop(0)
                cv[name] = convert(src, dst, rows)

    cs32 = ntmp[:, 0:8 * NSEQ].rearrange("p (k s) -> p k s", k=8)
    csb = nbf[:, 0:8 * NSEQ].rearrange("p (k s) -> p k s", k=8)
    P.dma("sp", cs32, cT_d.rearrange("(k p) s -> p k s", p=128), writes=[Bntmp], allow_slow_non_contiguous=True)
    act_(csb, cs32, AF.Silu, [Bntmp], [Bnbf])
    modrow = big[0:NSEQ, 0:12288].bitcast(F32)
    gt2 = big[0:NSEQ, 12288:12288 + 4096].bitcast(F32)
    bias2 = big[0:NSEQ, 16384:16384 + 12288].bitcast(F32)
    Bmodrow, Bg2, Bb2 = Buf("modrow"), Buf("g2"), Buf("b2")
    Bmodv = Buf("modv")
    layers = ([0] if DO_L0 else []) + ([1] if DO_L1 else [])
    for l in layers:
        P.dma("sp", bias2, ada_b_d[l:l + 1, :].partition_broadcast(NSEQ), writes=[Bb2])
        P.dma("sp", gt2[:, 0:D], norm_g_d[2 * l:2 * l + 1, :].partition_broadcast(NSEQ), writes=[Bg2])
        P.dma("sp", gt2[:, D:2 * D], norm_g_d[2 * l + 1:2 * l + 2, :].partition_broadcast(NSEQ), writes=[Bg2])
        for nchunk in range(12):
            slot, bslot = wslot()
            sv3 = slot.rearrange("p (k n) -> p k n", k=8)
            P.dma("pool", sv3, ada_w_d[l].rearrange("(k p) n -> p k n", p=128)[:, :, nchunk * 512:(nchunk + 1) * 512],
                  writes=[bslot])
            pb, bpb = ring_bank()
            for kc in range(8):
                mm(pb[0:NSEQ, :], csb[:, kc, :], sv3[:, kc, :], kc == 0, kc == 7, [Bnbf, bslot], [bpb])
            tt("dve", modrow[:, nchunk * 512:(nchunk + 1) * 512], pb[0:NSEQ, :],
               bias2[:, nchunk * 512:(nchunk + 1) * 512], ALU.add, [bpb, Bb2], [Bmodrow])
        stt("dve", modrow[:, D:2 * D], modrow[:, D:2 * D], 1.0, gt2[:, 0:D], ALU.add, ALU.mult, [Bmodrow, Bg2], [Bmodrow])
        stt("dve", modrow[:, 4 * D:5 * D], modrow[:, 4 * D:5 * D], 1.0, gt2[:, D:2 * D], ALU.add, ALU.mult,
            [Bmodrow, Bg2], [Bmodrow])
        for k, src in enumerate((1, 0, 2, 4, 3, 5)):
            P.dma("sp", modv[l, k, :, :], modrow[:, src * D:(src + 1) * D], reads=[Bmodrow], writes=[Bmodv])
    P.handoff([Bmodrow, Bg2, Bb2], l0_persist + conv_bufs + l1_bufs)

    def load_mod(l, k, seq, second=False):
        if second:
            P.dma("sp", modt2[:], modv[l, k, seq:seq + 1, :].partition_broadcast(128), reads=[Bmodv], writes=[Bmod2])
        else:
            P.dma("sp", modt[:], modv[l, k, seq:seq + 1, :].partition_broadcast(128), reads=[Bmodv], writes=[Bmod])

    By = {}

    def load_x(src, row0):
        for s in range(4):
            r = row0 + s * 128
            rd = [By[r]] if (src is y_d and r in By) else []
            P.dma("pool", xt[:, s, :], src[r:r + 128, :], reads=rd, writes=[Bxt[s]])

    def store_x(row0):
        toks = []
        for s in range(4):
            r = row0 + s * 128
            By.setdefault(r, Buf(f"y{r}"))
            toks.append(P.dma("pool", y_d[r:r + 128, :], xt[:, s, :], reads=[Bxt[s]], writes=[By[r]]))
        return toks

    def norm_to_hT(l, which, seq, router=False):
        memset("dve", sv[:, SS:SS + 4], 0.0, [Bsv])
        if router:
            memset("dve", sv[:, LG:LG + 32], 0.0, [Bsv])
        for s in range(4):
            act_(nbf[:], xt[:, s, :], AF.Square, [Bxt[s]], [Bnbf, Bsv], accum_out=sv[:, SS + s:SS + s + 1])
        act_(sv[:, RS:RS + 4], sv[:, SS:SS + 4], AF.Sqrt, [Bsv, Bconst], [Bsv], bias=epsc[:, 0:1], scale=1.0 / D)
        P.op("dve", lambda e: e.reciprocal(out=sv[:, RS:RS + 4], in_=sv[:, RS:RS + 4]), [Bsv], [Bsv])
        load_mod(l, 3 * which + 0, seq)
        load_mod(l, 3 * which + 1, seq, second=True)
        for s in range(4):
            stt("dve", ntmp[:], xt[:, s, :], sv[:, RS + s:RS + s + 1], modt[:], ALU.mult, ALU.mult,
                [Bxt[s], Bsv, Bmod], [Bntmp])
            dump("modA", modt[:], [Bmod])
            dump("sv0", sv[:, 0:16], [Bsv])
            dump("ntmp0", ntmp[:], [Bntmp])
            if router:
                tt("dve", ntmp[:], ntmp[:], modt2[:], ALU.add, [Bntmp, Bmod2], [Bntmp])
                cp("act", nbf[:], ntmp[:], [Bntmp], [Bnbf])
                tt("dve", lobf, ntmp[:], nbf[:], ALU.subtract, [Bntmp, Bnbf], [Blo])
            else:
                tt("dve", nbf[:], ntmp[:], modt2[:], ALU.add, [Bntmp, Bmod2], [Bnbf])
            pb, bpb = ring_bank()
            pbb = pb[:].bitcast(BF16)
            for c in range(8):
                tr(pbb[:, c * 128:(c + 1) * 128], nbf[:, c * 128:(c + 1) * 128], [Bnbf, Bconst], [bpb])
            cp("act", hT[:, :, s * 128:(s + 1) * 128], pbb[:, 0:1024].rearrange("p (c t) -> p c t", c=8), [bpb], [BhT])
            if router:
                pl, bpl = ring_bank()
                plb = pl[:].bitcast(BF16)
                for c in range(8):
                    tr(plb[:, c * 128:(c + 1) * 128], lobf[:, c * 128:(c + 1) * 128], [Blo, Bconst], [bpl])
                cp("dve", hTlo, plb[:, 0:1024].rearrange("p (c t) -> p c t", c=8), [bpl], [BhTlo])
                pr, bpr = ring_bank()
                n_mm = 0
                for kc in range(8):
                    for (lh, blh, rw) in ((hT[:, kc, s * 128:(s + 1) * 128], BhT, wr_hi[:, kc, :]),
                                          (hTlo[:, kc, :], BhTlo, wr_hi[:, kc, :]),
                                          (hT[:, kc, s * 128:(s + 1) * 128], BhT, wr_lo[:, kc, :])):
                        mm(pr[:, 0:8], lh, rw, n_mm == 0, n_mm == 23, [blh, BwrB], [bpr])
                        n_mm += 1
                cp("dve", sv[:, LG + s * 8:LG + s * 8 + 8], pr[:, 0:8], [bpr], [Bsv])
            if s == 3:
                dump("hT", hT[:].rearrange("p c t -> p (c t)"), [BhT])

    def load_cols(Wbf, cvb, c0, w):
        slot, bslot = wslot()
        v = slot.rearrange("p (k n) -> p k n", k=8)
        P.dma("sp", v[:, :, 0:w], Wbf.rearrange("(k p) n -> p k n", p=128)[:, :, c0:c0 + w], reads=cvb, writes=[bslot])
        return v, bslot

    def out_proj_residual(Wbf, cvb, srcT, BsrcT, l, seq, which_gate):
        halves = []
        for hlf in range(2):
            halves.append(load_cols(Wbf, cvb, hlf * 512, 512))
        load_mod(l, which_gate, seq)
        for s in range(4):
            (pa, ba), (pb_, bb_) = acc_pair()
            for hlf, (pbk, bbk) in enumerate(((pa, ba), (pb_, bb_))):
                v, bslot = halves[hlf]
                for kc in range(8):
                    mm(pbk[:], srcT[:, kc, s * 128:(s + 1) * 128], v[:, kc, :], kc == 0, kc == 7, [BsrcT, bslot], [bbk])
                tt("dve", ntmp[:, hlf * 512:(hlf + 1) * 512], pbk[:], modt[:, hlf * 512:(hlf + 1) * 512], ALU.mult,
                   [bbk, Bmod], [Bntmp])
            tt("dve", xt[:, s, :], xt[:, s, :], ntmp[:], ALU.add, [Bxt[s], Bntmp], [Bxt[s]])

    def ffn_gate_up(Wg, Wu, cvg, cvu, actv, Bact):
        for q in range(6):
            c0 = q * 512
            w = min(512, DFF - c0)
            vg, bg = load_cols(Wg, cvg, c0, w)
            vu, bu = load_cols(Wu, cvu, c0, w)
            for jj in range(w // 128):
                j = q * 4 + jj
                pg, bpg = ring_bank()
                pu, bpu = ring_bank()
                for kc in range(8):
                    mm(pg[:], vg[:, kc, jj * 128:(jj + 1) * 128], hT[:, kc, :], kc == 0, kc == 7, [bg, BhT], [bpg])
                for kc in range(8):
                    mm(pu[:], vu[:, kc, jj * 128:(jj + 1) * 128], hT[:, kc, :], kc == 0, kc == 7, [bu, BhT], [bpu])
                si = j % 2
                act_(sil[:, si, :], pg[:], AF.Silu, [bpg], [Bsil[si]])
                tt("dve", actv[:, j, :], pu[:], sil[:, si, :], ALU.mult, [bpu, Bsil[si]], [Bact[j]])

    def layer0_tile(seq, t):
        row0 = seq * S + t * T
        tok0 = t * T
        P.handoff(Bact0, conv_bufs)
        load_x(x_d, row0)
        norm_to_hT(0, 0, seq)
        if t == 0:
            memset("pool", yhist[:], 0.0, [Byh])
            if seq == 0:
                memset("pool", vc[:], 1.0, [Bvc])
        va, ba = load_cols(s_w_in0, cv["w_in0"], 0, 512)
        vg, bg = load_cols(s_w_in0, cv["w_in0"], 512, 512)
        cp("pool", yv[:, :, 0:30], yhist[:, :, 0:30], [Byh], [Byv])
        for c in range(4):
            pa, bpa = ring_bank()
            pg, bpg = ring_bank()
            for kc in range(8):
                mm(pa[:], va[:, kc, c * 128:(c + 1) * 128], hT[:, kc, :], kc == 0, kc == 7, [ba, BhT], [bpa])
            for kc in range(8):
                mm(pg[:], vg[:, kc, c * 128:(c + 1) * 128], hT[:, kc, :], kc == 0, kc == 7, [bg, BhT], [bpg])
            act_(sgm[:], pg[:], AF.Sigmoid, [bpg], [Bsgm])
            tt("dve", yv[:, c, 30:542], pa[:], sgm[:], ALU.mult, [bpa, Bsgm], [Byv])
        cp("pool", yhist[:, :, 0:30], yv[:, :, 512:542], [Byv], [Byh])
        dump("yv", yv[:].rearrange("p c t -> p (c t)"), [Byv])
        for c in range(4):
            db = c % 2
            for j in range(31):
                if j % 2 == 0:
                    ts("dve", dg[:, db, j, :], ident[:], cw[:, c, j:j + 1], None, ALU.mult, None, [Bconst, Bpar], [Bdg[db][j]])
                else:
                    act_(dg[:, db, j, :], ident[:], AF.Identity, [Bconst, Bpar], [Bdg[db][j]], scale=cw[:, c, j:j + 1])
            pc, bpc = ring_bank()
            for j in range(31):
                mm(pc[:], dg[:, db, j, :], yv[:, c, j:j + 512], j == 0, j == 30, [Bdg[db][j], Byv], [bpc])
            act_(a_bf[:, c, :], pc[:], AF.Identity, [bpc, Bpar], [Babf], bias=cb[:, c:c + 1], scale=1.0)
            act_(a2_bf[:, c, :], pc[:], AF.Square, [bpc, Bpar], [Ba2], bias=cb[:, c:c + 1], scale=1.0)
        for which, (c0, dstT, Bdst, gain, col0) in enumerate(((1024, qT, BqT, qg, 0), (1536, kT, BkT, kg, tok0))):
            vq, bq = load_cols(s_w_in0, cv["w_in0"], c0, 512)
            for c in range(4):
                pq, bpq = ring_bank()
                for kc in range(8):
                    mm(pq[:], vq[:, kc, c * 128:(c + 1) * 128], hT[:, kc, :], kc == 0, kc == 7, [bq, BhT], [bpq])
                si = c % 2
                act_(sil[:, si, :], pq[:], AF.Square, [bpq], [Bsil[si]])
                pm, bpm = ring_bank()
                mm(pm[:], bd[:], sil[:, si, :], True, True, [Bconst, Bsil[si]], [bpm])
                act_(ntmp[:, 0:512], pm[:], AF.Sqrt, [bpm, Bconst], [Bntmp], bias=epsc[:, 0:1], scale=1.0)
                P.op("dve", lambda e: e.reciprocal(out=ntmp[:, 0:512], in_=ntmp[:, 0:512]), [Bntmp], [Bntmp])
                stt("dve", dstT[:, c, col0:col0 + 512], pq[:], gain[:, 0:1], ntmp[:, 0:512], ALU.mult, ALU.mult,
                    [bpq, Bpar, Bntmp], [Bdst])
        vv, bv = load_cols(s_w_in0, cv["w_in0"], 2048, 512)
        for s in range(4):
            pv, bpv = ring_bank()
            for kc in range(8):
                mm(pv[:], hT[:, kc, s * 128:(s + 1) * 128], vv[:, kc, :], kc == 0, kc == 7, [BhT, bv], [bpv])
            dst = vc[:, t * 4 + s, :].rearrange("p (h d) -> p h d", h=8)[:, :, 0:64]
            cp("act", dst, pv[:].rearrange("p (h d) -> p h d", h=8), [bpv], [Bvc])
        vqi, bqi = load_cols(s_w_in0, cv["w_in0"], 2560, 512)
        for c in range(4):
            pq, bpq = ring_bank()
            for kc in range(8):
                mm(pq[:], vqi[:, kc, c * 128:(c + 1) * 128], hT[:, kc, :], kc == 0, kc == 7, [bqi, BhT], [bpq])
            cp("act", qiT[:, c, :], pq[:], [bpq], [BqiT])
        slot, bslot = wslot()
        vk = slot.rearrange("p (k n) -> p k n", k=8)
        srcw = s_w_in0.rearrange("(k p) n -> p k n", p=128)
        P.dma("sp", vk[:, :, 0:64], srcw[:, :, 3072:3136], reads=cv["w_in0"], writes=[bslot])
        P.dma("sp", vk[:, :, 64:128], srcw[:, :, 3072:3136], reads=cv["w_in0"], writes=[bslot])
        P.dma("sp", vk[:, :, 128:136], srcw[:, :, 3136:3144], reads=cv["w_in0"], writes=[bslot])
        pk, bpk = ring_bank()
        for kc in range(8):
            mm(pk[:], vk[:, kc, 0:128], hT[:, kc, :], kc == 0, kc == 7, [bslot, BhT], [bpk])
        cp("act", kiT[:, tok0:tok0 + 512], pk[:], [bpk], [BkiT])
        pw, bpw = ring_bank()
        for s in range(4):
            for kc in range(8):
                mm(pw[:, s * 8:(s + 1) * 8], hT[:, kc, s * 128:(s + 1) * 128], vk[:, kc, 128:136], kc == 0, kc == 7,
                   [bslot, BhT], [bpw])
        ts("dve", wi_t[:, :, :], pw[:, 0:32].rearrange("p (s e) -> p s e", s=4), float(512 ** -0.5), None, ALU.mult, None,
           [bpw], [Bwi])
        pm, bpm = ring_bank()
        pq2, bpq2 = ring_bank()
        for c in range(4):
            mm(pm[:], onesD[:], a_bf[:, c, :], c == 0, c == 3, [Bconst, Babf], [bpm])
        for c in range(4):
            mm(pq2[:], onesD[:], a2_bf[:, c, :], c == 0, c == 3, [Bconst, Ba2], [bpq2])
        cp("act", mean_sb[:], pm[:], [bpm], [Bmean])
        tt("dve", rstd_sb[:], mean_sb[:], mean_sb[:], ALU.mult, [Bmean], [Brstd])
        tt("dve", rstd_sb[:], pq2[:], rstd_sb[:], ALU.subtract, [bpq2, Brstd], [Brstd])
        ts("dve", rstd_sb[:], rstd_sb[:], 0.0, None, ALU.max, None, [Brstd], [Brstd])
        act_(rstd_sb[:], rstd_sb[:], AF.Sqrt, [Brstd, Bconst], [Brstd], bias=epsc[:, 0:1], scale=1.0)
        P.op("dve", lambda e: e.reciprocal(out=rstd_sb[:], in_=rstd_sb[:]), [Brstd], [Brstd])
        for c in range(4):
            tt("dve", sgm[:], a_bf[:, c, :], mean_sb[:], ALU.subtract, [Babf, Bmean], [Bsgm])
            tt("dve", sgm[:], sgm[:], rstd_sb[:], ALU.mult, [Bsgm, Brstd], [Bsgm])
            act_(mixT[:, c, :], sgm[:], AF.Silu, [Bsgm, Bpar], [BmixT], bias=cnb[:, c:c + 1], scale=cng[:, c:c + 1])
        dump("abf", a_bf[:].rearrange("p c t -> p (c t)"), [Babf])
        dump("rstd", rstd_sb[:], [Brstd])
        dump("mean", mean_sb[:], [Bmean])
        dump("mixA", mixT[:, 0:4, :], [BmixT])
        dump("qT", qT[:].rearrange("p c t -> p (c t)"), [BqT])
        dump("kT", kT[:, :, 0:512], [BkT])
        dump("kiT", kiT[:, 0:512], [BkiT])
        dump("qiT", qiT[:].rearrange("p c t -> p (c t)"), [BqiT])
        dump("wi", wi_t[:].rearrange("p c t -> p (c t)"), [Bwi])
        dump("vc", vc[:, 0:4, :], [Bvc])
        P.handoff(conv_bufs, attn_bufs)
        Brl = [Brl0, Brl1]
        BPT = [BPT0, BPT1]

        def phase_a(s):
            p = s % 2
            qi_ = t * 4 + s
            nb = qi_ + 1
            L = nb * 128
            tq = slice(s * 128, (s + 1) * 128)
            negm = negm2[:, p, :]
            P.handoff([Bnegm2[p]], [Bjd[p], Bja[p]])
            ri = 0
            for k0 in range(0, L, 512):
                w = min(512, L - k0)
                for h in range(8):
                    c, hp = h // 2, h % 2
                    pd, bpd = ring_bank("a")
                    mm(pd[:, 0:w], qiT[hp * 64:(hp + 1) * 64, c, tq], kiT[hp * 64:(hp + 1) * 64, k0:k0 + w], True, True,
                       [BqiT, BkiT], [bpd])
                    r = ri % 2
                    ri += 1
                    act_(rl[:, r, 0:w], pd[:, 0:w], AF.Relu, [bpd], [Brl[r]])
                    if h == 0:
                        ts("dve", score[:, k0:k0 + w], rl[:, r, 0:w], wi_t[:, s, 0:1], None, ALU.mult, None,
                           [Brl[r], Bwi], [Bscore])
                    else:
                        stt("dve", score[:, k0:k0 + w], rl[:, r, 0:w], wi_t[:, s, h:h + 1], score[:, k0:k0 + w],
                            ALU.mult, ALU.add, [Brl[r], Bwi, Bscore], [Bscore])
                    if h % 4 == 3:
                        yield
            mxs = sv[:, 112 + p * 4:112 + p * 4 + 1]
            mns = sv[:, 113 + p * 4:113 + p * 4 + 1]
            rgs = sv[:, 114 + p * 4:114 + p * 4 + 1]
            if nb > 2:
                P.op("dve", lambda e: e.tensor_reduce(out=mxs, in_=score[:, 0:L], axis=AX.X, op=ALU.max), [Bscore], [Bmm[p]])
                P.op("dve", lambda e: e.tensor_reduce(out=mns, in_=score[:, 0:L], axis=AX.X, op=ALU.min), [Bscore], [Bmm[p]])
            tt("dve", score[:, L - 128:L], score[:, L - 128:L], cneg[:], ALU.add, [Bscore, Bconst], [Bscore])
            if nb <= 2:
                memset("dve", thr_t[:, p, NIT:NIT + 1], -1e29, [Bthr[p]])
            else:
                Lh = (nb // 2) * 128
                La = L - Lh
                tt("dve", rgs, mxs, mns, ALU.subtract, [Bmm[p]], [Bmm[p]])
                ts("dve", stp_t[:, p, :], pw_t[:, :], rgs, None, ALU.mult, None, [Bmm[p], Bconst], [Bst[p]])
                ts("dve", stp2_t[:, p, :], stp_t[:, p, :], 2.0, None, ALU.mult, None, [Bst[p]], [Bst[p]])
                stt("dve", thr_t[:, p, 0:1], rgs, 0.5, mns, ALU.mult, ALU.add, [Bmm[p]], [Bthr[p]])
                memset("dve", cnt_t[:, p, :], 0.0, [Bcd[p]])
                memset("dve", cact_t[:, p, :], 0.0, [Bca[p]])
                for n in range(NIT):
                    ts("dve", negm[:, 0:Lh], score[:, 0:Lh], thr_t[:, p, n:n + 1], 0.0, ALU.is_gt, ALU.add,
                       [Bscore, Bthr[p]], [Bjd[p], Bcd[p]], accum_out=cnt_t[:, p, n:n + 1])
                    act_(negm[:, Lh:L], score[:, Lh:L], AF.Sign, [Bscore, Bthr[p]], [Bja[p], Bca[p]],
                         bias=thr_t[:, p, n:n + 1], scale=-1.0, accum_out=cact_t[:, p, n:n + 1])
                    stt("dve", uu_t[:, p, n:n + 1], cnt_t[:, p, n:n + 1], 2.0, cact_t[:, p, n:n + 1], ALU.mult, ALU.subtract,
                        [Bcd[p], Bca[p]], [Bst[p]])
                    ts("dve", dd_t[:, p, n:n + 1], uu_t[:, p, n:n + 1], float(2 * TOPK - La), stp2_t[:, p, n:n + 1],
                       ALU.is_gt, ALU.mult, [Bst[p]], [Bst[p]])
                    stt("dve", thr_t[:, p, n + 1:n + 2], dd_t[:, p, n:n + 1], stp_t[:, p, n:n + 1], thr_t[:, p, n:n + 1],
                        ALU.subtract, ALU.add, [Bst[p], Bthr[p]], [Bthr[p]])
                    yield
            ts("dve", negm[:, 0:L], score[:, 0:L], thr_t[:, p, NIT:NIT + 1], -30000.0, ALU.is_le, ALU.mult,
               [Bscore, Bthr[p]], [Bnegm2[p], Bjd[p], Bja[p]])

        def phase_b(s):
            p = s % 2
            qi_ = t * 4 + s
            nb = qi_ + 1
            tq = slice(s * 128, (s + 1) * 128)
            negm = negm2[:, p, :]
            (oa, boa), (ob, bob) = acc_pair()
            memset("dve", qz[:], 0.0, [Bqz])
            cp("dve", qz[0:64, :, 0, :], qT[0:64, :, tq], [BqT], [Bqz])
            cp("dve", qz[64:128, :, 1, :], qT[64:128, :, tq], [BqT], [Bqz])
            units = [(c, g0) for c in range(4) for g0 in range(0, nb, 2)]

            def emit_lt(u):
                c, g0 = u
                ng = min(2, nb - g0)
                lt, blt = ring_bank("b")
                for j in range(ng):
                    kb = g0 + j
                    mm(lt[:, j * 256:(j + 1) * 256], kT[:, c, kb * 128:(kb + 1) * 128],
                       qz[:, c, :, :].rearrange("p a t -> p (a t)"), True, False, [BkT, Bqz], [blt])
                    mm(lt[:, j * 256:(j + 1) * 256], negm[:, kb * 128:(kb + 1) * 128], ident2[:, 0:256], False, True,
                       [Bnegm2[p], Bconst], [blt])
                return lt, blt

            cur = emit_lt(units[0])
            for i, (c, g0) in enumerate(units):
                lt, blt = cur
                if i + 1 < len(units):
                    cur = emit_lt(units[i + 1])
                ng = min(2, nb - g0)
                p_ = i % 2
                act_(PT[:, p_, 0:ng * 256], lt[:, 0:ng * 256], AF.Exp, [blt], [BPT[p_]])
                for j in range(ng):
                    kb = g0 + j
                    for hh, (ob_, bob_) in enumerate(((oa, boa), (ob, bob))):
                        h = 2 * c + hh
                        mm(ob_[:, c * 65:c * 65 + 65], PT[:, p_, j * 256 + hh * 128:j * 256 + (hh + 1) * 128],
                           vc[:, kb, h * 65:(h + 1) * 65], kb == 0, kb == nb - 1, [BPT[p_], Bvc], [bob_])
                yield
            yield "epilogue"
            for half, (o_, bo_) in enumerate(((oa, boa), (ob, bob))):
                ov = o_[:, 0:260].rearrange("p (h d) -> p h d", h=4)
                rd = sv[:, RD + half * 4:RD + half * 4 + 4]
                recip3(rd.unsqueeze(2), ov[:, :, 64:65], [bo_], [Bsv])
                tt("dve", att[:].rearrange("p (c hh d) -> p c hh d", c=4, hh=2)[:, :, half, :], ov[:, :, 0:64],
                   rd.unsqueeze(2).to_broadcast([128, 4, 64]), ALU.mult, [bo_, Bsv], [Batt])
            ptb, bptb = ring_bank()
            ptbb = ptb[:].bitcast(BF16)
            for c in range(4):
                tr(ptbb[:, c * 128:(c + 1) * 128], att[:, c * 128:(c + 1) * 128], [Batt, Bconst], [bptb])
            cp("act", mixT[:, 4:8, tq], ptbb[:, 0:512].rearrange("p (c t) -> p c t", c=4), [bptb], [BmixT])

        def drain(g):
            for _ in g:
                pass

        def interleave(ga, gb):
            a_alive, b_alive = True, True
            while a_alive or b_alive:
                if a_alive:
                    try:
                        next(ga)
                    except StopIteration:
                        a_alive = False
                if b_alive:
                    try:
                        r = next(gb)
                        if r == "epilogue":
                            b_alive = False
                    except StopIteration:
                        b_alive = False
            drain(ga)
            drain(gb)

        drain(phase_a(0))
        for s in range(4):
            gb = phase_b(s)
            if s + 1 < 4:
                interleave(phase_a(s + 1), gb)
            else:
                drain(gb)
        out_proj_residual(s_w_out0, cv["w_out0"], mixT, BmixT, 0, seq, 2)
        dump("mixT", mixT[:].rearrange("p c t -> p (c t)"), [BmixT])
        dump("x1", xt[:].rearrange("p c t -> p (c t)"), Bxt)
        P.handoff(attn_bufs, Bact0)
        norm_to_hT(0, 1, seq)
        ffn_gate_up(s_ffn_g, s_ffn_u, cv["ffn_g"], cv["ffn_u"], act0, Bact0)
        load_mod(0, 5, seq)
        wd_v = s_ffn_d.rearrange("(j p) n -> p j n", p=128)
        for q in range(6):
            nj = min(4, NJ - q * 4)
            slot, bslot = wslot()
            v = slot.rearrange("p (j n) -> p j n", j=4)
            P.dma("sp", v[:, 0:nj, :], wd_v[:, q * 4:q * 4 + nj, :], reads=cv["ffn_d"], writes=[bslot])
            for jj in range(nj):
                j = q * 4 + jj
                for s in range(4):
                    for hlf in range(2):
                        bk = 2 * s + hlf
                        mm(banks[bk][:], act0[:, j, s * 128:(s + 1) * 128], v[:, jj, hlf * 512:(hlf + 1) * 512], j == 0,
                           j == NJ - 1, [Bact0[j], bslot], [Bbank[bk]])
        for s in range(4):
            for hlf in range(2):
                bk = 2 * s + hlf
                tt("dve", ntmp[:, hlf * 512:(hlf + 1) * 512], banks[bk][:], modt[:, hlf * 512:(hlf + 1) * 512], ALU.mult,
                   [Bbank[bk], Bmod], [Bntmp])
            tt("dve", xt[:, s, :], xt[:, s, :], ntmp[:], ALU.add, [Bxt[s], Bntmp], [Bxt[s]])
        return store_x(row0)

    def layer1_tile(seq, t, src):
        row0 = seq * S + t * T
        load_x(src, row0)
        norm_to_hT(1, 0, seq)
        if t == 0:
            memset("pool", zh[:], 0.0, [Bzh])
        for c in range(8):
            if c % 4 == 0:
                vb, bb = load_cols(s_sc_in, cv["sc_in"], c * 128, 512)
                vc_, bc_ = load_cols(s_sc_in, cv["sc_in"], D + c * 128, 512)
                vv_, bv_ = load_cols(s_sc_in, cv["sc_in"], 2 * D + c * 128, 512)
            cc = c % 4
            b0 = (c % 2) * 4
            (pbg, bpbg), (pcg, bpcg), (pvv, bpvv) = ((banks[b0 + i], Bbank[b0 + i]) for i in range(3))
            for (pp, bpp, vw, bw_) in ((pbg, bpbg, vb, bb), (pcg, bpcg, vc_, bc_), (pvv, bpvv, vv_, bv_)):
                for kc in range(8):
                    mm(pp[:], vw[:, kc, cc * 128:(cc + 1) * 128], hT[:, kc, :], kc == 0, kc == 7, [bw_, BhT], [bpp])
            cp("act", vsb[:], pvv[:], [bpvv], [Bvsb])
            cp("dve", zt[:, 0:2], zh[:, c, :], [Bzh], [Bzt])
            tt("dve", zt[:, 2:514], pcg[:], vsb[:], ALU.mult, [bpcg, Bvsb], [Bzt])
            cp("dve", zh[:, c, :], zt[:, 512:514], [Bzt], [Bzh])
            ts("dve", c3[:], zt[:, 2:514], sccw[:, c, 2:3], None, ALU.mult, None, [Bzt, Bpar], [Bc3])
            stt("dve", c3[:], zt[:, 1:513], sccw[:, c, 1:2], c3[:], ALU.mult, ALU.add, [Bzt, Bpar, Bc3], [Bc3])
            stt("dve", c3[:], zt[:, 0:512], sccw[:, c, 0:1], c3[:], ALU.mult, ALU.add, [Bzt, Bpar, Bc3], [Bc3])
            tt("dve", mT[:, c, :], pbg[:], c3[:], ALU.mult, [bpbg, Bc3], [BmT])
        out_proj_residual(s_sc_out, cv["sc_out"], mT, BmT, 1, seq, 2)
        norm_to_hT(1, 1, seq, router=True)
        for s in range(4):
            lg = sv[:, LG + s * 8:LG + s * 8 + 8]
            gt_ = sv[:, GT + s * 8:GT + s * 8 + 8]
            top = sv[:, W12 + 0:W12 + 8]
            P.op("dve", lambda e, lg=lg, top=top: e.max(out=top, in_=lg), [Bsv], [Bsv])
            tt("dve", sv[:, W12 + 8:W12 + 9], sv[:, W12 + 0:W12 + 1], sv[:, W12 + 1:W12 + 2], ALU.subtract, [Bsv], [Bsv])
            act_(sv[:, W12 + 9:W12 + 10], sv[:, W12 + 8:W12 + 9], AF.Sigmoid, [Bsv], [Bsv])
            ts("dve", sv[:, W12 + 10:W12 + 11], sv[:, W12 + 9:W12 + 10], -1.0, 1.0, ALU.mult, ALU.add, [Bsv], [Bsv])
            ts("dve", gt_, lg, sv[:, W12 + 0:W12 + 1], sv[:, W12 + 9:W12 + 10], ALU.is_equal, ALU.mult, [Bsv], [Bsv])
            ts("dve", sv[:, W12 + 11:W12 + 19], lg, sv[:, W12 + 1:W12 + 2], sv[:, W12 + 10:W12 + 11], ALU.is_equal, ALU.mult,
               [Bsv], [Bsv])
            tt("dve", gt_, gt_, sv[:, W12 + 11:W12 + 19], ALU.add, [Bsv], [Bsv])
        load_mod(1, 5, seq)
        for e_ in range(NE):
            wd_v = s_moe_d[e_].rearrange("(j p) n -> p j n", p=128)
            cvd = cv[f"moe_d{e_}"]
            for q in range(0, NJ, 2):
                P.dma("pool", wdres[:, q:q + 2, :], wd_v[:, q:q + 2, :], reads=cvd, writes=[Bwd[q // 2]])
            ffn_gate_up(s_moe_g[e_], s_moe_u[e_], cv[f"moe_g{e_}"], cv[f"moe_u{e_}"], act1, Bact1)
            for s in range(4):
                (pa, ba), (pb_, bb_) = acc_pair()
                for j in range(NJ):
                    for hlf, (pbk, bbk) in enumerate(((pa, ba), (pb_, bb_))):
                        mm(pbk[:], act1[:, j, s * 128:(s + 1) * 128], wdres[:, j, hlf * 512:(hlf + 1) * 512], j == 0,
                           j == NJ - 1, [Bact1[j], Bwd[j // 2]], [bbk])
                for hlf, (pbk, bbk) in enumerate(((pa, ba), (pb_, bb_))):
                    stt("dve", ntmp[:, hlf * 512:(hlf + 1) * 512], pbk[:], sv[:, GT + s * 8 + e_:GT + s * 8 + e_ + 1],
                        modt[:, hlf * 512:(hlf + 1) * 512], ALU.mult, ALU.mult, [bbk, Bsv, Bmod], [Bntmp])
                tt("dve", xt[:, s, :], xt[:, s, :], ntmp[:], ALU.add, [Bxt[s], Bntmp], [Bxt[s]])
        return store_x(row0)

    final = []
    if DO_L0:
        per_tile_jobs = (len(l1_conv_jobs) + NTILES - 1) // max(1, NTILES)
        for seq in range(NSEQ):
            for t in range(NTILES):
                toks = layer0_tile(seq, t)
                if seq == 0:
                    run_l1_conv(per_tile_jobs)
                final = toks if not DO_L1 else final
                if not DO_L1:
                    final_all = final
    run_l1_conv(len(l1_conv_jobs))
    Bystore = Buf("ystore")
    if DO_L1:
        P.handoff(l0_persist + conv_bufs + attn_bufs + Bact0, l1_bufs)
        P.dma("sp", wr32, router_d.rearrange("(k p) e -> p k e", p=128), writes=[BwrB])
        cp("dve", wr_hi, wr32, [BwrB], [BwrB])
        tt("dve", wrtmp, wr32, wr_hi, ALU.subtract, [BwrB], [BwrB])
        cp("dve", wr_lo, wrtmp, [BwrB], [BwrB])
        src = y_d if DO_L0 else x_d
        for seq in range(NSEQ):
            for t in range(NTILES):
                final = layer1_tile(seq, t, src)
    for lane in range(NLANES):
        v = P.lane_val.get(("pool", lane), 0)
        if v:
            P.wait_tok("sp", ("dma", "pool", lane, v))
    P.build()
    es.close()
    nc._dbg_list = dbg_list
    return nc


def _prep_shared(inp):
    f = lambda a: np.ascontiguousarray(a, dtype=np.float32)
    d = {}
    d["ada_w"] = f(inp["ada_w"])
    d["ada_b"] = f(inp["ada_b"])
    d["norm_g"] = f(inp["norm_g"].reshape(4, D))
    d["w_in0"] = f(inp["ab_w_in"][0])
    d["convwT"] = f(inp["ab_conv_w"][0].T)
    d["convb"] = f(inp["ab_conv_b"][0].reshape(4, 128).T)
    d["cng"] = f(inp["ab_cnorm_g"][0].reshape(4, 128).T)
    d["cnb"] = f(inp["ab_cnorm_b"][0].reshape(4, 128).T)
    d["qg2"] = f(np.concatenate([inp["ab_q_g"][0], inp["ab_q_g"][0]]).reshape(128, 1))
    d["kg2"] = f(np.concatenate([inp["ab_k_g"][0], inp["ab_k_g"][0]]).reshape(128, 1))
    d["w_out0"] = f(inp["ab_w_out"][0])
    d["ffn_g"] = f(inp["ffn_w_gate"][0])
    d["ffn_u"] = f(inp["ffn_w_up"][0])
    d["ffn_d"] = f(inp["ffn_w_down"][0])
    d["sc_in"] = f(inp["sc_w_in"][0])
    d["sc_cwT"] = f(inp["sc_conv_w"][0].T)
    d["sc_out"] = f(inp["sc_w_out"][0])
    d["router"] = f(inp["moe_router"][0])
    d["moe_g"] = f(inp["moe_w_gate"][0])
    d["moe_u"] = f(inp["moe_w_up"][0])
    d["moe_d"] = f(inp["moe_w_down"][0])
    return d


def run(inputs, n_cores=8, nseq=2, **bk):
    shared = _prep_shared(inputs)
    x = np.asarray(inputs["x"], dtype=np.float32)
    c = np.asarray(inputs["c"], dtype=np.float32)
    nc = build_program(NSEQ=nseq, **bk)
    in_maps = []
    for i in range(n_cores):
        m = dict(shared)
        m["x"] = np.ascontiguousarray(x[i * nseq:(i + 1) * nseq].reshape(nseq * S, D))
        m["cT"] = np.ascontiguousarray(c[i * nseq:(i + 1) * nseq].T)
        in_maps.append(m)
    res = run_bass_kernel_spmd(nc, in_maps, core_ids=list(range(n_cores)))
    outs = [np.asarray(r["y"]).reshape(nseq, S, D) for r in res.results]
    return np.concatenate(outs, axis=0).astype(np.float32)


def kernel(**inputs):
    return run(inputs, n_cores=8, nseq=2)
```

```python
import numpy as np
from contextlib import ExitStack
import concourse.bass as bass
import concourse.mybir as mybir
from concourse.bass_utils import run_bass_kernel_spmd

F32 = mybir.dt.float32
BF16 = mybir.dt.bfloat16
ALU = mybir.AluOpType
AF = mybir.ActivationFunctionType
AX = mybir.AxisListType

D = 1024
S = 4096
T = 512
NT = S // T
DFF = 2816
NJ = DFF // 128
INW = 3144
NE = 8
EPS = 1e-6
NIT = 16
TOPK = 256

CH = 4096
NLANES = 8


class Buf:
    __slots__ = ("name", "w", "r")

    def __init__(self, name=""):
        self.name = name
        self.w = None
        self.r = {}


class Prog:
    ENG = ("pe", "act", "dve", "pool", "sp")

    def __init__(self, nc):
        self.nc = nc
        self.q = {e: [] for e in self.ENG}
        self.n = {e: 0 for e in self.ENG}
        self.seen = {e: {} for e in self.ENG}
        self.sems = {}
        self.lane_rr = {e: 0 for e in self.ENG}
        self.lane_val = {}
        self.used_lanes = set()

    @staticmethod
    def _kv(t):
        if t[0] == "eng":
            return ("eng", t[1]), t[2]
        return ("dma", t[1], t[2]), t[3]

    def _collect(self, eng, reads, writes):
        out = {}

        def add(kind, t):
            key, val = self._kv(t)
            if t[0] == "eng" and t[1] == eng:
                if eng == "pe" or kind != "raw":
                    return
            if self.seen[eng].get(key, -1) >= val:
                return
            if out.get(key, -1) < val:
                out[key] = val

        for b in reads:
            if b.w is not None:
                add("raw", b.w)
        for b in writes:
            if b.w is not None:
                add("waw", b.w)
            for t in b.r.values():
                add("war", t)
        return out

    def _emit_waits(self, eng, waits):
        for key, val in waits.items():
            self.seen[eng][key] = val
            if key[0] == "eng":
                seg, off = divmod(val, CH)
                self.q[eng].append(("wait", ("eng", key[1], seg), off + 1))
            else:
                self.q[eng].append(("wait", key, val))

    def _update(self, tok, reads, writes):
        key, _ = self._kv(tok)
        for b in reads:
            b.r[key] = tok
        for b in writes:
            b.w = tok
            b.r = {}

    def op(self, eng, fn, reads=(), writes=()):
        self._emit_waits(eng, self._collect(eng, reads, writes))
        n = self.n[eng]
        self.n[eng] = n + 1
        self.q[eng].append(("op", fn, ("eng", eng, n // CH)))
        tok = ("eng", eng, n)
        self._update(tok, reads, writes)
        return tok

    def dma(self, eng, out, in_, reads=(), writes=(), **kw):
        lane = self.lane_rr[eng]
        self.lane_rr[eng] = (lane + 1) % NLANES
        waits = self._collect(eng, reads, writes)
        prev = self.lane_val.get((eng, lane), 0)
        key = ("dma", eng, lane)
        if prev > 0 and self.seen[eng].get(key, -1) < prev:
            waits[key] = max(waits.get(key, -1), prev)
        self._emit_waits(eng, waits)
        v = prev + 16
        self.lane_val[(eng, lane)] = v
        self.used_lanes.add((eng, lane))
        self.q[eng].append(("dma", out, in_, kw, key))
        tok = ("dma", eng, lane, v)
        self._update(tok, reads, writes)
        return tok

    def wait_tok(self, eng, tok):
        key, val = self._kv(tok)
        if self.seen[eng].get(key, -1) >= val:
            return
        self._emit_waits(eng, {key: val})

    def handoff(self, old, new):
        toks = {}
        for b in old:
            cand = list(b.r.values())
            if b.w is not None:
                cand.append(b.w)
            for t in cand:
                key, val = self._kv(t)
                if key not in toks or self._kv(toks[key])[1] < val:
                    toks[key] = t
        for b in new:
            for key, t in toks.items():
                if key not in b.r or self._kv(b.r[key])[1] < self._kv(t)[1]:
                    b.r[key] = t

    def build(self):
        nc = self.nc
        with ExitStack() as es:
            for e in self.ENG:
                nseg = (self.n[e] + CH - 1) // CH
                for s in range(nseg):
                    self.sems[("eng", e, s)] = es.enter_context(nc.semaphore(f"c_{e}_{s}"))
            for (e, lane) in sorted(self.used_lanes):
                self.sems[("dma", e, lane)] = es.enter_context(nc.semaphore(f"d_{e}_{lane}"))
            block = es.enter_context(nc.Block())
            sems = self.sems

            def run(engobj, items):
                for it in items:
                    if it[0] == "wait":
                        engobj.wait_ge(sems[it[1]], it[2])
                    elif it[0] == "op":
                        it[1](engobj).then_inc(sems[it[2]], 1)
                    else:
                        _, out, in_, kw, key = it
                        engobj.dma_start(out=out, in_=in_, **kw).then_inc(sems[key], 16)

            @block.tensor
            def _(e):
                run(e, self.q["pe"])

            @block.scalar
            def _(e):
                run(e, self.q["act"])

            @block.vector
            def _(e):
                run(e, self.q["dve"])

            @block.gpsimd
            def _(e):
                run(e, self.q["pool"])

            @block.sync
            def _(e):
                run(e, self.q["sp"])


def build_program(NSEQ=2, DO_L0=True, DO_L1=True, NTILES=NT, DEBUG=False):
    nc = bass.Bass("TRN2", target_bir_lowering=False)
    P = Prog(nc)
    NTOK = NSEQ * S

    def din(name, shape):
        return nc.dram_tensor(name, shape, F32, kind="ExternalInput").ap()

    def dscr(name, shape, dt=BF16):
        return nc.dram_tensor(name, shape, dt, kind="Internal").ap()

    x_d = din("x", [NTOK, D])
    cT_d = din("cT", [D, NSEQ])
    ada_w_d = din("ada_w", [2, D, 6 * D])
    ada_b_d = din("ada_b", [2, 6 * D])
    norm_g_d = din("norm_g", [4, D])
    w_in0_d = din("w_in0", [D, INW])
    convw_d = din("convwT", [512, 31])
    convb_d = din("convb", [128, 4])
    cng_d = din("cng", [128, 4])
    cnb_d = din("cnb", [128, 4])
    qg_d = din("qg2", [128, 1])
    kg_d = din("kg2", [128, 1])
    w_out0_d = din("w_out0", [D, D])
    ffn_g_d = din("ffn_g", [D, DFF])
    ffn_u_d = din("ffn_u", [D, DFF])
    ffn_d_d = din("ffn_d", [DFF, D])
    sc_in_d = din("sc_in", [D, 3 * D])
    sc_cw_d = din("sc_cwT", [D, 3])
    sc_out_d = din("sc_out", [D, D])
    router_d = din("router", [D, NE])
    if DO_L1:
        moe_g_d = din("moe_g", [NE, D, DFF])
        moe_u_d = din("moe_u", [NE, D, DFF])
        moe_d_d = din("moe_d", [NE, DFF, D])
    y_d = nc.dram_tensor("y", [NTOK, D], F32, kind="ExternalOutput").ap()

    s_w_in0 = dscr("s_w_in0", [D, INW])
    s_w_out0 = dscr("s_w_out0", [D, D])
    s_ffn_g = dscr("s_ffn_g", [D, DFF])
    s_ffn_u = dscr("s_ffn_u", [D, DFF])
    s_ffn_d = dscr("s_ffn_d", [DFF, D])
    s_sc_in = dscr("s_sc_in", [D, 3 * D])
    s_sc_out = dscr("s_sc_out", [D, D])
    s_moe_g = dscr("s_moe_g", [NE, D, DFF])
    s_moe_u = dscr("s_moe_u", [NE, D, DFF])
    s_moe_d = dscr("s_moe_d", [NE, DFF, D])
    modv = dscr("modv", [2, 6, NSEQ, D], F32)

    es = ExitStack()

    def sb(name, shape, dt):
        return es.enter_context(nc.sbuf_tensor(name, shape, dt))

    xt = sb("xt", [128, 4, D], F32)
    Bxt = [Buf(f"xt{i}") for i in range(4)]
    hT = sb("hT", [128, 8, T], BF16)
    BhT = Buf("hT")
    wring = sb("wring", [128, 4, 4096], BF16)
    Bw = [Buf(f"w{i}") for i in range(4)]
    modt = sb("modt", [128, D], F32)
    Bmod = Buf("mod")
    modt2 = sb("modt2", [128, D], F32)
    Bmod2 = Buf("mod2")
    ident = sb("ident", [128, 128], BF16)
    Bconst = Buf("const")
    idf = sb("idf", [128, 128], F32)
    bd = sb("bd", [128, 128], BF16)
    qz = sb("qz", [128, 4, 2, 128], BF16)
    Bqz = Buf("qz")
    onesD = sb("onesD", [128, 128], BF16)
    cneg = sb("cneg", [128, 128], F32)
    sil = sb("sil", [128, 2, T], BF16)
    Bsil = [Buf("sil0"), Buf("sil1")]
    ntmp = sb("ntmp", [128, D], F32)
    Bntmp = Buf("ntmp")
    nbf = sb("nbf", [128, D], BF16)
    Bnbf = Buf("nbf")
    sv = sb("sv", [128, 256], F32)
    Bsv = Buf("sv")
    cw = sb("cw", [128, 4, 31], F32)
    cb = sb("cb", [128, 4], F32)
    cng = sb("cng_s", [128, 4], F32)
    cnb = sb("cnb_s", [128, 4], F32)
    qg = sb("qg_s", [128, 1], F32)
    kg = sb("kg_s", [128, 1], F32)
    sccw = sb("sccw", [128, 8, 3], F32)
    Bpar = Buf("par")
    NBIG = 66048
    big = sb("big", [128, NBIG], BF16)

    def carve(off_bytes, nbytes, dt, pat=None, **kw):
        a = big[:, off_bytes // 2:(off_bytes + nbytes) // 2]
        if dt == F32:
            a = a.bitcast(F32)
        if pat:
            a = a.rearrange(pat, **kw)
        return a

    K = 1024
    o = 0
    kT = carve(o, 32 * K, BF16, "p (c s) -> p c s", c=4); o += 32 * K
    vc = carve(o, 32 * 8 * 65 * 2, BF16, "p (b f) -> p b f", b=32); o += 32 * 8 * 65 * 2
    o = (o + 63) // 64 * 64
    kiT = carve(o, 8 * K, BF16); o += 8 * K
    qT = carve(o, 4 * K, BF16, "p (c s) -> p c s", c=4); o += 4 * K
    qiT = carve(o, 4 * K, BF16, "p (c s) -> p c s", c=4); o += 4 * K
    mixT = carve(o, 8 * K, BF16, "p (c s) -> p c s", c=8); o += 8 * K
    yhist = carve(o, 4 * 32 * 2, BF16, "p (c s) -> p c s", c=4); o += 4 * 32 * 2
    wi_t = carve(o, 4 * 8 * 4, F32, "p (c s) -> p c s", c=4); o += 4 * 8 * 4
    W0 = o
    assert W0 + 40 * K <= NBIG * 2, (W0, NBIG * 2)
    BkT, Bvc, BkiT, BqT, BqiT, BmixT, Byh, Bwi = (Buf(n) for n in
                                                   ("kT", "vc", "kiT", "qT", "qiT", "mixT", "yh", "wi"))
    o = W0
    yv = carve(o, 4 * 544 * 2, BF16, "p (c s) -> p c s", c=4); o += 4 * 544 * 2
    dg = carve(o, 2 * 31 * 128 * 2, BF16, "p (b j s) -> p b j s", b=2, j=31); o += 2 * 31 * 128 * 2
    a_bf = carve(o, 4 * K, BF16, "p (c s) -> p c s", c=4); o += 4 * K
    a2_bf = carve(o, 4 * K, BF16, "p (c s) -> p c s", c=4); o += 4 * K
    mean_sb = carve(o, 2 * K, F32); o += 2 * K
    rstd_sb = carve(o, 2 * K, F32); o += 2 * K
    sgm = carve(o, 2 * K, F32); o += 2 * K
    assert o <= W0 + 40 * K, o - W0
    Byv, Babf, Ba2, Bmean, Brstd, Bsgm = (Buf(n) for n in ("yv", "abf", "a2", "mean", "rstd", "sgm"))
    Bdg = [[Buf(f"dg{b}_{j}") for j in range(31)] for b in range(2)]
    conv_bufs = [Byv, Babf, Ba2, Bmean, Brstd, Bsgm] + Bdg[0] + Bdg[1]
    o = W0
    score = carve(o, 16 * K, F32); o += 16 * K
    negm2 = carve(o, 16 * K, BF16, "p (c s) -> p c s", c=2); o += 16 * K
    rl = carve(o, 4 * K, F32, "p (c s) -> p c s", c=2); o += 4 * K
    PT = carve(o, 2 * K, BF16, "p (c s) -> p c s", c=2); o += 2 * K
    att = carve(o, 1 * K, BF16); o += 1 * K
    assert o <= W0 + 40 * K
    Bscore, Brl0, Brl1, BPT0, BPT1, Batt = (Buf(n) for n in ("score", "rl0", "rl1", "PT0", "PT1", "att"))
    Bnegm2 = [Buf("negm0"), Buf("negm1")]
    Bjd = [Buf("jd0"), Buf("jd1")]
    Bja = [Buf("ja0"), Buf("ja1")]
    attn_bufs = [Bscore, Brl0, Brl1, BPT0, BPT1, Batt] + Bnegm2 + Bjd + Bja
    act0 = carve(W0, 22 * K, BF16, "p (j s) -> p j s", j=NJ)
    Bact0 = [Buf(f"act0_{j}") for j in range(NJ)]
    l0_persist = [BkT, Bvc, BkiT, BqT, BqiT, BmixT, Byh, Bwi]
    o = 0
    wr32 = carve(o, 256, F32, "p (k e) -> p k e", e=NE)
    wr_hi = carve(o + 256, 128, BF16, "p (k e) -> p k e", e=NE)
    wr_lo = carve(o + 384, 128, BF16, "p (k e) -> p k e", e=NE)
    wrtmp = carve(o + 512, 256, F32, "p (k e) -> p k e", e=NE)
    lobf = carve(o + 1 * K, 2 * K, BF16)
    hTlo = carve(o + 3 * K, 2 * K, BF16, "p (c t) -> p c t", c=8)
    o += 32 * K
    Blo, BhTlo = Buf("lobf"), Buf("hTlo")
    act1 = carve(o, 22 * K, BF16, "p (j s) -> p j s", j=NJ); o += 22 * K
    mT = carve(o, 8 * K, BF16, "p (c s) -> p c s", c=8); o += 8 * K
    wdres = carve(o, 44 * K, BF16, "p (j n) -> p j n", j=NJ); o += 44 * K
    zt = carve(o, 516 * 4, F32); o += 516 * 4
    vsb = carve(o, 2 * K, F32); o += 2 * K
    c3 = carve(o, 2 * K, F32); o += 2 * K
    zh = carve(o, 8 * 2 * 4, F32, "p (c s) -> p c s", c=8); o += 64
    prod = carve(o, 8 * K, F32, "p (c s) -> p c s", c=2); o += 8 * K
    assert o <= NBIG * 2, o
    BwrB, BmT, Bzt, Bvsb, Bc3, Bzh = (Buf(n) for n in ("wrB", "mT", "zt", "vsb", "c3", "zh"))
    Bprod = [Buf("prod0"), Buf("prod1")]
    Bwd = [Buf(f"wd{j}") for j in range(NJ // 2)]
    Bact1 = [Buf(f"act1_{j}") for j in range(NJ)]
    l1_bufs = [BwrB, BmT, Bzt, Bvsb, Bc3, Bzh, Blo, BhTlo] + Bprod + Bact1 + Bwd

    SS, RS, MX, MN, RNG, LG, GT, W12, RD = 0, 4, 8, 9, 10, 16, 48, 80, 96
    NB2 = NIT + 2
    thr_t = sb("thr_t", [128, 2, NB2], F32)
    cnt_t = sb("cnt_t", [128, 2, NB2], F32)
    cact_t = sb("cact_t", [128, 2, NB2], F32)
    uu_t = sb("uu_t", [128, 2, NB2], F32)
    stp_t = sb("stp_t", [128, 2, NB2], F32)
    stp2_t = sb("stp2_t", [128, 2, NB2], F32)
    dd_t = sb("dd_t", [128, 2, NB2], F32)
    Bthr = [Buf("thr0"), Buf("thr1")]
    Bcd = [Buf("cd0"), Buf("cd1")]
    Bca = [Buf("ca0"), Buf("ca1")]
    Bst = [Buf("st0"), Buf("st1")]
    Bmm = [Buf("mm0"), Buf("mm1")]
    pw_t = sb("pw_t", [128, NIT + 2], F32)
    epsc = sb("epsc", [128, 1], F32)
    Bbis = Buf("bis")

    banks = [es.enter_context(nc.psum_tensor(f"ps{i}", [128, 512], F32)) for i in range(8)]
    Bbank = [Buf(f"bank{i}") for i in range(8)]
    st = {"ring": 0, "acc": 0, "w": 0}

    st.update({"ra": 0, "rb": 0})

    def ring_bank(group=None):
        if group == "a":
            i = 4 + st["ra"] % 2
            st["ra"] += 1
        elif group == "b":
            i = 6 + st["rb"] % 2
            st["rb"] += 1
        else:
            i = 4 + st["ring"] % 4
            st["ring"] += 1
        return banks[i], Bbank[i]

    def acc_pair():
        i = (st["acc"] % 2) * 2
        st["acc"] += 1
        return (banks[i], Bbank[i]), (banks[i + 1], Bbank[i + 1])

    def wslot():
        i = st["w"] % 4
        st["w"] += 1
        return wring[:, i, :], Bw[i]

    def mm(out, lhsT, rhs, start, stop, reads, writes):
        P.op("pe", lambda e: e.matmul(out, lhsT=lhsT, rhs=rhs, start=start, stop=stop), reads, writes)

    def recip3(out, in_, reads, writes):
        P.op("dve", lambda e: e.reciprocal(out=out, in_=in_), reads, writes)

    def tr(out, in_, reads, writes):
        P.op("pe", lambda e: e.transpose(out, in_, ident[:]), reads, writes)

    def act_(out, in_, func, reads, writes, **kw):
        P.op("act", lambda e: e.activation(out=out, in_=in_, func=func, **kw), reads, writes)

    def tt(eng, out, in0, in1, op, reads, writes):
        P.op(eng, lambda e: e.tensor_tensor(out=out, in0=in0, in1=in1, op=op), reads, writes)

    def ts(eng, out, in0, s1, s2, op0, op1, reads, writes, accum_out=None):
        if op1 is None:
            P.op(eng, lambda e: e.tensor_scalar(out=out, in0=in0, scalar1=s1, scalar2=None, op0=op0), reads, writes)
        elif accum_out is None:
            P.op(eng, lambda e: e.tensor_scalar(out=out, in0=in0, scalar1=s1, scalar2=s2, op0=op0, op1=op1), reads, writes)
        else:
            P.op(eng, lambda e: e.tensor_scalar(out=out, in0=in0, scalar1=s1, scalar2=s2, op0=op0, op1=op1,
                                                accum_out=accum_out), reads, writes)

    def stt(eng, out, in0, scalar, in1, op0, op1, reads, writes):
        P.op(eng, lambda e: e.scalar_tensor_tensor(out=out, in0=in0, scalar=scalar, in1=in1, op0=op0, op1=op1),
             reads, writes)

    def cp(eng, out, in_, reads, writes):
        if eng == "act":
            act_(out, in_, AF.Identity, reads, writes)
        else:
            P.op(eng, lambda e: e.tensor_copy(out=out, in_=in_), reads, writes)

    def memset(eng, ap, val, writes):
        P.op(eng, lambda e: e.memset(ap, val), (), writes)

    dbg_list = []

    def dump(name, ap, bufs):
        if not DEBUG:
            return
        if any(n == name for n, _ in dbg_list):
            return
        shp = list(ap.shape)
        dt_ = ap.dtype
        dten = nc.dram_tensor("dbg_" + name, shp, dt_, kind="ExternalOutput").ap()
        dbg_list.append((name, shp))
        P.dma("sp", dten, ap, reads=bufs)

    memset("pool", idf[:], 0.0, [Bconst])
    P.op("pool", lambda e: e.affine_select(out=idf[:], in_=idf[:], pattern=[[-1, 128]], compare_op=ALU.not_equal,
                                           fill=1.0, base=0, channel_multiplier=1), [Bconst], [Bconst])
    cp("dve", ident[:], idf[:], [Bconst], [Bconst])
    ident2 = idf[:].bitcast(BF16)
    cp("dve", ident2[:, 0:128], ident[:], [Bconst], [Bconst])
    cp("dve", ident2[:, 128:256], ident[:], [Bconst], [Bconst])
    memset("dve", bd[:], 0.0, [Bconst])
    memset("dve", bd[0:64, 0:64], 1.0 / 64, [Bconst])
    memset("dve", bd[64:128, 64:128], 1.0 / 64, [Bconst])
    memset("dve", onesD[:], 1.0 / 512, [Bconst])
    memset("dve", cneg[:], 0.0, [Bconst])
    memset("dve", cneg[0:64, 64:128], -1e30, [Bconst])
    memset("dve", epsc[:], EPS, [Bconst])
    for n in range(NIT + 2):
        memset("dve", pw_t[:, n:n + 1], 2.0 ** -(n + 2), [Bconst])
    P.dma("sp", cw[:], convw_d.rearrange("(c p) j -> p c j", p=128), writes=[Bpar])
    P.dma("sp", cb[:], convb_d, writes=[Bpar])
    P.dma("sp", cng[:], cng_d, writes=[Bpar])
    P.dma("sp", cnb[:], cnb_d, writes=[Bpar])
    P.dma("sp", qg[:], qg_d, writes=[Bpar])
    P.dma("sp", kg[:], kg_d, writes=[Bpar])
    P.dma("sp", sccw[:], sc_cw_d.rearrange("(c p) j -> p c j", p=128), writes=[Bpar])
    ts("dve", qg[:], qg[:], 0.125, None, ALU.mult, None, [Bpar], [Bpar])

    def convert(src, dst, rows, blk=256):
        bufs = []
        for r0 in range(0, rows, blk):
            r1 = min(rows, r0 + blk)
            b = Buf("cv")
            P.dma("pool", dst[r0:r1, :], src[r0:r1, :], writes=[b])
            bufs.append(b)
        return bufs

    cv = {}
    if DO_L0:
        cv["w_in0"] = convert(w_in0_d, s_w_in0, D)
        cv["w_out0"] = convert(w_out0_d, s_w_out0, D)
        cv["ffn_g"] = convert(ffn_g_d, s_ffn_g, D)
        cv["ffn_u"] = convert(ffn_u_d, s_ffn_u, D)
        cv["ffn_d"] = convert(ffn_d_d, s_ffn_d, DFF)
    l1_conv_jobs = []
    if DO_L1:
        l1_conv_jobs.append(("sc_in", sc_in_d, s_sc_in, D))
        l1_conv_jobs.append(("sc_out", sc_out_d, s_sc_out, D))
        for e in range(NE):
            l1_conv_jobs.append((f"moe_g{e}", moe_g_d[e], s_moe_g[e], D))
            l1_conv_jobs.append((f"moe_u{e}", moe_u_d[e], s_moe_u[e], D))
            l1_conv_jobs.append((f"moe_d{e}", moe_d_d[e], s_moe_d[e], DFF))

    def run_l1_conv(njobs):
        for _ in range(njobs):
            if l1_conv_jobs:
                name, src, dst, rows = l1_conv_jobs.pop(0)
                cv[name] = convert(src, dst, rows)

    cs32 = ntmp[:, 0:8 * NSEQ].rearrange("p (k s) -> p k s", k=8)
    csb = nbf[:, 0:8 * NSEQ].rearrange("p (k s) -> p k s", k=8)
    P.dma("sp", cs32, cT_d.rearrange("(k p) s -> p k s", p=128), writes=[Bntmp], allow_slow_non_contiguous=True)
    act_(csb, cs32, AF.Silu, [Bntmp], [Bnbf])
    modrow = big[0:NSEQ, 0:12288].bitcast(F32)
    gt2 = big[0:NSEQ, 12288:12288 + 4096].bitcast(F32)
    bias2 = big[0:NSEQ, 16384:16384 + 12288].bitcast(F32)
    Bmodrow, Bg2, Bb2 = Buf("modrow"), Buf("g2"), Buf("b2")
    Bmodv = Buf("modv")
    layers = ([0] if DO_L0 else []) + ([1] if DO_L1 else [])
    for l in layers:
        P.dma("sp", bias2, ada_b_d[l:l + 1, :].partition_broadcast(NSEQ), writes=[Bb2])
        P.dma("sp", gt2[:, 0:D], norm_g_d[2 * l:2 * l + 1, :].partition_broadcast(NSEQ), writes=[Bg2])
        P.dma("sp", gt2[:, D:2 * D], norm_g_d[2 * l + 1:2 * l + 2, :].partition_broadcast(NSEQ), writes=[Bg2])
        for nchunk in range(12):
            slot, bslot = wslot()
            sv3 = slot.rearrange("p (k n) -> p k n", k=8)
            P.dma("pool", sv3, ada_w_d[l].rearrange("(k p) n -> p k n", p=128)[:, :, nchunk * 512:(nchunk + 1) * 512],
                  writes=[bslot])
            pb, bpb = ring_bank()
            for kc in range(8):
                mm(pb[0:NSEQ, :], csb[:, kc, :], sv3[:, kc, :], kc == 0, kc == 7, [Bnbf, bslot], [bpb])
            tt("dve", modrow[:, nchunk * 512:(nchunk + 1) * 512], pb[0:NSEQ, :],
               bias2[:, nchunk * 512:(nchunk + 1) * 512], ALU.add, [bpb, Bb2], [Bmodrow])
        stt("dve", modrow[:, D:2 * D], modrow[:, D:2 * D], 1.0, gt2[:, 0:D], ALU.add, ALU.mult, [Bmodrow, Bg2], [Bmodrow])
        stt("dve", modrow[:, 4 * D:5 * D], modrow[:, 4 * D:5 * D], 1.0, gt2[:, D:2 * D], ALU.add, ALU.mult,
            [Bmodrow, Bg2], [Bmodrow])
        for k, src in enumerate((1, 0, 2, 4, 3, 5)):
            P.dma("sp", modv[l, k, :, :], modrow[:, src * D:(src + 1) * D], reads=[Bmodrow], writes=[Bmodv])
    P.handoff([Bmodrow, Bg2, Bb2], l0_persist + conv_bufs + l1_bufs)

    def load_mod(l, k, seq, second=False):
        if second:
            P.dma("sp", modt2[:], modv[l, k, seq:seq + 1, :].partition_broadcast(128), reads=[Bmodv], writes=[Bmod2])
        else:
            P.dma("sp", modt[:], modv[l, k, seq:seq + 1, :].partition_broadcast(128), reads=[Bmodv], writes=[Bmod])

    By = {}

    def load_x(src, row0):
        for s in range(4):
            r = row0 + s * 128
            rd = [By[r]] if (src is y_d and r in By) else []
            P.dma("pool", xt[:, s, :], src[r:r + 128, :], reads=rd, writes=[Bxt[s]])

    def store_x(row0):
        toks = []
        for s in range(4):
            r = row0 + s * 128
            By.setdefault(r, Buf(f"y{r}"))
            toks.append(P.dma("pool", y_d[r:r + 128, :], xt[:, s, :], reads=[Bxt[s]], writes=[By[r]]))
        return toks

    def norm_to_hT(l, which, seq, router=False):
        memset("dve", sv[:, SS:SS + 4], 0.0, [Bsv])
        if router:
            memset("dve", sv[:, LG:LG + 32], 0.0, [Bsv])
        for s in range(4):
            act_(nbf[:], xt[:, s, :], AF.Square, [Bxt[s]], [Bnbf, Bsv], accum_out=sv[:, SS + s:SS + s + 1])
        act_(sv[:, RS:RS + 4], sv[:, SS:SS + 4], AF.Sqrt, [Bsv, Bconst], [Bsv], bias=epsc[:, 0:1], scale=1.0 / D)
        P.op("dve", lambda e: e.reciprocal(out=sv[:, RS:RS + 4], in_=sv[:, RS:RS + 4]), [Bsv], [Bsv])
        load_mod(l, 3 * which + 0, seq)
        load_mod(l, 3 * which + 1, seq, second=True)
        for s in range(4):
            stt("dve", ntmp[:], xt[:, s, :], sv[:, RS + s:RS + s + 1], modt[:], ALU.mult, ALU.mult,
                [Bxt[s], Bsv, Bmod], [Bntmp])
            dump("modA", modt[:], [Bmod])
            dump("sv0", sv[:, 0:16], [Bsv])
            dump("ntmp0", ntmp[:], [Bntmp])
            if router:
                tt("dve", ntmp[:], ntmp[:], modt2[:], ALU.add, [Bntmp, Bmod2], [Bntmp])
                cp("act", nbf[:], ntmp[:], [Bntmp], [Bnbf])
                tt("dve", lobf, ntmp[:], nbf[:], ALU.subtract, [Bntmp, Bnbf], [Blo])
            else:
                tt("dve", nbf[:], ntmp[:], modt2[:], ALU.add, [Bntmp, Bmod2], [Bnbf])
            pb, bpb = ring_bank()
            pbb = pb[:].bitcast(BF16)
            for c in range(8):
                tr(pbb[:, c * 128:(c + 1) * 128], nbf[:, c * 128:(c + 1) * 128], [Bnbf, Bconst], [bpb])
            cp("act", hT[:, :, s * 128:(s + 1) * 128], pbb[:, 0:1024].rearrange("p (c t) -> p c t", c=8), [bpb], [BhT])
            if router:
                pl, bpl = ring_bank()
                plb = pl[:].bitcast(BF16)
                for c in range(8):
                    tr(plb[:, c * 128:(c + 1) * 128], lobf[:, c * 128:(c + 1) * 128], [Blo, Bconst], [bpl])
                cp("dve", hTlo, plb[:, 0:1024].rearrange("p (c t) -> p c t", c=8), [bpl], [BhTlo])
                pr, bpr = ring_bank()
                n_mm = 0
                for kc in range(8):
                    for (lh, blh, rw) in ((hT[:, kc, s * 128:(s + 1) * 128], BhT, wr_hi[:, kc, :]),
                                          (hTlo[:, kc, :], BhTlo, wr_hi[:, kc, :]),
                                          (hT[:, kc, s * 128:(s + 1) * 128], BhT, wr_lo[:, kc, :])):
                        mm(pr[:, 0:8], lh, rw, n_mm == 0, n_mm == 23, [blh, BwrB], [bpr])
                        n_mm += 1
                cp("dve", sv[:, LG + s * 8:LG + s * 8 + 8], pr[:, 0:8], [bpr], [Bsv])
            if s == 3:
                dump("hT", hT[:].rearrange("p c t -> p (c t)"), [BhT])

    def load_cols(Wbf, cvb, c0, w):
        slot, bslot = wslot()
        v = slot.rearrange("p (k n) -> p k n", k=8)
        P.dma("sp", v[:, :, 0:w], Wbf.rearrange("(k p) n -> p k n", p=128)[:, :, c0:c0 + w], reads=cvb, writes=[bslot])
        return v, bslot

    def out_proj_residual(Wbf, cvb, srcT, BsrcT, l, seq, which_gate):
        halves = []
        for hlf in range(2):
            halves.append(load_cols(Wbf, cvb, hlf * 512, 512))
        load_mod(l, which_gate, seq)
        for s in range(4):
            (pa, ba), (pb_, bb_) = acc_pair()
            for hlf, (pbk, bbk) in enumerate(((pa, ba), (pb_, bb_))):
                v, bslot = halves[hlf]
                for kc in range(8):
                    mm(pbk[:], srcT[:, kc, s * 128:(s + 1) * 128], v[:, kc, :], kc == 0, kc == 7, [BsrcT, bslot], [bbk])
                tt("dve", ntmp[:, hlf * 512:(hlf + 1) * 512], pbk[:], modt[:, hlf * 512:(hlf + 1) * 512], ALU.mult,
                   [bbk, Bmod], [Bntmp])
            tt("dve", xt[:, s, :], xt[:, s, :], ntmp[:], ALU.add, [Bxt[s], Bntmp], [Bxt[s]])

    def ffn_gate_up(Wg, Wu, cvg, cvu, actv, Bact):
        for q in range(6):
            c0 = q * 512
            w = min(512, DFF - c0)
            vg, bg = load_cols(Wg, cvg, c0, w)
            vu, bu = load_cols(Wu, cvu, c0, w)
            for jj in range(w // 128):
                j = q * 4 + jj
                pg, bpg = ring_bank()
                pu, bpu = ring_bank()
                for kc in range(8):
                    mm(pg[:], vg[:, kc, jj * 128:(jj + 1) * 128], hT[:, kc, :], kc == 0, kc == 7, [bg, BhT], [bpg])
                for kc in range(8):
                    mm(pu[:], vu[:, kc, jj * 128:(jj + 1) * 128], hT[:, kc, :], kc == 0, kc == 7, [bu, BhT], [bpu])
                si = j % 2
                act_(sil[:, si, :], pg[:], AF.Silu, [bpg], [Bsil[si]])
                tt("dve", actv[:, j, :], pu[:], sil[:, si, :], ALU.mult, [bpu, Bsil[si]], [Bact[j]])

    def layer0_tile(seq, t):
        row0 = seq * S + t * T
        tok0 = t * T
        P.handoff(Bact0, conv_bufs)
        load_x(x_d, row0)
        norm_to_hT(0, 0, seq)
        if t == 0:
            memset("pool", yhist[:], 0.0, [Byh])
            if seq == 0:
                memset("pool", vc[:], 1.0, [Bvc])
        va, ba = load_cols(s_w_in0, cv["w_in0"], 0, 512)
        vg, bg = load_cols(s_w_in0, cv["w_in0"], 512, 512)
        cp("pool", yv[:, :, 0:30], yhist[:, :, 0:30], [Byh], [Byv])
        for c in range(4):
            pa, bpa = ring_bank()
            pg, bpg = ring_bank()
            for kc in range(8):
                mm(pa[:], va[:, kc, c * 128:(c + 1) * 128], hT[:, kc, :], kc == 0, kc == 7, [ba, BhT], [bpa])
            for kc in range(8):
                mm(pg[:], vg[:, kc, c * 128:(c + 1) * 128], hT[:, kc, :], kc == 0, kc == 7, [bg, BhT], [bpg])
            act_(sgm[:], pg[:], AF.Sigmoid, [bpg], [Bsgm])
            tt("dve", yv[:, c, 30:542], pa[:], sgm[:], ALU.mult, [bpa, Bsgm], [Byv])
        cp("pool", yhist[:, :, 0:30], yv[:, :, 512:542], [Byv], [Byh])
        dump("yv", yv[:].rearrange("p c t -> p (c t)"), [Byv])
        for c in range(4):
            db = c % 2
            for j in range(31):
                if j % 2 == 0:
                    ts("dve", dg[:, db, j, :], ident[:], cw[:, c, j:j + 1], None, ALU.mult, None, [Bconst, Bpar], [Bdg[db][j]])
                else:
                    act_(dg[:, db, j, :], ident[:], AF.Identity, [Bconst, Bpar], [Bdg[db][j]], scale=cw[:, c, j:j + 1])
            pc, bpc = ring_bank()
            for j in range(31):
                mm(pc[:], dg[:, db, j, :], yv[:, c, j:j + 512], j == 0, j == 30, [Bdg[db][j], Byv], [bpc])
            act_(a_bf[:, c, :], pc[:], AF.Identity, [bpc, Bpar], [Babf], bias=cb[:, c:c + 1], scale=1.0)
            act_(a2_bf[:, c, :], pc[:], AF.Square, [bpc, Bpar], [Ba2], bias=cb[:, c:c + 1], scale=1.0)
        for which, (c0, dstT, Bdst, gain, col0) in enumerate(((1024, qT, BqT, qg, 0), (1536, kT, BkT, kg, tok0))):
            vq, bq = load_cols(s_w_in0, cv["w_in0"], c0, 512)
            for c in range(4):
                pq, bpq = ring_bank()
                for kc in range(8):
                    mm(pq[:], vq[:, kc, c * 128:(c + 1) * 128], hT[:, kc, :], kc == 0, kc == 7, [bq, BhT], [bpq])
                si = c % 2
                act_(sil[:, si, :], pq[:], AF.Square, [bpq], [Bsil[si]])
                pm, bpm = ring_bank()
                mm(pm[:], bd[:], sil[:, si, :], True, True, [Bconst, Bsil[si]], [bpm])
                act_(ntmp[:, 0:512], pm[:], AF.Sqrt, [bpm, Bconst], [Bntmp], bias=epsc[:, 0:1], scale=1.0)
                P.op("dve", lambda e: e.reciprocal(out=ntmp[:, 0:512], in_=ntmp[:, 0:512]), [Bntmp], [Bntmp])
                stt("dve", dstT[:, c, col0:col0 + 512], pq[:], gain[:, 0:1], ntmp[:, 0:512], ALU.mult, ALU.mult,
                    [bpq, Bpar, Bntmp], [Bdst])
        vv, bv = load_cols(s_w_in0, cv["w_in0"], 2048, 512)
        for s in range(4):
            pv, bpv = ring_bank()
            for kc in range(8):
                mm(pv[:], hT[:, kc, s * 128:(s + 1) * 128], vv[:, kc, :], kc == 0, kc == 7, [BhT, bv], [bpv])
            dst = vc[:, t * 4 + s, :].rearrange("p (h d) -> p h d", h=8)[:, :, 0:64]
            cp("act", dst, pv[:].rearrange("p (h d) -> p h d", h=8), [bpv], [Bvc])
        vqi, bqi = load_cols(s_w_in0, cv["w_in0"], 2560, 512)
        for c in range(4):
            pq, bpq = ring_bank()
            for kc in range(8):
                mm(pq[:], vqi[:, kc, c * 128:(c + 1) * 128], hT[:, kc, :], kc == 0, kc == 7, [bqi, BhT], [bpq])
            cp("act", qiT[:, c, :], pq[:], [bpq], [BqiT])
        slot, bslot = wslot()
        vk = slot.rearrange("p (k n) -> p k n", k=8)
        srcw = s_w_in0.rearrange("(k p) n -> p k n", p=128)
        P.dma("sp", vk[:, :, 0:64], srcw[:, :, 3072:3136], reads=cv["w_in0"], writes=[bslot])
        P.dma("sp", vk[:, :, 64:128], srcw[:, :, 3072:3136], reads=cv["w_in0"], writes=[bslot])
        P.dma("sp", vk[:, :, 128:136], srcw[:, :, 3136:3144], reads=cv["w_in0"], writes=[bslot])
        pk, bpk = ring_bank()
        for kc in range(8):
            mm(pk[:], vk[:, kc, 0:128], hT[:, kc, :], kc == 0, kc == 7, [bslot, BhT], [bpk])
        cp("act", kiT[:, tok0:tok0 + 512], pk[:], [bpk], [BkiT])
        pw, bpw = ring_bank()
        for s in range(4):
            for kc in range(8):
                mm(pw[:, s * 8:(s + 1) * 8], hT[:, kc, s * 128:(s + 1) * 128], vk[:, kc, 128:136], kc == 0, kc == 7,
                   [bslot, BhT], [bpw])
        ts("dve", wi_t[:, :, :], pw[:, 0:32].rearrange("p (s e) -> p s e", s=4), float(512 ** -0.5), None, ALU.mult, None,
           [bpw], [Bwi])
        pm, bpm = ring_bank()
        pq2, bpq2 = ring_bank()
        for c in range(4):
            mm(pm[:], onesD[:], a_bf[:, c, :], c == 0, c == 3, [Bconst, Babf], [bpm])
        for c in range(4):
            mm(pq2[:], onesD[:], a2_bf[:, c, :], c == 0, c == 3, [Bconst, Ba2], [bpq2])
        cp("act", mean_sb[:], pm[:], [bpm], [Bmean])
        tt("dve", rstd_sb[:], mean_sb[:], mean_sb[:], ALU.mult, [Bmean], [Brstd])
        tt("dve", rstd_sb[:], pq2[:], rstd_sb[:], ALU.subtract, [bpq2, Brstd], [Brstd])
        ts("dve", rstd_sb[:], rstd_sb[:], 0.0, None, ALU.max, None, [Brstd], [Brstd])
        act_(rstd_sb[:], rstd_sb[:], AF.Sqrt, [Brstd, Bconst], [Brstd], bias=epsc[:, 0:1], scale=1.0)
        P.op("dve", lambda e: e.reciprocal(out=rstd_sb[:], in_=rstd_sb[:]), [Brstd], [Brstd])
        for c in range(4):
            tt("dve", sgm[:], a_bf[:, c, :], mean_sb[:], ALU.subtract, [Babf, Bmean], [Bsgm])
            tt("dve", sgm[:], sgm[:], rstd_sb[:], ALU.mult, [Bsgm, Brstd], [Bsgm])
            act_(mixT[:, c, :], sgm[:], AF.Silu, [Bsgm, Bpar], [BmixT], bias=cnb[:, c:c + 1], scale=cng[:, c:c + 1])
        dump("abf", a_bf[:].rearrange("p c t -> p (c t)"), [Babf])
        dump("rstd", rstd_sb[:], [Brstd])
        dump("mean", mean_sb[:], [Bmean])
        dump("mixA", mixT[:, 0:4, :], [BmixT])
        dump("qT", qT[:].rearrange("p c t -> p (c t)"), [BqT])
        dump("kT", kT[:, :, 0:512], [BkT])
        dump("kiT", kiT[:, 0:512], [BkiT])
        dump("qiT", qiT[:].rearrange("p c t -> p (c t)"), [BqiT])
        dump("wi", wi_t[:].rearrange("p c t -> p (c t)"), [Bwi])
        dump("vc", vc[:, 0:4, :], [Bvc])
        P.handoff(conv_bufs, attn_bufs)
        Brl = [Brl0, Brl1]
        BPT = [BPT0, BPT1]

        def phase_a(s):
            p = s % 2
            qi_ = t * 4 + s
            nb = qi_ + 1
            L = nb * 128
            tq = slice(s * 128, (s + 1) * 128)
            negm = negm2[:, p, :]
            P.handoff([Bnegm2[p]], [Bjd[p], Bja[p]])
            ri = 0
            for k0 in range(0, L, 512):
                w = min(512, L - k0)
                for h in range(8):
                    c, hp = h // 2, h % 2
                    pd, bpd = ring_bank("a")
                    mm(pd[:, 0:w], qiT[hp * 64:(hp + 1) * 64, c, tq], kiT[hp * 64:(hp + 1) * 64, k0:k0 + w], True, True,
                       [BqiT, BkiT], [bpd])
                    r = ri % 2
                    ri += 1
                    act_(rl[:, r, 0:w], pd[:, 0:w], AF.Relu, [bpd], [Brl[r]])
                    if h == 0:
                        ts("dve", score[:, k0:k0 + w], rl[:, r, 0:w], wi_t[:, s, 0:1], None, ALU.mult, None,
                           [Brl[r], Bwi], [Bscore])
                    else:
                        stt("dve", score[:, k0:k0 + w], rl[:, r, 0:w], wi_t[:, s, h:h + 1], score[:, k0:k0 + w],
                            ALU.mult, ALU.add, [Brl[r], Bwi, Bscore], [Bscore])
                    if h % 4 == 3:
                        yield
            mxs = sv[:, 112 + p * 4:112 + p * 4 + 1]
            mns = sv[:, 113 + p * 4:113 + p * 4 + 1]
            rgs = sv[:, 114 + p * 4:114 + p * 4 + 1]
            if nb > 2:
                P.op("dve", lambda e: e.tensor_reduce(out=mxs, in_=score[:, 0:L], axis=AX.X, op=ALU.max), [Bscore], [Bmm[p]])
                P.op("dve", lambda e: e.tensor_reduce(out=mns, in_=score[:, 0:L], axis=AX.X, op=ALU.min), [Bscore], [Bmm[p]])
            tt("dve", score[:, L - 128:L], score[:, L - 128:L], cneg[:], ALU.add, [Bscore, Bconst], [Bscore])
            if nb <= 2:
                memset("dve", thr_t[:, p, NIT:NIT + 1], -1e29, [Bthr[p]])
            else:
                Lh = (nb // 2) * 128
                La = L - Lh
                tt("dve", rgs, mxs, mns, ALU.subtract, [Bmm[p]], [Bmm[p]])
                ts("dve", stp_t[:, p, :], pw_t[:, :], rgs, None, ALU.mult, None, [Bmm[p], Bconst], [Bst[p]])
                ts("dve", stp2_t[:, p, :], stp_t[:, p, :], 2.0, None, ALU.mult, None, [Bst[p]], [Bst[p]])
                stt("dve", thr_t[:, p, 0:1], rgs, 0.5, mns, ALU.mult, ALU.add, [Bmm[p]], [Bthr[p]])
                memset("dve", cnt_t[:, p, :], 0.0, [Bcd[p]])
                memset("dve", cact_t[:, p, :], 0.0, [Bca[p]])
                for n in range(NIT):
                    ts("dve", negm[:, 0:Lh], score[:, 0:Lh], thr_t[:, p, n:n + 1], 0.0, ALU.is_gt, ALU.add,
                       [Bscore, Bthr[p]], [Bjd[p], Bcd[p]], accum_out=cnt_t[:, p, n:n + 1])
                    act_(negm[:, Lh:L], score[:, Lh:L], AF.Sign, [Bscore, Bthr[p]], [Bja[p], Bca[p]],
                         bias=thr_t[:, p, n:n + 1], scale=-1.0, accum_out=cact_t[:, p, n:n + 1])
                    stt("dve", uu_t[:, p, n:n + 1], cnt_t[:, p, n:n + 1], 2.0, cact_t[:, p, n:n + 1], ALU.mult, ALU.subtract,
                        [Bcd[p], Bca[p]], [Bst[p]])
                    ts("dve", dd_t[:, p, n:n + 1], uu_t[:, p, n:n + 1], float(2 * TOPK - La), stp2_t[:, p, n:n + 1],
                       ALU.is_gt, ALU.mult, [Bst[p]], [Bst[p]])
                    stt("dve", thr_t[:, p, n + 1:n + 2], dd_t[:, p, n:n + 1], stp_t[:, p, n:n + 1], thr_t[:, p, n:n + 1],
                        ALU.subtract, ALU.add, [Bst[p], Bthr[p]], [Bthr[p]])
                    yield
            ts("dve", negm[:, 0:L], score[:, 0:L], thr_t[:, p, NIT:NIT + 1], -30000.0, ALU.is_le, ALU.mult,
               [Bscore, Bthr[p]], [Bnegm2[p], Bjd[p], Bja[p]])

        def phase_b(s):
            p = s % 2
            qi_ = t * 4 + s
            nb = qi_ + 1
            tq = slice(s * 128, (s + 1) * 128)
            negm = negm2[:, p, :]
            (oa, boa), (ob, bob) = acc_pair()
            memset("dve", qz[:], 0.0, [Bqz])
            cp("dve", qz[0:64, :, 0, :], qT[0:64, :, tq], [BqT], [Bqz])
            cp("dve", qz[64:128, :, 1, :], qT[64:128, :, tq], [BqT], [Bqz])
            units = [(c, g0) for c in range(4) for g0 in range(0, nb, 2)]

            def emit_lt(u):
                c, g0 = u
                ng = min(2, nb - g0)
                lt, blt = ring_bank("b")
                for j in range(ng):
                    kb = g0 + j
                    mm(lt[:, j * 256:(j + 1) * 256], kT[:, c, kb * 128:(kb + 1) * 128],
                       qz[:, c, :, :].rearrange("p a t -> p (a t)"), True, False, [BkT, Bqz], [blt])
                    mm(lt[:, j * 256:(j + 1) * 256], negm[:, kb * 128:(kb + 1) * 128], ident2[:, 0:256], False, True,
                       [Bnegm2[p], Bconst], [blt])
                return lt, blt

            cur = emit_lt(units[0])
            for i, (c, g0) in enumerate(units):
                lt, blt = cur
                if i + 1 < len(units):
                    cur = emit_lt(units[i + 1])
                ng = min(2, nb - g0)
                p_ = i % 2
                act_(PT[:, p_, 0:ng * 256], lt[:, 0:ng * 256], AF.Exp, [blt], [BPT[p_]])
                for j in range(ng):
                    kb = g0 + j
                    for hh, (ob_, bob_) in enumerate(((oa, boa), (ob, bob))):
                        h = 2 * c + hh
                        mm(ob_[:, c * 65:c * 65 + 65], PT[:, p_, j * 256 + hh * 128:j * 256 + (hh + 1) * 128],
                           vc[:, kb, h * 65:(h + 1) * 65], kb == 0, kb == nb - 1, [BPT[p_], Bvc], [bob_])
                yield
            yield "epilogue"
            for half, (o_, bo_) in enumerate(((oa, boa), (ob, bob))):
                ov = o_[:, 0:260].rearrange("p (h d) -> p h d", h=4)
                rd = sv[:, RD + half * 4:RD + half * 4 + 4]
                recip3(rd.unsqueeze(2), ov[:, :, 64:65], [bo_], [Bsv])
                tt("dve", att[:].rearrange("p (c hh d) -> p c hh d", c=4, hh=2)[:, :, half, :], ov[:, :, 0:64],
                   rd.unsqueeze(2).to_broadcast([128, 4, 64]), ALU.mult, [bo_, Bsv], [Batt])
            ptb, bptb = ring_bank()
            ptbb = ptb[:].bitcast(BF16)
            for c in range(4):
                tr(ptbb[:, c * 128:(c + 1) * 128], att[:, c * 128:(c + 1) * 128], [Batt, Bconst], [bptb])
            cp("act", mixT[:, 4:8, tq], ptbb[:, 0:512].rearrange("p (c t) -> p c t", c=4), [bptb], [BmixT])

        def drain(g):
            for _ in g:
                pass

        def interleave(ga, gb, ratio):
            a_alive, b_alive = True, True
            acc = 0.0
            while a_alive or b_alive:
                if a_alive:
                    try:
                        next(ga)
                    except StopIteration:
                        a_alive = False
                acc += ratio if a_alive else 1e9
                while b_alive and acc >= 1.0:
                    acc -= 1.0
                    try:
                        r = next(gb)
                        if r == "epilogue":
                            b_alive = False
                    except StopIteration:
                        b_alive = False
                if not b_alive:
                    acc = 0.0
            drain(ga)
            drain(gb)

        drain(phase_a(0))
        for s in range(4):
            gb = phase_b(s)
            if s + 1 < 4:
                nb_a = t * 4 + s + 2
                steps_a = 2 * ((nb_a * 128 + 511) // 512) + (NIT if nb_a > 2 else 0)
                steps_b = 4 * ((t * 4 + s + 1 + 1) // 2)
                interleave(phase_a(s + 1), gb, max(1.0, steps_b / max(1, steps_a)))
            else:
                drain(gb)
        out_proj_residual(s_w_out0, cv["w_out0"], mixT, BmixT, 0, seq, 2)
        dump("mixT", mixT[:].rearrange("p c t -> p (c t)"), [BmixT])
        dump("x1", xt[:].rearrange("p c t -> p (c t)"), Bxt)
        P.handoff(attn_bufs, Bact0)
        norm_to_hT(0, 1, seq)
        ffn_gate_up(s_ffn_g, s_ffn_u, cv["ffn_g"], cv["ffn_u"], act0, Bact0)
        load_mod(0, 5, seq)
        wd_v = s_ffn_d.rearrange("(j p) n -> p j n", p=128)
        for q in range(6):
            nj = min(4, NJ - q * 4)
            slot, bslot = wslot()
            v = slot.rearrange("p (j n) -> p j n", j=4)
            P.dma("sp", v[:, 0:nj, :], wd_v[:, q * 4:q * 4 + nj, :], reads=cv["ffn_d"], writes=[bslot])
            for jj in range(nj):
                j = q * 4 + jj
                for s in range(4):
                    for hlf in range(2):
                        bk = 2 * s + hlf
                        mm(banks[bk][:], act0[:, j, s * 128:(s + 1) * 128], v[:, jj, hlf * 512:(hlf + 1) * 512], j == 0,
                           j == NJ - 1, [Bact0[j], bslot], [Bbank[bk]])
        for s in range(4):
            for hlf in range(2):
                bk = 2 * s + hlf
                tt("dve", ntmp[:, hlf * 512:(hlf + 1) * 512], banks[bk][:], modt[:, hlf * 512:(hlf + 1) * 512], ALU.mult,
                   [Bbank[bk], Bmod], [Bntmp])
            tt("dve", xt[:, s, :], xt[:, s, :], ntmp[:], ALU.add, [Bxt[s], Bntmp], [Bxt[s]])
        return store_x(row0)

    def layer1_tile(seq, t, src):
        row0 = seq * S + t * T
        load_x(src, row0)
        norm_to_hT(1, 0, seq)
        if t == 0:
            memset("pool", zh[:], 0.0, [Bzh])
        for c in range(8):
            if c % 4 == 0:
                vb, bb = load_cols(s_sc_in, cv["sc_in"], c * 128, 512)
                vc_, bc_ = load_cols(s_sc_in, cv["sc_in"], D + c * 128, 512)
                vv_, bv_ = load_cols(s_sc_in, cv["sc_in"], 2 * D + c * 128, 512)
            cc = c % 4
            b0 = (c % 2) * 4
            (pbg, bpbg), (pcg, bpcg), (pvv, bpvv) = ((banks[b0 + i], Bbank[b0 + i]) for i in range(3))
            for (pp, bpp, vw, bw_) in ((pbg, bpbg, vb, bb), (pcg, bpcg, vc_, bc_), (pvv, bpvv, vv_, bv_)):
                for kc in range(8):
                    mm(pp[:], vw[:, kc, cc * 128:(cc + 1) * 128], hT[:, kc, :], kc == 0, kc == 7, [bw_, BhT], [bpp])
            cp("act", vsb[:], pvv[:], [bpvv], [Bvsb])
            cp("dve", zt[:, 0:2], zh[:, c, :], [Bzh], [Bzt])
            tt("dve", zt[:, 2:514], pcg[:], vsb[:], ALU.mult, [bpcg, Bvsb], [Bzt])
            cp("dve", zh[:, c, :], zt[:, 512:514], [Bzt], [Bzh])
            ts("dve", c3[:], zt[:, 2:514], sccw[:, c, 2:3], None, ALU.mult, None, [Bzt, Bpar], [Bc3])
            stt("dve", c3[:], zt[:, 1:513], sccw[:, c, 1:2], c3[:], ALU.mult, ALU.add, [Bzt, Bpar, Bc3], [Bc3])
            stt("dve", c3[:], zt[:, 0:512], sccw[:, c, 0:1], c3[:], ALU.mult, ALU.add, [Bzt, Bpar, Bc3], [Bc3])
            tt("dve", mT[:, c, :], pbg[:], c3[:], ALU.mult, [bpbg, Bc3], [BmT])
        out_proj_residual(s_sc_out, cv["sc_out"], mT, BmT, 1, seq, 2)
        norm_to_hT(1, 1, seq, router=True)
        for s in range(4):
            lg = sv[:, LG + s * 8:LG + s * 8 + 8]
            gt_ = sv[:, GT + s * 8:GT + s * 8 + 8]
            top = sv[:, W12 + 0:W12 + 8]
            P.op("dve", lambda e, lg=lg, top=top: e.max(out=top, in_=lg), [Bsv], [Bsv])
            tt("dve", sv[:, W12 + 8:W12 + 9], sv[:, W12 + 0:W12 + 1], sv[:, W12 + 1:W12 + 2], ALU.subtract, [Bsv], [Bsv])
            act_(sv[:, W12 + 9:W12 + 10], sv[:, W12 + 8:W12 + 9], AF.Sigmoid, [Bsv], [Bsv])
            ts("dve", sv[:, W12 + 10:W12 + 11], sv[:, W12 + 9:W12 + 10], -1.0, 1.0, ALU.mult, ALU.add, [Bsv], [Bsv])
            ts("dve", gt_, lg, sv[:, W12 + 0:W12 + 1], sv[:, W12 + 9:W12 + 10], ALU.is_equal, ALU.mult, [Bsv], [Bsv])
            ts("dve", sv[:, W12 + 11:W12 + 19], lg, sv[:, W12 + 1:W12 + 2], sv[:, W12 + 10:W12 + 11], ALU.is_equal, ALU.mult,
               [Bsv], [Bsv])
            tt("dve", gt_, gt_, sv[:, W12 + 11:W12 + 19], ALU.add, [Bsv], [Bsv])
        load_mod(1, 5, seq)
        for e_ in range(NE):
            wd_v = s_moe_d[e_].rearrange("(j p) n -> p j n", p=128)
            cvd = cv[f"moe_d{e_}"]
            for q in range(0, NJ, 2):
                P.dma("pool", wdres[:, q:q + 2, :], wd_v[:, q:q + 2, :], reads=cvd, writes=[Bwd[q // 2]])
            ffn_gate_up(s_moe_g[e_], s_moe_u[e_], cv[f"moe_g{e_}"], cv[f"moe_u{e_}"], act1, Bact1)
            for s in range(4):
                (pa, ba), (pb_, bb_) = acc_pair()
                for j in range(NJ):
                    for hlf, (pbk, bbk) in enumerate(((pa, ba), (pb_, bb_))):
                        mm(pbk[:], act1[:, j, s * 128:(s + 1) * 128], wdres[:, j, hlf * 512:(hlf + 1) * 512], j == 0,
                           j == NJ - 1, [Bact1[j], Bwd[j // 2]], [bbk])
                for hlf, (pbk, bbk) in enumerate(((pa, ba), (pb_, bb_))):
                    stt("dve", ntmp[:, hlf * 512:(hlf + 1) * 512], pbk[:], sv[:, GT + s * 8 + e_:GT + s * 8 + e_ + 1],
                        modt[:, hlf * 512:(hlf + 1) * 512], ALU.mult, ALU.mult, [bbk, Bsv, Bmod], [Bntmp])
                tt("dve", xt[:, s, :], xt[:, s, :], ntmp[:], ALU.add, [Bxt[s], Bntmp], [Bxt[s]])
        return store_x(row0)

    final = []
    if DO_L0:
        per_tile_jobs = (len(l1_conv_jobs) + NTILES - 1) // max(1, NTILES)
        for seq in range(NSEQ):
            for t in range(NTILES):
                toks = layer0_tile(seq, t)
                if seq == 0:
                    run_l1_conv(per_tile_jobs)
                final = toks if not DO_L1 else final
                if not DO_L1:
                    final_all = final
    run_l1_conv(len(l1_conv_jobs))
    Bystore = Buf("ystore")
    if DO_L1:
        P.handoff(l0_persist + conv_bufs + attn_bufs + Bact0, l1_bufs)
        P.dma("sp", wr32, router_d.rearrange("(k p) e -> p k e", p=128), writes=[BwrB])
        cp("dve", wr_hi, wr32, [BwrB], [BwrB])
        tt("dve", wrtmp, wr32, wr_hi, ALU.subtract, [BwrB], [BwrB])
        cp("dve", wr_lo, wrtmp, [BwrB], [BwrB])
        src = y_d if DO_L0 else x_d
        for seq in range(NSEQ):
            for t in range(NTILES):
                final = layer1_tile(seq, t, src)
    for lane in range(NLANES):
        v = P.lane_val.get(("pool", lane), 0)
        if v:
            P.wait_tok("sp", ("dma", "pool", lane, v))
    P.build()
    es.close()
    nc._dbg_list = dbg_list
    return nc


def _prep_shared(inp):
    f = lambda a: np.ascontiguousarray(a, dtype=np.float32)
    d = {}
    d["ada_w"] = f(inp["ada_w"])
    d["ada_b"] = f(inp["ada_b"])
    d["norm_g"] = f(inp["norm_g"].reshape(4, D))
    d["w_in0"] = f(inp["ab_w_in"][0])
    d["convwT"] = f(inp["ab_conv_w"][0].T)
    d["convb"] = f(inp["ab_conv_b"][0].reshape(4, 128).T)
    d["cng"] = f(inp["ab_cnorm_g"][0].reshape(4, 128).T)
    d["cnb"] = f(inp["ab_cnorm_b"][0].reshape(4, 128).T)
    d["qg2"] = f(np.concatenate([inp["ab_q_g"][0], inp["ab_q_g"][0]]).reshape(128, 1))
    d["kg2"] = f(np.concatenate([inp["ab_k_g"][0], inp["ab_k_g"][0]]).reshape(128, 1))
    d["w_out0"] = f(inp["ab_w_out"][0])
    d["ffn_g"] = f(inp["ffn_w_gate"][0])
    d["ffn_u"] = f(inp["ffn_w_up"][0])
    d["ffn_d"] = f(inp["ffn_w_down"][0])
    d["sc_in"] = f(inp["sc_w_in"][0])
    d["sc_cwT"] = f(inp["sc_conv_w"][0].T)
    d["sc_out"] = f(inp["sc_w_out"][0])
    d["router"] = f(inp["moe_router"][0])
    d["moe_g"] = f(inp["moe_w_gate"][0])
    d["moe_u"] = f(inp["moe_w_up"][0])
    d["moe_d"] = f(inp["moe_w_down"][0])
    return d


def run(inputs, n_cores=8, nseq=2, **bk):
    shared = _prep_shared(inputs)
    x = np.asarray(inputs["x"], dtype=np.float32)
    c = np.asarray(inputs["c"], dtype=np.float32)
    nc = build_program(NSEQ=nseq, **bk)
    in_maps = []
    for i in range(n_cores):
        m = dict(shared)
        m["x"] = np.ascontiguousarray(x[i * nseq:(i + 1) * nseq].reshape(nseq * S, D))
        m["cT"] = np.ascontiguousarray(c[i * nseq:(i + 1) * nseq].T)
        in_maps.append(m)
    res = run_bass_kernel_spmd(nc, in_maps, core_ids=list(range(n_cores)))
    outs = [np.asarray(r["y"]).reshape(nseq, S, D) for r in res.results]
    return np.concatenate(outs, axis=0).astype(np.float32)


def kernel(**inputs):
    return run(inputs, n_cores=8, nseq=2)
```

```python
import numpy as np
from contextlib import ExitStack
import concourse.bass as bass
import concourse.mybir as mybir
from concourse.bass_utils import run_bass_kernel_spmd

F32 = mybir.dt.float32
BF16 = mybir.dt.bfloat16
ALU = mybir.AluOpType
AF = mybir.ActivationFunctionType
AX = mybir.AxisListType

D = 1024
S = 4096
T = 512
NT = S // T
DFF = 2816
NJ = DFF // 128
INW = 3144
NE = 8
EPS = 1e-6
NIT = 16
TOPK = 256

CH = 4096
NLANES = 8


class Buf:
    __slots__ = ("name", "w", "r")

    def __init__(self, name=""):
        self.name = name
        self.w = None
        self.r = {}


class Prog:
    ENG = ("pe", "act", "dve", "pool", "sp")

    def __init__(self, nc):
        self.nc = nc
        self.q = {e: [] for e in self.ENG}
        self.n = {e: 0 for e in self.ENG}
        self.seen = {e: {} for e in self.ENG}
        self.sems = {}
        self.lane_rr = {e: 0 for e in self.ENG}
        self.lane_val = {}
        self.used_lanes = set()

    @staticmethod
    def _kv(t):
        if t[0] == "eng":
            return ("eng", t[1]), t[2]
        return ("dma", t[1], t[2]), t[3]

    def _collect(self, eng, reads, writes):
        out = {}

        def add(kind, t):
            key, val = self._kv(t)
            if t[0] == "eng" and t[1] == eng:
                if eng == "pe" or kind != "raw":
                    return
            if self.seen[eng].get(key, -1) >= val:
                return
            if out.get(key, -1) < val:
                out[key] = val

        for b in reads:
            if b.w is not None:
                add("raw", b.w)
        for b in writes:
            if b.w is not None:
                add("waw", b.w)
            for t in b.r.values():
                add("war", t)
        return out

    def _emit_waits(self, eng, waits):
        for key, val in waits.items():
            self.seen[eng][key] = val
            if key[0] == "eng":
                seg, off = divmod(val, CH)
                self.q[eng].append(("wait", ("eng", key[1], seg), off + 1))
            else:
                self.q[eng].append(("wait", key, val))

    def _update(self, tok, reads, writes):
        key, _ = self._kv(tok)
        for b in reads:
            b.r[key] = tok
        for b in writes:
            b.w = tok
            b.r = {}

    def op(self, eng, fn, reads=(), writes=()):
        self._emit_waits(eng, self._collect(eng, reads, writes))
        n = self.n[eng]
        self.n[eng] = n + 1
        self.q[eng].append(("op", fn, ("eng", eng, n // CH)))
        tok = ("eng", eng, n)
        self._update(tok, reads, writes)
        return tok

    def dma(self, eng, out, in_, reads=(), writes=(), **kw):
        lane = self.lane_rr[eng]
        self.lane_rr[eng] = (lane + 1) % NLANES
        waits = self._collect(eng, reads, writes)
        prev = self.lane_val.get((eng, lane), 0)
        key = ("dma", eng, lane)
        if prev > 0 and self.seen[eng].get(key, -1) < prev:
            waits[key] = max(waits.get(key, -1), prev)
        self._emit_waits(eng, waits)
        v = prev + 16
        self.lane_val[(eng, lane)] = v
        self.used_lanes.add((eng, lane))
        self.q[eng].append(("dma", out, in_, kw, key))
        tok = ("dma", eng, lane, v)
        self._update(tok, reads, writes)
        return tok

    def wait_tok(self, eng, tok):
        key, val = self._kv(tok)
        if self.seen[eng].get(key, -1) >= val:
            return
        self._emit_waits(eng, {key: val})

    def handoff(self, old, new):
        toks = {}
        for b in old:
            cand = list(b.r.values())
            if b.w is not None:
                cand.append(b.w)
            for t in cand:
                key, val = self._kv(t)
                if key not in toks or self._kv(toks[key])[1] < val:
                    toks[key] = t
        for b in new:
            for key, t in toks.items():
                if key not in b.r or self._kv(b.r[key])[1] < self._kv(t)[1]:
                    b.r[key] = t

    def build(self):
        nc = self.nc
        with ExitStack() as es:
            for e in self.ENG:
                nseg = (self.n[e] + CH - 1) // CH
                for s in range(nseg):
                    self.sems[("eng", e, s)] = es.enter_context(nc.semaphore(f"c_{e}_{s}"))
            for (e, lane) in sorted(self.used_lanes):
                self.sems[("dma", e, lane)] = es.enter_context(nc.semaphore(f"d_{e}_{lane}"))
            block = es.enter_context(nc.Block())
            sems = self.sems

            def run(engobj, items):
                for it in items:
                    if it[0] == "wait":
                        engobj.wait_ge(sems[it[1]], it[2])
                    elif it[0] == "op":
                        it[1](engobj).then_inc(sems[it[2]], 1)
                    else:
                        _, out, in_, kw, key = it
                        engobj.dma_start(out=out, in_=in_, **kw).then_inc(sems[key], 16)

            @block.tensor
            def _(e):
                run(e, self.q["pe"])

            @block.scalar
            def _(e):
                run(e, self.q["act"])

            @block.vector
            def _(e):
                run(e, self.q["dve"])

            @block.gpsimd
            def _(e):
                run(e, self.q["pool"])

            @block.sync
            def _(e):
                run(e, self.q["sp"])


def build_program(NSEQ=2, DO_L0=True, DO_L1=True, NTILES=NT, DEBUG=False):
    nc = bass.Bass("TRN2", target_bir_lowering=False)
    P = Prog(nc)
    NTOK = NSEQ * S

    def din(name, shape):
        return nc.dram_tensor(name, shape, F32, kind="ExternalInput").ap()

    def dscr(name, shape, dt=BF16):
        return nc.dram_tensor(name, shape, dt, kind="Internal").ap()

    x_d = din("x", [NTOK, D])
    cT_d = din("cT", [D, NSEQ])
    ada_w_d = din("ada_w", [2, D, 6 * D])
    ada_b_d = din("ada_b", [2, 6 * D])
    norm_g_d = din("norm_g", [4, D])
    w_in0_d = din("w_in0", [D, INW])
    convw_d = din("convwT", [512, 31])
    convb_d = din("convb", [128, 4])
    cng_d = din("cng", [128, 4])
    cnb_d = din("cnb", [128, 4])
    qg_d = din("qg2", [128, 1])
    kg_d = din("kg2", [128, 1])
    w_out0_d = din("w_out0", [D, D])
    ffn_g_d = din("ffn_g", [D, DFF])
    ffn_u_d = din("ffn_u", [D, DFF])
    ffn_d_d = din("ffn_d", [DFF, D])
    sc_in_d = din("sc_in", [D, 3 * D])
    sc_cw_d = din("sc_cwT", [D, 3])
    sc_out_d = din("sc_out", [D, D])
    router_d = din("router", [D, NE])
    if DO_L1:
        moe_g_d = din("moe_g", [NE, D, DFF])
        moe_u_d = din("moe_u", [NE, D, DFF])
        moe_d_d = din("moe_d", [NE, DFF, D])
    y_d = nc.dram_tensor("y", [NTOK, D], F32, kind="ExternalOutput").ap()

    s_w_in0 = dscr("s_w_in0", [D, INW])
    s_w_out0 = dscr("s_w_out0", [D, D])
    s_ffn_g = dscr("s_ffn_g", [D, DFF])
    s_ffn_u = dscr("s_ffn_u", [D, DFF])
    s_ffn_d = dscr("s_ffn_d", [DFF, D])
    s_sc_in = dscr("s_sc_in", [D, 3 * D])
    s_sc_out = dscr("s_sc_out", [D, D])
    s_moe_g = dscr("s_moe_g", [NE, D, DFF])
    s_moe_u = dscr("s_moe_u", [NE, D, DFF])
    s_moe_d = dscr("s_moe_d", [NE, DFF, D])
    modv = dscr("modv", [2, 6, NSEQ, D], F32)

    es = ExitStack()

    def sb(name, shape, dt):
        return es.enter_context(nc.sbuf_tensor(name, shape, dt))

    xt = sb("xt", [128, 4, D], F32)
    Bxt = [Buf(f"xt{i}") for i in range(4)]
    hT = sb("hT", [128, 8, T], BF16)
    BhT = Buf("hT")
    wring = sb("wring", [128, 4, 4096], BF16)
    Bw = [Buf(f"w{i}") for i in range(4)]
    modt = sb("modt", [128, D], F32)
    Bmod = Buf("mod")
    modt2 = sb("modt2", [128, D], F32)
    Bmod2 = Buf("mod2")
    ident = sb("ident", [128, 128], BF16)
    Bconst = Buf("const")
    idf = sb("idf", [128, 128], F32)
    bd = sb("bd", [128, 128], BF16)
    qz = sb("qz", [128, 4, 2, 128], BF16)
    Bqz = Buf("qz")
    onesD = sb("onesD", [128, 128], BF16)
    cneg = sb("cneg", [128, 128], F32)
    sil = sb("sil", [128, 2, T], BF16)
    Bsil = [Buf("sil0"), Buf("sil1")]
    ntmp = sb("ntmp", [128, D], F32)
    Bntmp = Buf("ntmp")
    nbf = sb("nbf", [128, D], BF16)
    Bnbf = Buf("nbf")
    sv = sb("sv", [128, 256], F32)
    Bsv = Buf("sv")
    cw = sb("cw", [128, 4, 31], F32)
    cb = sb("cb", [128, 4], F32)
    cng = sb("cng_s", [128, 4], F32)
    cnb = sb("cnb_s", [128, 4], F32)
    qg = sb("qg_s", [128, 1], F32)
    kg = sb("kg_s", [128, 1], F32)
    sccw = sb("sccw", [128, 8, 3], F32)
    Bpar = Buf("par")
    NBIG = 66048
    big = sb("big", [128, NBIG], BF16)

    def carve(off_bytes, nbytes, dt, pat=None, **kw):
        a = big[:, off_bytes // 2:(off_bytes + nbytes) // 2]
        if dt == F32:
            a = a.bitcast(F32)
        if pat:
            a = a.rearrange(pat, **kw)
        return a

    K = 1024
    o = 0
    kT = carve(o, 32 * K, BF16, "p (c s) -> p c s", c=4); o += 32 * K
    vc = carve(o, 32 * 8 * 65 * 2, BF16, "p (b f) -> p b f", b=32); o += 32 * 8 * 65 * 2
    o = (o + 63) // 64 * 64
    kiT = carve(o, 8 * K, BF16); o += 8 * K
    qT = carve(o, 4 * K, BF16, "p (c s) -> p c s", c=4); o += 4 * K
    qiT = carve(o, 4 * K, BF16, "p (c s) -> p c s", c=4); o += 4 * K
    mixT = carve(o, 8 * K, BF16, "p (c s) -> p c s", c=8); o += 8 * K
    yhist = carve(o, 4 * 32 * 2, BF16, "p (c s) -> p c s", c=4); o += 4 * 32 * 2
    wi_t = carve(o, 4 * 8 * 4, F32, "p (c s) -> p c s", c=4); o += 4 * 8 * 4
    W0 = o
    assert W0 + 40 * K <= NBIG * 2, (W0, NBIG * 2)
    BkT, Bvc, BkiT, BqT, BqiT, BmixT, Byh, Bwi = (Buf(n) for n in
                                                   ("kT", "vc", "kiT", "qT", "qiT", "mixT", "yh", "wi"))
    o = W0
    yv = carve(o, 4 * 544 * 2, BF16, "p (c s) -> p c s", c=4); o += 4 * 544 * 2
    dg = carve(o, 2 * 31 * 128 * 2, BF16, "p (b j s) -> p b j s", b=2, j=31); o += 2 * 31 * 128 * 2
    a_bf = carve(o, 4 * K, BF16, "p (c s) -> p c s", c=4); o += 4 * K
    a2_bf = carve(o, 4 * K, BF16, "p (c s) -> p c s", c=4); o += 4 * K
    mean_sb = carve(o, 2 * K, F32); o += 2 * K
    rstd_sb = carve(o, 2 * K, F32); o += 2 * K
    sgm = carve(o, 2 * K, F32); o += 2 * K
    assert o <= W0 + 40 * K, o - W0
    Byv, Babf, Ba2, Bmean, Brstd, Bsgm = (Buf(n) for n in ("yv", "abf", "a2", "mean", "rstd", "sgm"))
    Bdg = [[Buf(f"dg{b}_{j}") for j in range(31)] for b in range(2)]
    conv_bufs = [Byv, Babf, Ba2, Bmean, Brstd, Bsgm] + Bdg[0] + Bdg[1]
    o = W0
    score = carve(o, 16 * K, F32); o += 16 * K
    negm2 = carve(o, 16 * K, BF16, "p (c s) -> p c s", c=2); o += 16 * K
    rl = carve(o, 4 * K, F32, "p (c s) -> p c s", c=2); o += 4 * K
    PT = carve(o, 3 * K, BF16, "p (c s) -> p c s", c=3); o += 3 * K
    att = carve(o, 1 * K, BF16); o += 1 * K
    assert o <= W0 + 40 * K
    Bscore, Brl0, Brl1, BPT0, BPT1, BPT2, Batt = (Buf(n) for n in ("score", "rl0", "rl1", "PT0", "PT1", "PT2", "att"))
    Bnegm2 = [Buf("negm0"), Buf("negm1")]
    Bjd = [Buf("jd0"), Buf("jd1")]
    Bja = [Buf("ja0"), Buf("ja1")]
    attn_bufs = [Bscore, Brl0, Brl1, BPT0, BPT1, BPT2, Batt] + Bnegm2 + Bjd + Bja
    act0 = carve(W0, 22 * K, BF16, "p (j s) -> p j s", j=NJ)
    Bact0 = [Buf(f"act0_{j}") for j in range(NJ)]
    l0_persist = [BkT, Bvc, BkiT, BqT, BqiT, BmixT, Byh, Bwi]
    o = 0
    wr32 = carve(o, 256, F32, "p (k e) -> p k e", e=NE)
    wr_hi = carve(o + 256, 128, BF16, "p (k e) -> p k e", e=NE)
    wr_lo = carve(o + 384, 128, BF16, "p (k e) -> p k e", e=NE)
    wrtmp = carve(o + 512, 256, F32, "p (k e) -> p k e", e=NE)
    lobf = carve(o + 1 * K, 2 * K, BF16)
    hTlo = carve(o + 3 * K, 2 * K, BF16, "p (c t) -> p c t", c=8)
    o += 32 * K
    Blo, BhTlo = Buf("lobf"), Buf("hTlo")
    act1 = carve(o, 22 * K, BF16, "p (j s) -> p j s", j=NJ); o += 22 * K
    mT = carve(o, 8 * K, BF16, "p (c s) -> p c s", c=8); o += 8 * K
    wdres = carve(o, 44 * K, BF16, "p (j n) -> p j n", j=NJ); o += 44 * K
    zt = carve(o, 516 * 4, F32); o += 516 * 4
    vsb = carve(o, 2 * K, F32); o += 2 * K
    c3 = carve(o, 2 * K, F32); o += 2 * K
    zh = carve(o, 8 * 2 * 4, F32, "p (c s) -> p c s", c=8); o += 64
    prod = carve(o, 8 * K, F32, "p (c s) -> p c s", c=2); o += 8 * K
    assert o <= NBIG * 2, o
    BwrB, BmT, Bzt, Bvsb, Bc3, Bzh = (Buf(n) for n in ("wrB", "mT", "zt", "vsb", "c3", "zh"))
    Bprod = [Buf("prod0"), Buf("prod1")]
    Bwd = [Buf(f"wd{j}") for j in range(NJ // 2)]
    Bact1 = [Buf(f"act1_{j}") for j in range(NJ)]
    l1_bufs = [BwrB, BmT, Bzt, Bvsb, Bc3, Bzh, Blo, BhTlo] + Bprod + Bact1 + Bwd

    SS, RS, MX, MN, RNG, LG, GT, W12, RD = 0, 4, 8, 9, 10, 16, 48, 80, 96
    NB2 = NIT + 2
    thr_t = sb("thr_t", [128, 2, NB2], F32)
    cnt_t = sb("cnt_t", [128, 2, NB2], F32)
    cact_t = sb("cact_t", [128, 2, NB2], F32)
    uu_t = sb("uu_t", [128, 2, NB2], F32)
    stp_t = sb("stp_t", [128, 2, NB2], F32)
    stp2_t = sb("stp2_t", [128, 2, NB2], F32)
    dd_t = sb("dd_t", [128, 2, NB2], F32)
    Bthr = [Buf("thr0"), Buf("thr1")]
    Bcd = [Buf("cd0"), Buf("cd1")]
    Bca = [Buf("ca0"), Buf("ca1")]
    Bst = [Buf("st0"), Buf("st1")]
    Bmm = [Buf("mm0"), Buf("mm1")]
    pw_t = sb("pw_t", [128, NIT + 2], F32)
    epsc = sb("epsc", [128, 1], F32)
    Bbis = Buf("bis")

    banks = [es.enter_context(nc.psum_tensor(f"ps{i}", [128, 512], F32)) for i in range(8)]
    Bbank = [Buf(f"bank{i}") for i in range(8)]
    st = {"ring": 0, "acc": 0, "w": 0}

    st.update({"ra": 0, "rb": 0})

    def ring_bank(group=None):
        if group == "a":
            i = 4 + st["ra"] % 2
            st["ra"] += 1
        elif group == "b":
            i = (2, 3, 6, 7)[st["rb"] % 4]
            st["rb"] += 1
        else:
            i = 4 + st["ring"] % 4
            st["ring"] += 1
        return banks[i], Bbank[i]

    def acc_pair():
        i = (st["acc"] % 2) * 2
        st["acc"] += 1
        return (banks[i], Bbank[i]), (banks[i + 1], Bbank[i + 1])

    def wslot():
        i = st["w"] % 4
        st["w"] += 1
        return wring[:, i, :], Bw[i]

    def mm(out, lhsT, rhs, start, stop, reads, writes):
        P.op("pe", lambda e: e.matmul(out, lhsT=lhsT, rhs=rhs, start=start, stop=stop), reads, writes)

    def recip3(out, in_, reads, writes):
        P.op("dve", lambda e: e.reciprocal(out=out, in_=in_), reads, writes)

    def tr(out, in_, reads, writes):
        P.op("pe", lambda e: e.transpose(out, in_, ident[:]), reads, writes)

    def act_(out, in_, func, reads, writes, **kw):
        P.op("act", lambda e: e.activation(out=out, in_=in_, func=func, **kw), reads, writes)

    def tt(eng, out, in0, in1, op, reads, writes):
        P.op(eng, lambda e: e.tensor_tensor(out=out, in0=in0, in1=in1, op=op), reads, writes)

    def ts(eng, out, in0, s1, s2, op0, op1, reads, writes, accum_out=None):
        if op1 is None:
            P.op(eng, lambda e: e.tensor_scalar(out=out, in0=in0, scalar1=s1, scalar2=None, op0=op0), reads, writes)
        elif accum_out is None:
            P.op(eng, lambda e: e.tensor_scalar(out=out, in0=in0, scalar1=s1, scalar2=s2, op0=op0, op1=op1), reads, writes)
        else:
            P.op(eng, lambda e: e.tensor_scalar(out=out, in0=in0, scalar1=s1, scalar2=s2, op0=op0, op1=op1,
                                                accum_out=accum_out), reads, writes)

    def stt(eng, out, in0, scalar, in1, op0, op1, reads, writes):
        P.op(eng, lambda e: e.scalar_tensor_tensor(out=out, in0=in0, scalar=scalar, in1=in1, op0=op0, op1=op1),
             reads, writes)

    def cp(eng, out, in_, reads, writes):
        if eng == "act":
            act_(out, in_, AF.Identity, reads, writes)
        else:
            P.op(eng, lambda e: e.tensor_copy(out=out, in_=in_), reads, writes)

    def memset(eng, ap, val, writes):
        P.op(eng, lambda e: e.memset(ap, val), (), writes)

    dbg_list = []

    def dump(name, ap, bufs):
        if not DEBUG:
            return
        if any(n == name for n, _ in dbg_list):
            return
        shp = list(ap.shape)
        dt_ = ap.dtype
        dten = nc.dram_tensor("dbg_" + name, shp, dt_, kind="ExternalOutput").ap()
        dbg_list.append((name, shp))
        P.dma("sp", dten, ap, reads=bufs)

    memset("pool", idf[:], 0.0, [Bconst])
    P.op("pool", lambda e: e.affine_select(out=idf[:], in_=idf[:], pattern=[[-1, 128]], compare_op=ALU.not_equal,
                                           fill=1.0, base=0, channel_multiplier=1), [Bconst], [Bconst])
    cp("dve", ident[:], idf[:], [Bconst], [Bconst])
    ident2 = idf[:].bitcast(BF16)
    cp("dve", ident2[:, 0:128], ident[:], [Bconst], [Bconst])
    cp("dve", ident2[:, 128:256], ident[:], [Bconst], [Bconst])
    memset("dve", bd[:], 0.0, [Bconst])
    memset("dve", bd[0:64, 0:64], 1.0 / 64, [Bconst])
    memset("dve", bd[64:128, 64:128], 1.0 / 64, [Bconst])
    memset("dve", onesD[:], 1.0 / 512, [Bconst])
    memset("dve", cneg[:], 0.0, [Bconst])
    memset("dve", cneg[0:64, 64:128], -1e30, [Bconst])
    memset("dve", epsc[:], EPS, [Bconst])
    for n in range(NIT + 2):
        memset("dve", pw_t[:, n:n + 1], 2.0 ** -(n + 2), [Bconst])
    P.dma("sp", cw[:], convw_d.rearrange("(c p) j -> p c j", p=128), writes=[Bpar])
    P.dma("sp", cb[:], convb_d, writes=[Bpar])
    P.dma("sp", cng[:], cng_d, writes=[Bpar])
    P.dma("sp", cnb[:], cnb_d, writes=[Bpar])
    P.dma("sp", qg[:], qg_d, writes=[Bpar])
    P.dma("sp", kg[:], kg_d, writes=[Bpar])
    P.dma("sp", sccw[:], sc_cw_d.rearrange("(c p) j -> p c j", p=128), writes=[Bpar])
    ts("dve", qg[:], qg[:], 0.125, None, ALU.mult, None, [Bpar], [Bpar])

    def convert(src, dst, rows, blk=256):
        bufs = []
        for r0 in range(0, rows, blk):
            r1 = min(rows, r0 + blk)
            b = Buf("cv")
            P.dma("pool", dst[r0:r1, :], src[r0:r1, :], writes=[b])
            bufs.append(b)
        return bufs

    cv = {}
    if DO_L0:
        cv["w_in0"] = convert(w_in0_d, s_w_in0, D)
        cv["w_out0"] = convert(w_out0_d, s_w_out0, D)
        cv["ffn_g"] = convert(ffn_g_d, s_ffn_g, D)
        cv["ffn_u"] = convert(ffn_u_d, s_ffn_u, D)
        cv["ffn_d"] = convert(ffn_d_d, s_ffn_d, DFF)
    l1_conv_jobs = []
    if DO_L1:
        l1_conv_jobs.append(("sc_in", sc_in_d, s_sc_in, D))
        l1_conv_jobs.append(("sc_out", sc_out_d, s_sc_out, D))
        for e in range(NE):
            l1_conv_jobs.append((f"moe_g{e}", moe_g_d[e], s_moe_g[e], D))
            l1_conv_jobs.append((f"moe_u{e}", moe_u_d[e], s_moe_u[e], D))
            l1_conv_jobs.append((f"moe_d{e}", moe_d_d[e], s_moe_d[e], DFF))

    def run_l1_conv(njobs):
        for _ in range(njobs):
            if l1_conv_jobs:
                name, src, dst, rows = l1_conv_jobs.pop(0)
                cv[name] = convert(src, dst, rows)

    cs32 = ntmp[:, 0:8 * NSEQ].rearrange("p (k s) -> p k s", k=8)
    csb = nbf[:, 0:8 * NSEQ].rearrange("p (k s) -> p k s", k=8)
    P.dma("sp", cs32, cT_d.rearrange("(k p) s -> p k s", p=128), writes=[Bntmp], allow_slow_non_contiguous=True)
    act_(csb, cs32, AF.Silu, [Bntmp], [Bnbf])
    modrow = big[0:NSEQ, 0:12288].bitcast(F32)
    gt2 = big[0:NSEQ, 12288:12288 + 4096].bitcast(F32)
    bias2 = big[0:NSEQ, 16384:16384 + 12288].bitcast(F32)
    Bmodrow, Bg2, Bb2 = Buf("modrow"), Buf("g2"), Buf("b2")
    Bmodv = Buf("modv")
    layers = ([0] if DO_L0 else []) + ([1] if DO_L1 else [])
    for l in layers:
        P.dma("sp", bias2, ada_b_d[l:l + 1, :].partition_broadcast(NSEQ), writes=[Bb2])
        P.dma("sp", gt2[:, 0:D], norm_g_d[2 * l:2 * l + 1, :].partition_broadcast(NSEQ), writes=[Bg2])
        P.dma("sp", gt2[:, D:2 * D], norm_g_d[2 * l + 1:2 * l + 2, :].partition_broadcast(NSEQ), writes=[Bg2])
        for nchunk in range(12):
            slot, bslot = wslot()
            sv3 = slot.rearrange("p (k n) -> p k n", k=8)
            P.dma("pool", sv3, ada_w_d[l].rearrange("(k p) n -> p k n", p=128)[:, :, nchunk * 512:(nchunk + 1) * 512],
                  writes=[bslot])
            pb, bpb = ring_bank()
            for kc in range(8):
                mm(pb[0:NSEQ, :], csb[:, kc, :], sv3[:, kc, :], kc == 0, kc == 7, [Bnbf, bslot], [bpb])
            tt("dve", modrow[:, nchunk * 512:(nchunk + 1) * 512], pb[0:NSEQ, :],
               bias2[:, nchunk * 512:(nchunk + 1) * 512], ALU.add, [bpb, Bb2], [Bmodrow])
        stt("dve", modrow[:, D:2 * D], modrow[:, D:2 * D], 1.0, gt2[:, 0:D], ALU.add, ALU.mult, [Bmodrow, Bg2], [Bmodrow])
        stt("dve", modrow[:, 4 * D:5 * D], modrow[:, 4 * D:5 * D], 1.0, gt2[:, D:2 * D], ALU.add, ALU.mult,
            [Bmodrow, Bg2], [Bmodrow])
        for k, src in enumerate((1, 0, 2, 4, 3, 5)):
            P.dma("sp", modv[l, k, :, :], modrow[:, src * D:(src + 1) * D], reads=[Bmodrow], writes=[Bmodv])
    P.handoff([Bmodrow, Bg2, Bb2], l0_persist + conv_bufs + l1_bufs)

    def load_mod(l, k, seq, second=False):
        if second:
            P.dma("sp", modt2[:], modv[l, k, seq:seq + 1, :].partition_broadcast(128), reads=[Bmodv], writes=[Bmod2])
        else:
            P.dma("sp", modt[:], modv[l, k, seq:seq + 1, :].partition_broadcast(128), reads=[Bmodv], writes=[Bmod])

    By = {}

    def load_x(src, row0):
        for s in range(4):
            r = row0 + s * 128
            rd = [By[r]] if (src is y_d and r in By) else []
            P.dma("pool", xt[:, s, :], src[r:r + 128, :], reads=rd, writes=[Bxt[s]])

    def store_x(row0):
        toks = []
        for s in range(4):
            r = row0 + s * 128
            By.setdefault(r, Buf(f"y{r}"))
            toks.append(P.dma("pool", y_d[r:r + 128, :], xt[:, s, :], reads=[Bxt[s]], writes=[By[r]]))
        return toks

    def norm_to_hT(l, which, seq, router=False):
        memset("dve", sv[:, SS:SS + 4], 0.0, [Bsv])
        if router:
            memset("dve", sv[:, LG:LG + 32], 0.0, [Bsv])
        for s in range(4):
            act_(nbf[:], xt[:, s, :], AF.Square, [Bxt[s]], [Bnbf, Bsv], accum_out=sv[:, SS + s:SS + s + 1])
        act_(sv[:, RS:RS + 4], sv[:, SS:SS + 4], AF.Sqrt, [Bsv, Bconst], [Bsv], bias=epsc[:, 0:1], scale=1.0 / D)
        P.op("dve", lambda e: e.reciprocal(out=sv[:, RS:RS + 4], in_=sv[:, RS:RS + 4]), [Bsv], [Bsv])
        load_mod(l, 3 * which + 0, seq)
        load_mod(l, 3 * which + 1, seq, second=True)
        for s in range(4):
            stt("dve", ntmp[:], xt[:, s, :], sv[:, RS + s:RS + s + 1], modt[:], ALU.mult, ALU.mult,
                [Bxt[s], Bsv, Bmod], [Bntmp])
            dump("modA", modt[:], [Bmod])
            dump("sv0", sv[:, 0:16], [Bsv])
            dump("ntmp0", ntmp[:], [Bntmp])
            if router:
                tt("dve", ntmp[:], ntmp[:], modt2[:], ALU.add, [Bntmp, Bmod2], [Bntmp])
                cp("act", nbf[:], ntmp[:], [Bntmp], [Bnbf])
                tt("dve", lobf, ntmp[:], nbf[:], ALU.subtract, [Bntmp, Bnbf], [Blo])
            else:
                tt("dve", nbf[:], ntmp[:], modt2[:], ALU.add, [Bntmp, Bmod2], [Bnbf])
            pb, bpb = ring_bank()
            pbb = pb[:].bitcast(BF16)
            for c in range(8):
                tr(pbb[:, c * 128:(c + 1) * 128], nbf[:, c * 128:(c + 1) * 128], [Bnbf, Bconst], [bpb])
            cp("act", hT[:, :, s * 128:(s + 1) * 128], pbb[:, 0:1024].rearrange("p (c t) -> p c t", c=8), [bpb], [BhT])
            if router:
                pl, bpl = ring_bank()
                plb = pl[:].bitcast(BF16)
                for c in range(8):
                    tr(plb[:, c * 128:(c + 1) * 128], lobf[:, c * 128:(c + 1) * 128], [Blo, Bconst], [bpl])
                cp("dve", hTlo, plb[:, 0:1024].rearrange("p (c t) -> p c t", c=8), [bpl], [BhTlo])
                pr, bpr = ring_bank()
                n_mm = 0
                for kc in range(8):
                    for (lh, blh, rw) in ((hT[:, kc, s * 128:(s + 1) * 128], BhT, wr_hi[:, kc, :]),
                                          (hTlo[:, kc, :], BhTlo, wr_hi[:, kc, :]),
                                          (hT[:, kc, s * 128:(s + 1) * 128], BhT, wr_lo[:, kc, :])):
                        mm(pr[:, 0:8], lh, rw, n_mm == 0, n_mm == 23, [blh, BwrB], [bpr])
                        n_mm += 1
                cp("dve", sv[:, LG + s * 8:LG + s * 8 + 8], pr[:, 0:8], [bpr], [Bsv])
            if s == 3:
                dump("hT", hT[:].rearrange("p c t -> p (c t)"), [BhT])

    def load_cols(Wbf, cvb, c0, w):
        slot, bslot = wslot()
        v = slot.rearrange("p (k n) -> p k n", k=8)
        P.dma("sp", v[:, :, 0:w], Wbf.rearrange("(k p) n -> p k n", p=128)[:, :, c0:c0 + w], reads=cvb, writes=[bslot])
        return v, bslot

    def out_proj_residual(Wbf, cvb, srcT, BsrcT, l, seq, which_gate):
        halves = []
        for hlf in range(2):
            halves.append(load_cols(Wbf, cvb, hlf * 512, 512))
        load_mod(l, which_gate, seq)
        for s in range(4):
            (pa, ba), (pb_, bb_) = acc_pair()
            for hlf, (pbk, bbk) in enumerate(((pa, ba), (pb_, bb_))):
                v, bslot = halves[hlf]
                for kc in range(8):
                    mm(pbk[:], srcT[:, kc, s * 128:(s + 1) * 128], v[:, kc, :], kc == 0, kc == 7, [BsrcT, bslot], [bbk])
                tt("dve", ntmp[:, hlf * 512:(hlf + 1) * 512], pbk[:], modt[:, hlf * 512:(hlf + 1) * 512], ALU.mult,
                   [bbk, Bmod], [Bntmp])
            tt("dve", xt[:, s, :], xt[:, s, :], ntmp[:], ALU.add, [Bxt[s], Bntmp], [Bxt[s]])

    def ffn_gate_up(Wg, Wu, cvg, cvu, actv, Bact):
        for q in range(6):
            c0 = q * 512
            w = min(512, DFF - c0)
            vg, bg = load_cols(Wg, cvg, c0, w)
            vu, bu = load_cols(Wu, cvu, c0, w)
            for jj in range(w // 128):
                j = q * 4 + jj
                pg, bpg = ring_bank()
                pu, bpu = ring_bank()
                for kc in range(8):
                    mm(pg[:], vg[:, kc, jj * 128:(jj + 1) * 128], hT[:, kc, :], kc == 0, kc == 7, [bg, BhT], [bpg])
                for kc in range(8):
                    mm(pu[:], vu[:, kc, jj * 128:(jj + 1) * 128], hT[:, kc, :], kc == 0, kc == 7, [bu, BhT], [bpu])
                si = j % 2
                act_(sil[:, si, :], pg[:], AF.Silu, [bpg], [Bsil[si]])
                tt("dve", actv[:, j, :], pu[:], sil[:, si, :], ALU.mult, [bpu, Bsil[si]], [Bact[j]])

    def layer0_tile(seq, t):
        row0 = seq * S + t * T
        tok0 = t * T
        P.handoff(Bact0, conv_bufs)
        load_x(x_d, row0)
        norm_to_hT(0, 0, seq)
        if t == 0:
            memset("pool", yhist[:], 0.0, [Byh])
            if seq == 0:
                memset("pool", vc[:], 1.0, [Bvc])
        va, ba = load_cols(s_w_in0, cv["w_in0"], 0, 512)
        vg, bg = load_cols(s_w_in0, cv["w_in0"], 512, 512)
        cp("pool", yv[:, :, 0:30], yhist[:, :, 0:30], [Byh], [Byv])
        for c in range(4):
            pa, bpa = ring_bank()
            pg, bpg = ring_bank()
            for kc in range(8):
                mm(pa[:], va[:, kc, c * 128:(c + 1) * 128], hT[:, kc, :], kc == 0, kc == 7, [ba, BhT], [bpa])
            for kc in range(8):
                mm(pg[:], vg[:, kc, c * 128:(c + 1) * 128], hT[:, kc, :], kc == 0, kc == 7, [bg, BhT], [bpg])
            act_(sgm[:], pg[:], AF.Sigmoid, [bpg], [Bsgm])
            tt("dve", yv[:, c, 30:542], pa[:], sgm[:], ALU.mult, [bpa, Bsgm], [Byv])
        cp("pool", yhist[:, :, 0:30], yv[:, :, 512:542], [Byv], [Byh])
        dump("yv", yv[:].rearrange("p c t -> p (c t)"), [Byv])
        for c in range(4):
            db = c % 2
            for j in range(31):
                if j % 2 == 0:
                    ts("dve", dg[:, db, j, :], ident[:], cw[:, c, j:j + 1], None, ALU.mult, None, [Bconst, Bpar], [Bdg[db][j]])
                else:
                    act_(dg[:, db, j, :], ident[:], AF.Identity, [Bconst, Bpar], [Bdg[db][j]], scale=cw[:, c, j:j + 1])
            pc, bpc = ring_bank()
            for j in range(31):
                mm(pc[:], dg[:, db, j, :], yv[:, c, j:j + 512], j == 0, j == 30, [Bdg[db][j], Byv], [bpc])
            act_(a_bf[:, c, :], pc[:], AF.Identity, [bpc, Bpar], [Babf], bias=cb[:, c:c + 1], scale=1.0)
            act_(a2_bf[:, c, :], pc[:], AF.Square, [bpc, Bpar], [Ba2], bias=cb[:, c:c + 1], scale=1.0)
        for which, (c0, dstT, Bdst, gain, col0) in enumerate(((1024, qT, BqT, qg, 0), (1536, kT, BkT, kg, tok0))):
            vq, bq = load_cols(s_w_in0, cv["w_in0"], c0, 512)
            for c in range(4):
                pq, bpq = ring_bank()
                for kc in range(8):
                    mm(pq[:], vq[:, kc, c * 128:(c + 1) * 128], hT[:, kc, :], kc == 0, kc == 7, [bq, BhT], [bpq])
                si = c % 2
                act_(sil[:, si, :], pq[:], AF.Square, [bpq], [Bsil[si]])
                pm, bpm = ring_bank()
                mm(pm[:], bd[:], sil[:, si, :], True, True, [Bconst, Bsil[si]], [bpm])
                act_(ntmp[:, 0:512], pm[:], AF.Sqrt, [bpm, Bconst], [Bntmp], bias=epsc[:, 0:1], scale=1.0)
                P.op("dve", lambda e: e.reciprocal(out=ntmp[:, 0:512], in_=ntmp[:, 0:512]), [Bntmp], [Bntmp])
                stt("dve", dstT[:, c, col0:col0 + 512], pq[:], gain[:, 0:1], ntmp[:, 0:512], ALU.mult, ALU.mult,
                    [bpq, Bpar, Bntmp], [Bdst])
        vv, bv = load_cols(s_w_in0, cv["w_in0"], 2048, 512)
        for s in range(4):
            pv, bpv = ring_bank()
            for kc in range(8):
                mm(pv[:], hT[:, kc, s * 128:(s + 1) * 128], vv[:, kc, :], kc == 0, kc == 7, [BhT, bv], [bpv])
            dst = vc[:, t * 4 + s, :].rearrange("p (h d) -> p h d", h=8)[:, :, 0:64]
            cp("act", dst, pv[:].rearrange("p (h d) -> p h d", h=8), [bpv], [Bvc])
        vqi, bqi = load_cols(s_w_in0, cv["w_in0"], 2560, 512)
        for c in range(4):
            pq, bpq = ring_bank()
            for kc in range(8):
                mm(pq[:], vqi[:, kc, c * 128:(c + 1) * 128], hT[:, kc, :], kc == 0, kc == 7, [bqi, BhT], [bpq])
            cp("act", qiT[:, c, :], pq[:], [bpq], [BqiT])
        slot, bslot = wslot()
        vk = slot.rearrange("p (k n) -> p k n", k=8)
        srcw = s_w_in0.rearrange("(k p) n -> p k n", p=128)
        P.dma("sp", vk[:, :, 0:64], srcw[:, :, 3072:3136], reads=cv["w_in0"], writes=[bslot])
        P.dma("sp", vk[:, :, 64:128], srcw[:, :, 3072:3136], reads=cv["w_in0"], writes=[bslot])
        P.dma("sp", vk[:, :, 128:136], srcw[:, :, 3136:3144], reads=cv["w_in0"], writes=[bslot])
        pk, bpk = ring_bank()
        for kc in range(8):
            mm(pk[:], vk[:, kc, 0:128], hT[:, kc, :], kc == 0, kc == 7, [bslot, BhT], [bpk])
        cp("act", kiT[:, tok0:tok0 + 512], pk[:], [bpk], [BkiT])
        pw, bpw = ring_bank()
        for s in range(4):
            for kc in range(8):
                mm(pw[:, s * 8:(s + 1) * 8], hT[:, kc, s * 128:(s + 1) * 128], vk[:, kc, 128:136], kc == 0, kc == 7,
                   [bslot, BhT], [bpw])
        ts("dve", wi_t[:, :, :], pw[:, 0:32].rearrange("p (s e) -> p s e", s=4), float(512 ** -0.5), None, ALU.mult, None,
           [bpw], [Bwi])
        pm, bpm = ring_bank()
        pq2, bpq2 = ring_bank()
        for c in range(4):
            mm(pm[:], onesD[:], a_bf[:, c, :], c == 0, c == 3, [Bconst, Babf], [bpm])
        for c in range(4):
            mm(pq2[:], onesD[:], a2_bf[:, c, :], c == 0, c == 3, [Bconst, Ba2], [bpq2])
        cp("act", mean_sb[:], pm[:], [bpm], [Bmean])
        tt("dve", rstd_sb[:], mean_sb[:], mean_sb[:], ALU.mult, [Bmean], [Brstd])
        tt("dve", rstd_sb[:], pq2[:], rstd_sb[:], ALU.subtract, [bpq2, Brstd], [Brstd])
        ts("dve", rstd_sb[:], rstd_sb[:], 0.0, None, ALU.max, None, [Brstd], [Brstd])
        act_(rstd_sb[:], rstd_sb[:], AF.Sqrt, [Brstd, Bconst], [Brstd], bias=epsc[:, 0:1], scale=1.0)
        P.op("dve", lambda e: e.reciprocal(out=rstd_sb[:], in_=rstd_sb[:]), [Brstd], [Brstd])
        for c in range(4):
            tt("dve", sgm[:], a_bf[:, c, :], mean_sb[:], ALU.subtract, [Babf, Bmean], [Bsgm])
            tt("dve", sgm[:], sgm[:], rstd_sb[:], ALU.mult, [Bsgm, Brstd], [Bsgm])
            act_(mixT[:, c, :], sgm[:], AF.Silu, [Bsgm, Bpar], [BmixT], bias=cnb[:, c:c + 1], scale=cng[:, c:c + 1])
        dump("abf", a_bf[:].rearrange("p c t -> p (c t)"), [Babf])
        dump("rstd", rstd_sb[:], [Brstd])
        dump("mean", mean_sb[:], [Bmean])
        dump("mixA", mixT[:, 0:4, :], [BmixT])
        dump("qT", qT[:].rearrange("p c t -> p (c t)"), [BqT])
        dump("kT", kT[:, :, 0:512], [BkT])
        dump("kiT", kiT[:, 0:512], [BkiT])
        dump("qiT", qiT[:].rearrange("p c t -> p (c t)"), [BqiT])
        dump("wi", wi_t[:].rearrange("p c t -> p (c t)"), [Bwi])
        dump("vc", vc[:, 0:4, :], [Bvc])
        P.handoff(conv_bufs, attn_bufs)
        Brl = [Brl0, Brl1]
        BPT = [BPT0, BPT1, BPT2]

        def phase_a(s):
            p = s % 2
            qi_ = t * 4 + s
            nb = qi_ + 1
            L = nb * 128
            tq = slice(s * 128, (s + 1) * 128)
            negm = negm2[:, p, :]
            P.handoff([Bnegm2[p]], [Bjd[p], Bja[p]])
            ri = 0
            for k0 in range(0, L, 512):
                w = min(512, L - k0)
                for h in range(8):
                    c, hp = h // 2, h % 2
                    pd, bpd = ring_bank("a")
                    mm(pd[:, 0:w], qiT[hp * 64:(hp + 1) * 64, c, tq], kiT[hp * 64:(hp + 1) * 64, k0:k0 + w], True, True,
                       [BqiT, BkiT], [bpd])
                    r = ri % 2
                    ri += 1
                    act_(rl[:, r, 0:w], pd[:, 0:w], AF.Relu, [bpd], [Brl[r]])
                    if h == 0:
                        ts("dve", score[:, k0:k0 + w], rl[:, r, 0:w], wi_t[:, s, 0:1], None, ALU.mult, None,
                           [Brl[r], Bwi], [Bscore])
                    else:
                        stt("dve", score[:, k0:k0 + w], rl[:, r, 0:w], wi_t[:, s, h:h + 1], score[:, k0:k0 + w],
                            ALU.mult, ALU.add, [Brl[r], Bwi, Bscore], [Bscore])
                    if h % 4 == 3:
                        yield
            mxs = sv[:, 112 + p * 4:112 + p * 4 + 1]
            mns = sv[:, 113 + p * 4:113 + p * 4 + 1]
            rgs = sv[:, 114 + p * 4:114 + p * 4 + 1]
            if nb > 2:
                P.op("dve", lambda e: e.tensor_reduce(out=mxs, in_=score[:, 0:L], axis=AX.X, op=ALU.max), [Bscore], [Bmm[p]])
                P.op("dve", lambda e: e.tensor_reduce(out=mns, in_=score[:, 0:L], axis=AX.X, op=ALU.min), [Bscore], [Bmm[p]])
            tt("dve", score[:, L - 128:L], score[:, L - 128:L], cneg[:], ALU.add, [Bscore, Bconst], [Bscore])
            if nb <= 2:
                memset("dve", thr_t[:, p, NIT:NIT + 1], -1e29, [Bthr[p]])
            else:
                Lh = (nb // 2) * 128
                La = L - Lh
                tt("dve", rgs, mxs, mns, ALU.subtract, [Bmm[p]], [Bmm[p]])
                ts("dve", stp_t[:, p, :], pw_t[:, :], rgs, None, ALU.mult, None, [Bmm[p], Bconst], [Bst[p]])
                ts("dve", stp2_t[:, p, :], stp_t[:, p, :], 2.0, None, ALU.mult, None, [Bst[p]], [Bst[p]])
                stt("dve", thr_t[:, p, 0:1], rgs, 0.5, mns, ALU.mult, ALU.add, [Bmm[p]], [Bthr[p]])
                memset("dve", cnt_t[:, p, :], 0.0, [Bcd[p]])
                memset("dve", cact_t[:, p, :], 0.0, [Bca[p]])
                for n in range(NIT):
                    ts("dve", negm[:, 0:Lh], score[:, 0:Lh], thr_t[:, p, n:n + 1], 0.0, ALU.is_gt, ALU.add,
                       [Bscore, Bthr[p]], [Bjd[p], Bcd[p]], accum_out=cnt_t[:, p, n:n + 1])
                    act_(negm[:, Lh:L], score[:, Lh:L], AF.Sign, [Bscore, Bthr[p]], [Bja[p], Bca[p]],
                         bias=thr_t[:, p, n:n + 1], scale=-1.0, accum_out=cact_t[:, p, n:n + 1])
                    stt("dve", uu_t[:, p, n:n + 1], cnt_t[:, p, n:n + 1], 2.0, cact_t[:, p, n:n + 1], ALU.mult, ALU.subtract,
                        [Bcd[p], Bca[p]], [Bst[p]])
                    ts("dve", dd_t[:, p, n:n + 1], uu_t[:, p, n:n + 1], float(2 * TOPK - La), stp2_t[:, p, n:n + 1],
                       ALU.is_gt, ALU.mult, [Bst[p]], [Bst[p]])
                    stt("dve", thr_t[:, p, n + 1:n + 2], dd_t[:, p, n:n + 1], stp_t[:, p, n:n + 1], thr_t[:, p, n:n + 1],
                        ALU.subtract, ALU.add, [Bst[p], Bthr[p]], [Bthr[p]])
                    yield
            ts("dve", negm[:, 0:L], score[:, 0:L], thr_t[:, p, NIT:NIT + 1], -30000.0, ALU.is_le, ALU.mult,
               [Bscore, Bthr[p]], [Bnegm2[p], Bjd[p], Bja[p]])

        def phase_b(s):
            p = s % 2
            qi_ = t * 4 + s
            nb = qi_ + 1
            tq = slice(s * 128, (s + 1) * 128)
            negm = negm2[:, p, :]
            (oa, boa), (ob, bob) = (banks[0], Bbank[0]), (banks[1], Bbank[1])
            memset("dve", qz[:], 0.0, [Bqz])
            cp("dve", qz[0:64, :, 0, :], qT[0:64, :, tq], [BqT], [Bqz])
            cp("dve", qz[64:128, :, 1, :], qT[64:128, :, tq], [BqT], [Bqz])
            units = [(c, g0) for c in range(4) for g0 in range(0, nb, 2)]

            def emit_lt(u):
                c, g0 = u
                ng = min(2, nb - g0)
                lt, blt = ring_bank("b")
                for j in range(ng):
                    kb = g0 + j
                    mm(lt[:, j * 256:(j + 1) * 256], kT[:, c, kb * 128:(kb + 1) * 128],
                       qz[:, c, :, :].rearrange("p a t -> p (a t)"), True, False, [BkT, Bqz], [blt])
                    mm(lt[:, j * 256:(j + 1) * 256], negm[:, kb * 128:(kb + 1) * 128], ident2[:, 0:256], False, True,
                       [Bnegm2[p], Bconst], [blt])
                return lt, blt

            pend = [emit_lt(u) for u in units[0:2]]
            for i, (c, g0) in enumerate(units):
                lt, blt = pend.pop(0)
                if i + 2 < len(units):
                    pend.append(emit_lt(units[i + 2]))
                ng = min(2, nb - g0)
                p_ = i % 3
                act_(PT[:, p_, 0:ng * 256], lt[:, 0:ng * 256], AF.Exp, [blt], [BPT[p_]])
                for j in range(ng):
                    kb = g0 + j
                    for hh, (ob_, bob_) in enumerate(((oa, boa), (ob, bob))):
                        h = 2 * c + hh
                        mm(ob_[:, c * 65:c * 65 + 65], PT[:, p_, j * 256 + hh * 128:j * 256 + (hh + 1) * 128],
                           vc[:, kb, h * 65:(h + 1) * 65], kb == 0, kb == nb - 1, [BPT[p_], Bvc], [bob_])
                yield
            yield "epilogue"
            for half, (o_, bo_) in enumerate(((oa, boa), (ob, bob))):
                ov = o_[:, 0:260].rearrange("p (h d) -> p h d", h=4)
                rd = sv[:, RD + half * 4:RD + half * 4 + 4]
                recip3(rd.unsqueeze(2), ov[:, :, 64:65], [bo_], [Bsv])
                tt("dve", att[:].rearrange("p (c hh d) -> p c hh d", c=4, hh=2)[:, :, half, :], ov[:, :, 0:64],
                   rd.unsqueeze(2).to_broadcast([128, 4, 64]), ALU.mult, [bo_, Bsv], [Batt])
            ptb, bptb = ring_bank()
            ptbb = ptb[:].bitcast(BF16)
            for c in range(4):
                tr(ptbb[:, c * 128:(c + 1) * 128], att[:, c * 128:(c + 1) * 128], [Batt, Bconst], [bptb])
            cp("act", mixT[:, 4:8, tq], ptbb[:, 0:512].rearrange("p (c t) -> p c t", c=4), [bptb], [BmixT])

        def drain(g):
            for _ in g:
                pass

        def interleave(ga, gb):
            a_alive, b_alive = True, True
            while a_alive or b_alive:
                if a_alive:
                    try:
                        next(ga)
                    except StopIteration:
                        a_alive = False
                if b_alive:
                    try:
                        r = next(gb)
                        if r == "epilogue":
                            b_alive = False
                    except StopIteration:
                        b_alive = False
            drain(ga)
            drain(gb)

        drain(phase_a(0))
        for s in range(4):
            gb = phase_b(s)
            if s + 1 < 4:
                interleave(phase_a(s + 1), gb)
            else:
                drain(gb)
        out_proj_residual(s_w_out0, cv["w_out0"], mixT, BmixT, 0, seq, 2)
        dump("mixT", mixT[:].rearrange("p c t -> p (c t)"), [BmixT])
        dump("x1", xt[:].rearrange("p c t -> p (c t)"), Bxt)
        P.handoff(attn_bufs, Bact0)
        norm_to_hT(0, 1, seq)
        ffn_gate_up(s_ffn_g, s_ffn_u, cv["ffn_g"], cv["ffn_u"], act0, Bact0)
        load_mod(0, 5, seq)
        wd_v = s_ffn_d.rearrange("(j p) n -> p j n", p=128)
        for q in range(6):
            nj = min(4, NJ - q * 4)
            slot, bslot = wslot()
            v = slot.rearrange("p (j n) -> p j n", j=4)
            P.dma("sp", v[:, 0:nj, :], wd_v[:, q * 4:q * 4 + nj, :], reads=cv["ffn_d"], writes=[bslot])
            for jj in range(nj):
                j = q * 4 + jj
                for s in range(4):
                    for hlf in range(2):
                        bk = 2 * s + hlf
                        mm(banks[bk][:], act0[:, j, s * 128:(s + 1) * 128], v[:, jj, hlf * 512:(hlf + 1) * 512], j == 0,
                           j == NJ - 1, [Bact0[j], bslot], [Bbank[bk]])
        for s in range(4):
            for hlf in range(2):
                bk = 2 * s + hlf
                tt("dve", ntmp[:, hlf * 512:(hlf + 1) * 512], banks[bk][:], modt[:, hlf * 512:(hlf + 1) * 512], ALU.mult,
                   [Bbank[bk], Bmod], [Bntmp])
            tt("dve", xt[:, s, :], xt[:, s, :], ntmp[:], ALU.add, [Bxt[s], Bntmp], [Bxt[s]])
        return store_x(row0)

    def layer1_tile(seq, t, src):
        row0 = seq * S + t * T
        load_x(src, row0)
        norm_to_hT(1, 0, seq)
        if t == 0:
            memset("pool", zh[:], 0.0, [Bzh])
        for c in range(8):
            if c % 4 == 0:
                vb, bb = load_cols(s_sc_in, cv["sc_in"], c * 128, 512)
                vc_, bc_ = load_cols(s_sc_in, cv["sc_in"], D + c * 128, 512)
                vv_, bv_ = load_cols(s_sc_in, cv["sc_in"], 2 * D + c * 128, 512)
            cc = c % 4
            b0 = (c % 2) * 4
            (pbg, bpbg), (pcg, bpcg), (pvv, bpvv) = ((banks[b0 + i], Bbank[b0 + i]) for i in range(3))
            for (pp, bpp, vw, bw_) in ((pbg, bpbg, vb, bb), (pcg, bpcg, vc_, bc_), (pvv, bpvv, vv_, bv_)):
                for kc in range(8):
                    mm(pp[:], vw[:, kc, cc * 128:(cc + 1) * 128], hT[:, kc, :], kc == 0, kc == 7, [bw_, BhT], [bpp])
            cp("act", vsb[:], pvv[:], [bpvv], [Bvsb])
            cp("dve", zt[:, 0:2], zh[:, c, :], [Bzh], [Bzt])
            tt("dve", zt[:, 2:514], pcg[:], vsb[:], ALU.mult, [bpcg, Bvsb], [Bzt])
            cp("dve", zh[:, c, :], zt[:, 512:514], [Bzt], [Bzh])
            ts("dve", c3[:], zt[:, 2:514], sccw[:, c, 2:3], None, ALU.mult, None, [Bzt, Bpar], [Bc3])
            stt("dve", c3[:], zt[:, 1:513], sccw[:, c, 1:2], c3[:], ALU.mult, ALU.add, [Bzt, Bpar, Bc3], [Bc3])
            stt("dve", c3[:], zt[:, 0:512], sccw[:, c, 0:1], c3[:], ALU.mult, ALU.add, [Bzt, Bpar, Bc3], [Bc3])
            tt("dve", mT[:, c, :], pbg[:], c3[:], ALU.mult, [bpbg, Bc3], [BmT])
        out_proj_residual(s_sc_out, cv["sc_out"], mT, BmT, 1, seq, 2)
        norm_to_hT(1, 1, seq, router=True)
        for s in range(4):
            lg = sv[:, LG + s * 8:LG + s * 8 + 8]
            gt_ = sv[:, GT + s * 8:GT + s * 8 + 8]
            top = sv[:, W12 + 0:W12 + 8]
            P.op("dve", lambda e, lg=lg, top=top: e.max(out=top, in_=lg), [Bsv], [Bsv])
            tt("dve", sv[:, W12 + 8:W12 + 9], sv[:, W12 + 0:W12 + 1], sv[:, W12 + 1:W12 + 2], ALU.subtract, [Bsv], [Bsv])
            act_(sv[:, W12 + 9:W12 + 10], sv[:, W12 + 8:W12 + 9], AF.Sigmoid, [Bsv], [Bsv])
            ts("dve", sv[:, W12 + 10:W12 + 11], sv[:, W12 + 9:W12 + 10], -1.0, 1.0, ALU.mult, ALU.add, [Bsv], [Bsv])
            ts("dve", gt_, lg, sv[:, W12 + 0:W12 + 1], sv[:, W12 + 9:W12 + 10], ALU.is_equal, ALU.mult, [Bsv], [Bsv])
            ts("dve", sv[:, W12 + 11:W12 + 19], lg, sv[:, W12 + 1:W12 + 2], sv[:, W12 + 10:W12 + 11], ALU.is_equal, ALU.mult,
               [Bsv], [Bsv])
            tt("dve", gt_, gt_, sv[:, W12 + 11:W12 + 19], ALU.add, [Bsv], [Bsv])
        load_mod(1, 5, seq)
        for e_ in range(NE):
            wd_v = s_moe_d[e_].rearrange("(j p) n -> p j n", p=128)
            cvd = cv[f"moe_d{e_}"]
            for q in range(0, NJ, 2):
                P.dma("pool", wdres[:, q:q + 2, :], wd_v[:, q:q + 2, :], reads=cvd, writes=[Bwd[q // 2]])
            ffn_gate_up(s_moe_g[e_], s_moe_u[e_], cv[f"moe_g{e_}"], cv[f"moe_u{e_}"], act1, Bact1)
            for s in range(4):
                (pa, ba), (pb_, bb_) = acc_pair()
                for j in range(NJ):
                    for hlf, (pbk, bbk) in enumerate(((pa, ba), (pb_, bb_))):
                        mm(pbk[:], act1[:, j, s * 128:(s + 1) * 128], wdres[:, j, hlf * 512:(hlf + 1) * 512], j == 0,
                           j == NJ - 1, [Bact1[j], Bwd[j // 2]], [bbk])
                for hlf, (pbk, bbk) in enumerate(((pa, ba), (pb_, bb_))):
                    stt("dve", ntmp[:, hlf * 512:(hlf + 1) * 512], pbk[:], sv[:, GT + s * 8 + e_:GT + s * 8 + e_ + 1],
                        modt[:, hlf * 512:(hlf + 1) * 512], ALU.mult, ALU.mult, [bbk, Bsv, Bmod], [Bntmp])
                tt("dve", xt[:, s, :], xt[:, s, :], ntmp[:], ALU.add, [Bxt[s], Bntmp], [Bxt[s]])
        return store_x(row0)

    final = []
    if DO_L0:
        per_tile_jobs = (len(l1_conv_jobs) + NTILES - 1) // max(1, NTILES)
        for seq in range(NSEQ):
            for t in range(NTILES):
                toks = layer0_tile(seq, t)
                if seq == 0:
                    run_l1_conv(per_tile_jobs)
                final = toks if not DO_L1 else final
                if not DO_L1:
                    final_all = final
    run_l1_conv(len(l1_conv_jobs))
    Bystore = Buf("ystore")
    if DO_L1:
        P.handoff(l0_persist + conv_bufs + attn_bufs + Bact0, l1_bufs)
        P.dma("sp", wr32, router_d.rearrange("(k p) e -> p k e", p=128), writes=[BwrB])
        cp("dve", wr_hi, wr32, [BwrB], [BwrB])
        tt("dve", wrtmp, wr32, wr_hi, ALU.subtract, [BwrB], [BwrB])
        cp("dve", wr_lo, wrtmp, [BwrB], [BwrB])
        src = y_d if DO_L0 else x_d
        for seq in range(NSEQ):
            for t in range(NTILES):
                final = layer1_tile(seq, t, src)
    for lane in range(NLANES):
        v = P.lane_val.get(("pool", lane), 0)
        if v:
            P.wait_tok("sp", ("dma", "pool", lane, v))
    P.build()
    es.close()
    nc._dbg_list = dbg_list
    return nc


def _prep_shared(inp):
    f = lambda a: np.ascontiguousarray(a, dtype=np.float32)
    d = {}
    d["ada_w"] = f(inp["ada_w"])
    d["ada_b"] = f(inp["ada_b"])
    d["norm_g"] = f(inp["norm_g"].reshape(4, D))
    d["w_in0"] = f(inp["ab_w_in"][0])
    d["convwT"] = f(inp["ab_conv_w"][0].T)
    d["convb"] = f(inp["ab_conv_b"][0].reshape(4, 128).T)
    d["cng"] = f(inp["ab_cnorm_g"][0].reshape(4, 128).T)
    d["cnb"] = f(inp["ab_cnorm_b"][0].reshape(4, 128).T)
    d["qg2"] = f(np.concatenate([inp["ab_q_g"][0], inp["ab_q_g"][0]]).reshape(128, 1))
    d["kg2"] = f(np.concatenate([inp["ab_k_g"][0], inp["ab_k_g"][0]]).reshape(128, 1))
    d["w_out0"] = f(inp["ab_w_out"][0])
    d["ffn_g"] = f(inp["ffn_w_gate"][0])
    d["ffn_u"] = f(inp["ffn_w_up"][0])
    d["ffn_d"] = f(inp["ffn_w_down"][0])
    d["sc_in"] = f(inp["sc_w_in"][0])
    d["sc_cwT"] = f(inp["sc_conv_w"][0].T)
    d["sc_out"] = f(inp["sc_w_out"][0])
    d["router"] = f(inp["moe_router"][0])
    d["moe_g"] = f(inp["moe_w_gate"][0])
    d["moe_u"] = f(inp["moe_w_up"][0])
    d["moe_d"] = f(inp["moe_w_down"][0])
    return d


def run(inputs, n_cores=8, nseq=2, **bk):
    shared = _prep_shared(inputs)
    x = np.asarray(inputs["x"], dtype=np.float32)
    c = np.asarray(inputs["c"], dtype=np.float32)
    nc = build_program(NSEQ=nseq, **bk)
    in_maps = []
    for i in range(n_cores):
        m = dict(shared)
        m["x"] = np.ascontiguousarray(x[i * nseq:(i + 1) * nseq].reshape(nseq * S, D))
        m["cT"] = np.ascontiguousarray(c[i * nseq:(i + 1) * nseq].T)
        in_maps.append(m)
    res = run_bass_kernel_spmd(nc, in_maps, core_ids=list(range(n_cores)))
    outs = [np.asarray(r["y"]).reshape(nseq, S, D) for r in res.results]
    return np.concatenate(outs, axis=0).astype(np.float32)


def kernel(**inputs):
    return run(inputs, n_cores=8, nseq=2)
```
